# Optimizing a Trainium2 kernel written in Bass

```python
import jax, jax.numpy as jnp
from jax import lax
import numpy as np

D_MODEL = 2048
BATCH = 1
SEQ = 8192
DEPTH = 1

PLE_DIM = 256
HG_HEADS = 8
HG_DK = 128
HG_DV = 128
HG_WIDTH = HG_HEADS * HG_DK
HG_CHUNK = 64
MLA_HEADS = 8
MLA_NOPE = 128
MLA_ROPE = 64
MLA_V = 128
MLA_Q_LORA = 512
MLA_KV_LORA = 512
MLA_QK = MLA_NOPE + MLA_ROPE
ROPE_THETA = 10000.0
ATTN_BLOCK = 128
PEER_HEADS = 8
PEER_NKEYS = 128
PEER_N = PEER_NKEYS * PEER_NKEYS
PEER_QDIM = 256
PEER_TOPK = 16
PEER_BLOCK = 128
IN_SIZES = (HG_WIDTH, HG_WIDTH, HG_WIDTH, HG_WIDTH,
            MLA_Q_LORA, MLA_KV_LORA, MLA_ROPE, D_MODEL, D_MODEL)
IN_WIDTH = 4 * HG_WIDTH + MLA_Q_LORA + MLA_KV_LORA + MLA_ROPE + 2 * D_MODEL
NORM_EPS = 1e-6
MASK_VALUE = -1e30

kernel_name = "hybrid_hgrn2_mla_peer_block"


def rms_norm(x, gain):
    xf = x.astype(jnp.float32)
    y = xf * lax.rsqrt(jnp.mean(xf * xf, axis=-1, keepdims=True) + NORM_EPS)
    return (y * gain.astype(jnp.float32)).astype(x.dtype)


def apply_rope(x, positions):
    half = x.shape[-1] // 2
    freqs = ROPE_THETA ** (-jnp.arange(half, dtype=jnp.float32) / half)
    ang = positions.astype(jnp.float32)[..., None] * freqs
    if x.ndim == 4:
        ang = ang[:, :, None, :]
    cos, sin = jnp.cos(ang), jnp.sin(ang)
    xf = x.astype(jnp.float32)
    x1, x2 = xf[..., :half], xf[..., half:]
    out = jnp.concatenate([x1 * cos - x2 * sin, x2 * cos + x1 * sin], axis=-1)
    return out.astype(x.dtype)


def hgrn2_chunk_scan(q, k, v, g):
    B, T, H, dk = q.shape
    dv = v.shape[-1]
    C = HG_CHUNK
    NC = T // C

    def to_chunks(a):
        return a.reshape(B, NC, C, H, a.shape[-1]).transpose(0, 3, 1, 2, 4)

    q, k, v, g = to_chunks(q), to_chunks(k), to_chunks(v), to_chunks(g)
    b = jnp.cumsum(g, axis=3)
    b_ref = b[:, :, :, C // 2 - 1:C // 2, :]
    q_in = q * jnp.exp(b - b_ref)
    k_in = k * jnp.exp(b_ref - b)
    A = jnp.einsum('bhncd,bhnsd->bhncs', q_in, k_in)
    causal = jnp.tril(jnp.ones((C, C), dtype=bool))
    A = jnp.where(causal, A, 0.0)
    o_intra = jnp.einsum('bhncs,bhnsv->bhncv', A, v)

    b_last = b[:, :, :, -1, :]
    k_dec = k * jnp.exp(b_last[:, :, :, None, :] - b)
    dS = jnp.einsum('bhncd,bhncv->bhndv', k_dec, v)
    decay = jnp.exp(b_last)

    def step(S, inp):
        dec_n, dS_n = inp
        return dec_n[..., None] * S + dS_n, S

    S0 = jnp.zeros((B, H, dk, dv), jnp.float32)
    _, S_prev = lax.scan(step, S0, (jnp.moveaxis(decay, 2, 0), jnp.moveaxis(dS, 2, 0)))
    S_prev = jnp.moveaxis(S_prev, 0, 2)
    o_inter = jnp.einsum('bhncd,bhndv->bhncv', q * jnp.exp(b), S_prev)
    o = o_intra + o_inter
    return o.transpose(0, 2, 3, 1, 4).reshape(B, T, H, dv)


def hgrn2_branch(hq, hf, hi, hog, lb, gain):
    B, T, _ = hq.shape
    f = lb + (1.0 - lb) * jax.nn.sigmoid(hf.astype(jnp.float32))
    g = jnp.log(f)
    k = 1.0 - f
    shp = (B, T, HG_HEADS, HG_DK)
    o = hgrn2_chunk_scan(hq.astype(jnp.float32).reshape(shp), k.reshape(shp),
                         hi.astype(jnp.float32).reshape(B, T, HG_HEADS, HG_DV), g.reshape(shp))
    o = o * lax.rsqrt(jnp.mean(o * o, axis=-1, keepdims=True) + NORM_EPS)
    o = o * gain.astype(jnp.float32).reshape(HG_HEADS, HG_DV)
    o = o.reshape(B, T, HG_HEADS * HG_DV) * jax.nn.silu(hog.astype(jnp.float32))
    return o.astype(hq.dtype)


def causal_block_attention(q, k, v):
    B, H, T, dq = q.shape
    dv = v.shape[-1]
    NB = T // ATTN_BLOCK
    scale = 1.0 / np.sqrt(dq).astype(np.float32)
    qb = q.reshape(B, H, NB, ATTN_BLOCK, dq).transpose(2, 0, 1, 3, 4)
    key_idx = jnp.arange(T)

    def one_block(args):
        q_blk, bi = args
        s = jnp.einsum('bhqd,bhkd->bhqk', q_blk, k,
                       preferred_element_type=jnp.float32) * scale
        q_idx = bi * ATTN_BLOCK + jnp.arange(ATTN_BLOCK)
        mask = key_idx[None, :] <= q_idx[:, None]
        s = jnp.where(mask, s, MASK_VALUE)
        pr = jax.nn.softmax(s, axis=-1).astype(v.dtype)
        return jnp.einsum('bhqk,bhkd->bhqd', pr, v)

    out = lax.map(one_block, (qb, jnp.arange(NB)))
    return out.transpose(1, 2, 0, 3, 4).reshape(B, H, T, dv)


def mla_branch(mq, mkv, mkr, positions, q_norm, kv_norm, w_uq, w_ukv):
    B, T, _ = mq.shape
    H = MLA_HEADS
    cq = rms_norm(mq, q_norm)
    q = (cq @ w_uq).reshape(B, T, H, MLA_QK)
    q_nope, q_rope = q[..., :MLA_NOPE], apply_rope(q[..., MLA_NOPE:], positions)
    ckv = rms_norm(mkv, kv_norm)
    kv = (ckv @ w_ukv).reshape(B, T, H, MLA_NOPE + MLA_V)
    k_nope, v = kv[..., :MLA_NOPE], kv[..., MLA_NOPE:]
    k_rope = apply_rope(mkr, positions)
    k_rope = jnp.broadcast_to(k_rope[:, :, None, :], (B, T, H, MLA_ROPE))
    qf = jnp.concatenate([q_nope, q_rope], axis=-1).transpose(0, 2, 1, 3)
    kf = jnp.concatenate([k_nope, k_rope], axis=-1).transpose(0, 2, 1, 3)
    o = causal_block_attention(qf, kf, v.transpose(0, 2, 1, 3))
    return o.transpose(0, 2, 1, 3).reshape(B, T, H * MLA_V)


def peer_ffn(h, wq, k1, k2, u_tab, v_tab):
    B, T, D = h.shape
    N = B * T
    K = PEER_TOPK
    half = PEER_QDIM // 2
    hf = h.reshape(N, D)
    q = (hf @ wq).reshape(N, PEER_HEADS, 2, half)
    s1 = jnp.einsum('nhc,kc->nhk', q[:, :, 0], k1)
    s2 = jnp.einsum('nhc,kc->nhk', q[:, :, 1], k2)
    v1, i1 = lax.top_k(s1, K)
    v2, i2 = lax.top_k(s2, K)
    cand = (v1[..., :, None] + v2[..., None, :]).reshape(N, PEER_HEADS, K * K)
    cv, ci = lax.top_k(cand, K)
    e_idx = (jnp.take_along_axis(i1, ci // K, axis=-1) * PEER_NKEYS
             + jnp.take_along_axis(i2, ci % K, axis=-1))
    gate = jax.nn.softmax(cv.astype(jnp.float32), axis=-1).astype(h.dtype)
    NB = N // PEER_BLOCK
    h_b = hf.reshape(NB, PEER_BLOCK, D)
    e_b = e_idx.reshape(NB, PEER_BLOCK, PEER_HEADS, K)
    g_b = gate.reshape(NB, PEER_BLOCK, PEER_HEADS, K)

    def one_block(args):
        hb, eb, gb = args
        z = jnp.einsum('thkd,td->thk', u_tab[eb], hb)
        a = jax.nn.gelu(z, approximate=False) * gb
        return jnp.einsum('thk,thkd->td', a, v_tab[eb])

    out = lax.map(one_block, (h_b, e_b, g_b))
    return out.reshape(B, T, D)


def setup_inputs(seed: int = 0) -> dict:
    key = jax.random.key(seed)
    ks = jax.random.split(key, 24)
    f32 = jnp.float32

    def nrm(k, shape, scale):
        return jax.random.normal(k, shape, f32) * scale

    def gain(k, shape):
        return 1.0 + 0.05 * jax.random.normal(k, shape, f32)

    L, D = DEPTH, D_MODEL
    return {
        "x": nrm(ks[0], (BATCH, SEQ, D), 1.0),
        "p": nrm(ks[1], (DEPTH, BATCH, SEQ, PLE_DIM), 1.0),
        "positions": jnp.broadcast_to(jnp.arange(SEQ, dtype=jnp.int32), (BATCH, SEQ)),
        "norm_mix": gain(ks[2], (L, D)),
        "w_in": nrm(ks[3], (L, D, IN_WIDTH), D ** -0.5),
        "lb_logits": nrm(ks[4], (DEPTH + 1, HG_WIDTH), 0.5),
        "hg_norm": gain(ks[5], (L, HG_WIDTH)),
        "mla_q_norm": gain(ks[6], (L, MLA_Q_LORA)),
        "mla_kv_norm": gain(ks[7], (L, MLA_KV_LORA)),
        "w_uq": nrm(ks[8], (L, MLA_Q_LORA, MLA_HEADS * MLA_QK), MLA_Q_LORA ** -0.5),
        "w_ukv": nrm(ks[9], (L, MLA_KV_LORA, MLA_HEADS * (MLA_NOPE + MLA_V)), MLA_KV_LORA ** -0.5),
        "w_a": nrm(ks[10], (L, HG_WIDTH, D), HG_WIDTH ** -0.5),
        "w_b": nrm(ks[11], (L, MLA_HEADS * MLA_V, D), (MLA_HEADS * MLA_V) ** -0.5),
        "w_o": nrm(ks[12], (L, D, D), D ** -0.5),
        "norm_ffn": gain(ks[13], (L, D)),
        "peer_wq": nrm(ks[14], (L, D, PEER_HEADS * PEER_QDIM), D ** -0.5),
        "peer_k1": nrm(ks[15], (L, PEER_NKEYS, PEER_QDIM // 2), (PEER_QDIM // 2) ** -0.5),
        "peer_k2": nrm(ks[16], (L, PEER_NKEYS, PEER_QDIM // 2), (PEER_QDIM // 2) ** -0.5),
        "peer_u": nrm(ks[17], (L, PEER_N, D), D ** -0.5),
        "peer_v": nrm(ks[18], (L, PEER_N, D), PEER_HEADS ** -0.5),
        "norm_ple": gain(ks[19], (L, D)),
        "w_pg": nrm(ks[20], (L, D, D), D ** -0.5),
        "w_pe": nrm(ks[21], (L, PLE_DIM, D), PLE_DIM ** -0.5),
        "norm_final": gain(ks[22], (D,)),
    }


def reference(x, p, positions, norm_mix, w_in, lb_logits, hg_norm, mla_q_norm, mla_kv_norm,
              w_uq, w_ukv, w_a, w_b, w_o, norm_ffn, peer_wq, peer_k1, peer_k2, peer_u, peer_v,
              norm_ple, w_pg, w_pe, norm_final):
    lb_all = jnp.cumsum(jax.nn.softmax(lb_logits.astype(jnp.float32), axis=0), axis=0)
    split_points = [int(s) for s in np.cumsum(IN_SIZES)[:-1]]
    for i in range(DEPTH):
        h = rms_norm(x, norm_mix[i])
        proj = h @ w_in[i]
        hq, hf, hi, hog, mq, mkv, mkr, ga, gb = jnp.split(proj, split_points, axis=-1)
        o_a = hgrn2_branch(hq, hf, hi, hog, lb_all[i], hg_norm[i])
        o_b = mla_branch(mq, mkv, mkr, positions, mla_q_norm[i], mla_kv_norm[i],
                         w_uq[i], w_ukv[i])
        y = jax.nn.sigmoid(ga) * (o_a @ w_a[i]) + jax.nn.sigmoid(gb) * (o_b @ w_b[i])
        x = x + y @ w_o[i]
        x = x + peer_ffn(rms_norm(x, norm_ffn[i]), peer_wq[i], peer_k1[i], peer_k2[i],
                         peer_u[i], peer_v[i])
        hp = rms_norm(x, norm_ple[i])
        x = x + jax.nn.sigmoid(hp @ w_pg[i]) * (p[i] @ w_pe[i])
    return rms_norm(x, norm_final)
```

```python
import numpy as np
from contextlib import ExitStack
import concourse.bass as bass
import concourse.mybir as mybir
from concourse.bass_utils import run_bass_kernel_spmd

F32 = mybir.dt.float32
BF16 = mybir.dt.bfloat16
I32 = mybir.dt.int32
AF = mybir.ActivationFunctionType
ALU = mybir.AluOpType
AX = mybir.AxisListType

ENGS = ("pe", "act", "dve", "pool", "sp")
NCORES = 8
D = 2048
KD = 16
TOWN = 1024
EPS = 1e-6
PI = float(np.pi)


class Buf:
    __slots__ = ("name", "w", "r", "excl")

    def __init__(self, name, excl=False):
        self.name = name
        self.w = {}
        self.r = {}
        self.excl = excl


class Prog:
    SAME_ENG_SYNC = True

    def __init__(self, nc, es, dma_slots=8):
        self.nc = nc
        self.es = es
        self.q = {e: [] for e in ENGS}
        self.cnt = {e: 0 for e in ENGS}
        self.sem = {e: es.enter_context(nc.semaphore("s_" + e)) for e in ENGS}
        self.K = dma_slots
        self.dsem = {}
        self.dcnt = {}
        for e in ("sp", "pool", "act"):
            self.dsem[e] = [es.enter_context(nc.semaphore(f"d_{e}{k}")) for k in range(dma_slots)]
            self.dcnt[e] = 0
        self.seen = {e: {} for e in ENGS}
        self.nbuf = 0

    def buf(self, name=None, excl=False):
        self.nbuf += 1
        return Buf(name or f"b{self.nbuf}", excl)

    def _deps(self, eng, r, w, wacc=()):
        deps = {}

        def add(k, v):
            if deps.get(k, 0) < v:
                deps[k] = v

        for b in r:
            for k, v in b.w.items():
                add(k, v)
            if b.excl:
                for k, v in b.r.items():
                    if k != eng:
                        add(k, v)
        for b in w:
            for k, v in b.w.items():
                add(k, v)
            for k, v in b.r.items():
                add(k, v)
        for b in wacc:
            for k, v in b.r.items():
                add(k, v)
        waits = []
        seen = self.seen[eng]
        for k, v in deps.items():
            if k == eng and (eng == "pe" or not self.SAME_ENG_SYNC):
                continue
            if seen.get(k, 0) >= v:
                continue
            seen[k] = v
            waits.append((k, v))
        return waits

    def _mark(self, ev, r, w, wacc=()):
        k, v = ev
        for b in r:
            if b.r.get(k, 0) < v:
                b.r[k] = v
        for b in w:
            b.w = {k: v}
            b.r = {}
        for b in wacc:
            if b.w.get(k, 0) < v:
                b.w[k] = v
            b.r = {}

    def op(self, eng, fn, r=(), w=()):
        waits = self._deps(eng, r, w)
        self.cnt[eng] += 1
        ev = (eng, self.cnt[eng])
        self.q[eng].append((waits, fn, self.sem[eng], 1))
        self._mark(ev, r, w)
        return ev

    def dma(self, eng, out, in_, r=(), w=(), wacc=(), **kw):
        waits = self._deps(eng, r, w, wacc)
        j = self.dcnt[eng]
        self.dcnt[eng] = j + 1
        k = j % self.K
        val = 16 * (j // self.K + 1)
        key = ("d", eng, k)
        if j >= self.K:
            pv = val - 16
            if self.seen[eng].get(key, 0) < pv:
                self.seen[eng][key] = pv
                waits.append((key, pv))
        self.q[eng].append((waits, lambda e: e.dma_start(out=out, in_=in_, **kw), self.dsem[eng][k], 16))
        ev = (key, val)
        self._mark(ev, r, w, wacc)
        return ev

    def barrier(self):
        targets = [(e, self.cnt[e]) for e in ENGS if self.cnt[e] > 0]
        for eng in ("sp", "pool", "act"):
            j = self.dcnt[eng]
            for k in range(self.K):
                n = (j - 1 - k) // self.K + 1 if j > k else 0
                if n > 0:
                    targets.append((("d", eng, k), 16 * n))
        for e in ENGS:
            waits = []
            for k, v in targets:
                if k == e:
                    continue
                if self.seen[e].get(k, 0) >= v:
                    continue
                self.seen[e][k] = v
                waits.append((k, v))
            if waits:
                self.q[e].append((waits, None, None, 0))

    def _semh(self, k):
        if isinstance(k, tuple):
            return self.dsem[k[1]][k[2]]
        return self.sem[k]

    def emit(self):
        nc = self.nc
        with nc.Block() as block:
            def mk(ename):
                def body(e):
                    for waits, fn, sem, inc in self.q[ename]:
                        for k, v in waits:
                            e.wait_ge(self._semh(k), v)
                        if fn is not None:
                            ins = fn(e)
                            ins.then_inc(sem, inc)
                return body

            block.tensor(mk("pe"))
            block.scalar(mk("act"))
            block.vector(mk("dve"))
            block.gpsimd(mk("pool"))
            block.sync(mk("sp"))


class T:
    __slots__ = ("t", "b")

    def __init__(self, t, b):
        self.t = t
        self.b = b

    def __getitem__(self, idx):
        return self.t[idx]


class NS:
    pass


class Ring:
    def __init__(self, tiles):
        self.tiles = tiles
        self.i = 0

    def next(self):
        t = self.tiles[self.i % len(self.tiles)]
        self.i += 1
        return t


def host_consts():
    c = {}
    c["c_ident"] = np.eye(128, dtype=np.float32)
    s = np.arange(128)[:, None]
    t = np.arange(128)[None, :]
    same = (s // 64) == (t // 64)
    U = (same & (s <= t)).astype(np.float32)
    R = (same & ((s % 64) <= 31)).astype(np.float32)
    L = same.astype(np.float32)
    c["c_tri"] = np.ascontiguousarray(np.stack([U, U - R, L - U], axis=1))
    c["c_causal"] = (s <= t).astype(np.float32)
    ind = np.zeros((128, 2), np.float32)
    ind[:64, 0] = 1.0
    ind[64:, 1] = 1.0
    c["c_ind"] = ind
    half = 32
    freqs = (10000.0 ** (-np.arange(half, dtype=np.float32) / half)).astype(np.float32)
    c["c_freq"] = np.ascontiguousarray(np.tile(freqs[None, :], (128, 2)))
    ph = np.concatenate([np.full(32, PI / 2), np.zeros(32)]).astype(np.float32)
    c["c_phase"] = np.ascontiguousarray(np.tile(ph[None, :], (128, 1)))
    return c


def build(NPREV=7, taps=(), stop_after=None, lite=False):
    NTP = NPREV * 8
    NT = NTP + 8
    nc = bass.Bass("TRN2", target_bir_lowering=False)

    def din(name, shape, dt=F32):
        return nc.dram_tensor(name, list(shape), dt, kind="ExternalInput").ap()

    xprev = din("xprev", [max(NPREV, 1) * 1024, D])
    xown = din("xown", [1024, D])
    posT = din("posT", [128, NT], I32)
    kvalid = din("kvalid", [128, NT])
    p_own = din("p_own", [1024, 256])
    w_in = din("w_in", [D, 9280])
    gT = {n: din(n, [128, 16]) for n in ("g_mix", "g_ffn", "g_ple")}
    g_final = din("g_final", [1, D])
    lbT = din("lbT", [128, 16])
    lb_logits = din("lb_logits", [2, 1024])
    g_hg = din("g_hg", [128, 8])
    g_q = din("g_q", [128, 4])
    g_kv = din("g_kv", [128, 4])
    w_uq = din("w_uq", [512, 1536])
    w_ukv = din("w_ukv", [512, 2048])
    w_a = din("w_a", [1024, D])
    w_b = din("w_b", [1024, D])
    w_o = din("w_o", [D, D])
    peer_wq = din("peer_wq", [D, D])
    k1T = din("k1T", [128, 128])
    k2T = din("k2T", [128, 128])
    peer_uT = din("peer_uT", [D, 128 if lite else 16384])
    peer_v = din("peer_v", [128 if lite else 16384, D])
    w_pg = din("w_pg", [D, D])
    w_pe = din("w_pe", [256, D])
    consts = {k: din(k, v.shape) for k, v in host_consts().items()}
    out = nc.dram_tensor("out", [1024, D], F32, kind="ExternalOutput").ap()
    tap_out = {}
    for name, shape in taps:
        tap_out[name] = nc.dram_tensor("tap_" + name, list(shape), F32, kind="ExternalOutput").ap()
    KT_s = nc.dram_tensor("KT_s", [128, NT, 8, 128], BF16).ap()
    KR_s = nc.dram_tensor("KR_s", [64, NT * 128], BF16).ap()
    V_s = nc.dram_tensor("V_s", [NT, 128, 8, 128], BF16).ap()
    xres = nc.dram_tensor("xres", [1024, D], F32).ap()

    with ExitStack() as es:
        P = Prog(nc, es)

        def sb(name, shape, dt, st=None):
            t = (st or es).enter_context(nc.sbuf_tensor(name, list(shape), dt))
            return T(t, P.buf(name))

        banks = [T(es.enter_context(nc.psum_tensor(f"bank{i}", [128, 512], F32)), P.buf(f"bank{i}", excl=True)) for i in range(8)]
        bring = Ring(banks)

        def B(l):
            return [x.b for x in l]

        def mm(o, lhsT, rhs, st, sp, r, w):
            P.op("pe", lambda e: e.matmul(o, lhsT=lhsT, rhs=rhs, start=st, stop=sp), B(r), B(w))

        def tr(o, i, ident, r, w):
            P.op("pe", lambda e: e.transpose(out=o, in_=i, identity=ident), B(r), B(w))

        def act(o, i, func, r, w, bias=None, scale=None, accum=None):
            kw = {}
            if bias is not None:
                kw["bias"] = bias
            if scale is not None:
                kw["scale"] = scale
            if accum is not None:
                kw["accum_out"] = accum
            P.op("act", lambda e: e.activation(out=o, in_=i, func=func, **kw), B(r), B(w))

        def ts(eng, o, i, s1, s2, op0, op1, r, w):
            if op1 is None:
                P.op(eng, lambda e: e.tensor_scalar(out=o, in0=i, scalar1=s1, scalar2=None, op0=op0), B(r), B(w))
            else:
                P.op(eng, lambda e: e.tensor_scalar(out=o, in0=i, scalar1=s1, scalar2=s2, op0=op0, op1=op1), B(r), B(w))

        def tt(eng, o, a, b, op, r, w):
            P.op(eng, lambda e: e.tensor_tensor(out=o, in0=a, in1=b, op=op), B(r), B(w))

        def stt(o, a, s, b, op0, op1, r, w):
            P.op("dve", lambda e: e.scalar_tensor_tensor(out=o, in0=a, scalar=s, in1=b, op0=op0, op1=op1), B(r), B(w))

        def cp(eng, o, i, r, w):
            if eng == "act":
                P.op("act", lambda e: e.copy(out=o, in_=i), B(r), B(w))
            else:
                P.op(eng, lambda e: e.tensor_copy(out=o, in_=i), B(r), B(w))

        wc_state = [0]

        def wcast(o, i, gain, r, w, engines=("act", "dve", "act", "dve", "pool")):
            eng = engines[wc_state[0] % len(engines)]
            wc_state[0] += 1
            if eng == "act":
                if gain is None:
                    cp("act", o, i, r, w)
                else:
                    act(o, i, AF.Copy, r, w, scale=gain)
            else:
                if gain is None:
                    cp(eng, o, i, r, w)
                else:
                    ts(eng, o, i, gain, None, ALU.mult, None, r, w)

        def dma(eng, o, i, r, w, wacc=(), **kw):
            P.dma(eng, o, i, B(r), B(w), B(wacc), **kw)

        def tap(name, src_ap, src_t, dst_slice=None):
            if name in tap_out:
                dst = tap_out[name] if dst_slice is None else dst_slice(tap_out[name])
                dma("pool", dst, src_ap, [src_t], [], max_dma_last_dim=2048)

        def rstd_from_ss(ss, n, tmp, rs, st_r):
            ts("dve", tmp[:], ss[:], 1.0 / n, EPS, ALU.mult, ALU.add, [ss], [tmp])
            act(tmp[:], tmp[:], AF.Sqrt, [tmp], [tmp])
            P.op("dve", lambda e: e.reciprocal(out=rs[:], in_=tmp[:]), B([tmp]), B([rs]))

        identf = sb("identf", [128, 128], F32)
        identb = sb("identb", [128, 128], BF16)
        tri = sb("tri", [128, 3, 128], F32)
        causal = sb("causal", [128, 128], F32)
        ind = sb("ind", [128, 2], F32)
        kval = sb("kval", [128, NT], F32)
        gt = {n: sb("sb_" + n, [128, 16], F32) for n in gT}
        lbc = sb("lbc", [128, 16], F32)
        ghg = sb("ghg", [128, 8], F32)
        gq = sb("gq", [128, 4], F32)
        gkv = sb("gkv", [128, 4], F32)
        dma("sp", identf[:], consts["c_ident"], [], [identf])
        dma("sp", tri[:], consts["c_tri"], [], [tri])
        dma("sp", causal[:], consts["c_causal"], [], [causal])
        dma("sp", ind[:], consts["c_ind"], [], [ind])
        dma("sp", kval[:], kvalid, [], [kval])
        for n in gT:
            dma("sp", gt[n][:], gT[n], [], [gt[n]])
        dma("sp", lbc[:], lbT, [], [lbc])
        dma("sp", ghg[:], g_hg, [], [ghg])
        dma("sp", gq[:], g_q, [], [gq])
        dma("sp", gkv[:], g_kv, [], [gkv])
        cp("dve", identb[:], identf[:], [identf], [identb])
        Utri = tri[:, 0, :]
        M1 = tri[:, 1, :]
        M2 = tri[:, 2, :]

        s1 = es.enter_context(ExitStack())
        tbl = sb("tbl", [128, NT, 64], F32, s1)
        with ExitStack() as s0:
            posi = sb("posi", [128, NT], I32, s0)
            posf = sb("posf", [128, NT], F32, s0)
            frq = sb("frq", [128, 64], F32, s0)
            phs = sb("phs", [128, 64], F32, s0)
            kk = sb("kk", [128, NT, 64], F32, s0)
            ki = sb("ki", [128, NT, 64], I32, s0)
            dma("sp", posi[:], posT, [], [posi])
            dma("sp", frq[:], consts["c_freq"], [], [frq])
            dma("sp", phs[:], consts["c_phase"], [], [phs])
            cp("dve", posf[:], posi[:], [posi], [posf])
            tt("dve", tbl[:], posf[:].unsqueeze(2).to_broadcast([128, NT, 64]),
               frq[:].unsqueeze(1).to_broadcast([128, NT, 64]), ALU.mult, [posf, frq], [tbl])
            tt("dve", tbl[:], tbl[:], phs[:].unsqueeze(1).to_broadcast([128, NT, 64]), ALU.add, [tbl, phs], [tbl])
            ts("dve", kk[:], tbl[:], 1.0 / (2 * PI), None, ALU.mult, None, [tbl], [kk])
            cp("dve", ki[:], kk[:], [kk], [ki])
            cp("dve", kk[:], ki[:], [ki], [kk])
            C1 = 6.28125
            C2 = float(2 * np.pi - 6.28125)
            stt(tbl[:], kk[:], -C1, tbl[:], ALU.mult, ALU.add, [kk, tbl], [tbl])
            stt(tbl[:], kk[:], -C2, tbl[:], ALU.mult, ALU.add, [kk, tbl], [tbl])
            ts("dve", kk[:], tbl[:], PI, -2 * PI, ALU.is_gt, ALU.mult, [tbl], [kk])
            tt("dve", tbl[:], tbl[:], kk[:], ALU.add, [tbl, kk], [tbl])
            ts("dve", kk[:], tbl[:], -PI, 2 * PI, ALU.is_lt, ALU.mult, [tbl], [kk])
            tt("dve", tbl[:], tbl[:], kk[:], ALU.add, [tbl, kk], [tbl])
            ts("dve", tbl[:], tbl[:], PI, -PI, ALU.min, ALU.max, [tbl], [tbl])
            act(tbl[:], tbl[:], AF.Sin, [tbl], [tbl])
            P.barrier()
        tap("tbl", tbl[:, NT - 1, :], tbl)

        lbrow = sb("lbrow", [128, 1024], F32, s1)
        omlrow = sb("omlrow", [128, 1024], F32, s1)
        with ExitStack() as s0:
            l1 = sb("l1", [128, 1024], F32, s0)
            dma("sp", lbrow[:], lb_logits[0:1, :].to_broadcast([128, 1024]), [], [lbrow])
            dma("sp", l1[:], lb_logits[1:2, :].to_broadcast([128, 1024]), [], [l1])
            tt("dve", lbrow[:], lbrow[:], l1[:], ALU.subtract, [lbrow, l1], [lbrow])
            act(lbrow[:], lbrow[:], AF.Sigmoid, [lbrow], [lbrow])
            ts("dve", omlrow[:], lbrow[:], -1.0, 1.0, ALU.mult, ALU.add, [lbrow], [omlrow])
            P.barrier()

        g = NS()
        g.__dict__.update(locals())
        if stop_after != "p0":
            phase1(g)
        sO = es.enter_context(ExitStack())
        g.oaT = oaT = sb("oaT", [128, 8, 1024], BF16, sO)
        g.obT = obT = sb("obT", [128, 8, 1024], BF16, sO)
        g.xres_t = T(None, P.buf("xres"))
        g.xres_dep = []
        early = stop_after is not None and (stop_after in ("p0", "p1a") or stop_after.startswith("x"))
        if not early:
            phase1b(g)
        if not early and stop_after != "p1b":
            phase1c(g)
        if not early and stop_after not in ("p1b", "p1c"):
            phase2(g)
        P.barrier()
        sO.close()
        s1.close()
        if not early and stop_after not in ("p1b", "p1c", "p2"):
            if stop_after != "nopeer":
                phase3(g)
            phase4(g)
        P.barrier()
        P.emit()
    return nc


def phase1(g):
    P, sb, mm, tr, act, ts, tt, stt, cp, dma, tap = g.P, g.sb, g.mm, g.tr, g.act, g.ts, g.tt, g.stt, g.cp, g.dma, g.tap
    NT, NTP, bring, s1 = g.NT, g.NTP, g.bring, g.s1
    identb, identf, tbl, lbrow, omlrow, ind, kval = g.identb, g.identf, g.tbl, g.lbrow, g.omlrow, g.ind, g.kval
    Utri, M1, M2, tri = g.Utri, g.M1, g.M2, g.tri
    w_in = g.w_in
    B = g.B

    es = g.es
    S = sb("S", [128, 8, 128], F32, s1)
    kmax2 = sb("kmax2", [128, 8], F32, s1)
    krmax2 = sb("krmax2", [128, 1], F32, s1)
    g.S, g.kmax2, g.krmax2 = S, kmax2, krmax2
    P.op("dve", lambda e: e.memset(S[:], 0.0), [], B([S]))
    P.op("dve", lambda e: e.memset(kmax2[:], 0.0), [], B([kmax2]))
    P.op("dve", lambda e: e.memset(krmax2[:], 0.0), [], B([krmax2]))
    kts = T(None, P.buf("KT_s"))
    krs = T(None, P.buf("KR_s"))
    vs = T(None, P.buf("V_s"))
    g.kts, g.krs, g.vs = kts, krs, vs

    with ExitStack() as sa:
        wA = sb("wA", [128, 16, 2048], BF16, sa)
        wB = sb("wB", [128, 16, 576], BF16, sa)
        wkv = sb("wkv", [128, 4, 2048], BF16, sa)
        gmix = g.gt["g_mix"]
        with ExitStack() as sl:
            stg = Ring([sb(f"stg{i}", [128, 2048], F32, sl) for i in range(2)])
            for k in range(16):
                s = stg.next()
                dma("sp", s[:, :], w_in[k * 128:(k + 1) * 128, 1024:3072], [], [s])
                g.wcast(wA[:, k, :], s[:, :], gmix[:, k:k + 1], [s, gmix], [wA])
                s = stg.next()
                dma("sp", s[:, 0:576], w_in[k * 128:(k + 1) * 128, 4608:5184], [], [s])
                g.wcast(wB[:, k, :], s[:, 0:576], gmix[:, k:k + 1], [s, gmix], [wB])
            for c in range(4):
                s = stg.next()
                dma("sp", s[:, :], g.w_ukv[c * 128:(c + 1) * 128, :], [], [s])
                g.wcast(wkv[:, c, :], s[:, :], g.gkv[:, c:c + 1], [s, g.gkv], [wkv])
            P.barrier()

        xring = Ring([sb(f"xt{i}", [128, 2048], F32, sa) for i in range(2)])
        xnring = Ring([sb(f"xn{i}", [128, 2048], BF16, sa) for i in range(2)])
        hTring = Ring([sb(f"hT{i}", [128, 16, 128], BF16, sa) for i in range(2)])
        ss = sb("ss", [128, 4], F32, sa)
        tmp = sb("tmp", [128, 4], F32, sa)
        rs = sb("rs", [128, 4], F32, sa)
        fsb = sb("fsb", [128, 1024], F32, sa)
        ksb = sb("ksb", [128, 1024], F32, sa)
        vsb = sb("vsb", [128, 1024], BF16, sa)
        e2 = sb("e2", [128, 1024], F32, sa)
        kd = [sb(f"kd{j}", [128, 1024], BF16, sa) for j in range(2)]
        dec = sb("dec", [128, 8, 2], F32, sa)
        junk = sb("junk", [128, 512], BF16, sa)
        ckvn = sb("ckvn", [128, 512], BF16, sa)
        ckvT = sb("ckvT", [128, 4, 128], BF16, sa)
        knT = Ring([sb(f"knT{i}", [128, 8, 128], BF16, sa) for i in range(2)])
        Vt = Ring([sb(f"Vt{i}", [128, 8, 128], BF16, sa) for i in range(2)])
        sq = sb("sq", [128, 8, 128], F32, sa)
        knb = sb("knb", [128, 8, 128], BF16, sa)
        kn2 = sb("kn2", [128, 8], F32, sa)
        ra = sb("ra", [128, 64], F32, sa)
        rb = sb("rb", [128, 64], F32, sa)
        krp = sb("krp", [128, 64], BF16, sa)
        mkr = sb("mkr", [128, 64], F32, sa)
        krn = sb("krn", [128, 1], F32, sa)
        krT = Ring([sb(f"krT{i}", [64, 128], BF16, sa) for i in range(2)])

        sa_flags = g.stop_after or ""
        if sa_flags == "xw":
            return
        def tile_body(t):
            own = t >= NTP
            xt = xring.next()
            src = g.xown[(t - NTP) * 128:(t - NTP + 1) * 128, :] if own else g.xprev[t * 128:(t + 1) * 128, :]
            dma("sp", xt[:], src, [], [xt])
            xn = xnring.next()
            act(xn[:], xt[:], AF.Square, [xt], [xn, ss], accum=ss[:, 0:1])
            g.rstd_from_ss(T(ss.t[:, 0:1], ss.b), D, T(tmp.t[:, 0:1], tmp.b), T(rs.t[:, 0:1], rs.b), None)
            act(xn[:], xt[:], AF.Copy, [xt, rs], [xn], scale=rs[:, 0:1])
            hT = hTring.next()
            for half in range(2):
                bk = bring.next()
                bv = bk.t[:].bitcast(BF16)
                for kk in range(8):
                    k = half * 8 + kk
                    tr(bv[:, kk * 128:(kk + 1) * 128], xn[:, k * 128:(k + 1) * 128], identb[:], [xn, identb], [bk])
                cp("dve" if half == 0 else "act", hT[:, half * 8:(half + 1) * 8, :],
                   bv.rearrange("p (k t) -> p k t", k=8), [bk], [hT])
            yield
            if not own:
                bA = [bring.next() for _ in range(4)]
                for n in range(4):
                    for k in range(16):
                        mm(bA[n][:], hT[:, k, :], wA[:, k, n * 512:(n + 1) * 512], k == 0, k == 15, [hT, wA], [bA[n]])
            bkv = bring.next()
            bkr = bring.next()
            for k in range(16):
                mm(bkv[:], hT[:, k, :], wB[:, k, 0:512], k == 0, k == 15, [hT, wB], [bkv])
            for k in range(16):
                mm(bkr[:, 0:64], hT[:, k, :], wB[:, k, 512:576], k == 0, k == 15, [hT, wB], [bkr])
            act(junk[:], bkv[:], AF.Square, [bkv], [junk, ss], accum=ss[:, 1:2])
            g.rstd_from_ss(T(ss.t[:, 1:2], ss.b), 512, T(tmp.t[:, 1:2], tmp.b), T(rs.t[:, 1:2], rs.b), None)
            act(ckvn[:], bkv[:], AF.Copy, [bkv, rs], [ckvn], scale=rs[:, 1:2])
            cp("dve", mkr[:], bkr[:, 0:64], [bkr], [mkr])
            if sa_flags == "xproj":
                return
            if not own:
                for n in range(2):
                    act(fsb[:, n * 512:(n + 1) * 512], bA[n][:], AF.Sigmoid, [bA[n]], [fsb])
                for n in range(2):
                    cp("act", vsb[:, n * 512:(n + 1) * 512], bA[2 + n][:], [bA[2 + n]], [vsb])
                tt("dve", fsb[:], fsb[:], omlrow[:], ALU.mult, [fsb, omlrow], [fsb])
                tt("dve", fsb[:], fsb[:], lbrow[:], ALU.add, [fsb, lbrow], [fsb])
                ts("dve", ksb[:], fsb[:], -1.0, 1.0, ALU.mult, ALU.add, [fsb], [ksb])
                act(fsb[:], fsb[:], AF.Ln, [fsb], [fsb])
            if sa_flags == "xhg":
                return
            bk = bring.next()
            bv = bk.t[:].bitcast(BF16)
            for c in range(4):
                tr(bv[:, c * 128:(c + 1) * 128], ckvn[:, c * 128:(c + 1) * 128], identb[:], [ckvn, identb], [bk])
            cp("dve", ckvT[:], bv[:, 0:512].rearrange("p (c t) -> p c t", c=4), [bk], [ckvT])
            if sa_flags == "xkv1":
                return
            wkv4 = wkv.t[:].rearrange("p c (h two d) -> p c h two d", h=8, two=2)
            btk = [bring.next() for _ in range(4)]
            for n in range(4):
                for c in range(4):
                    mm(btk[n][:], ckvT[:, c, :], wkv[:, c, n * 512:(n + 1) * 512], c == 0, c == 3, [ckvT, wkv], [btk[n]])
            if sa_flags == "xkv3a":
                return
            vt = Vt.next()
            for n in range(4):
                bview = btk[n][:].rearrange("p (h two d) -> p h two d", h=2, two=2)
                cp("dve" if n % 2 == 0 else "act", vt[:, 2 * n:2 * n + 2, :], bview[:, :, 1, :], [btk[n]], [vt])
                cp("act" if n % 2 == 0 else "dve", knb[:, 2 * n:2 * n + 2, :], bview[:, :, 0, :], [btk[n]], [knb])
            bkn = bring.next()
            bknv = bkn.t[:].bitcast(BF16)
            for h in range(8):
                tr(bknv[:, h * 128:(h + 1) * 128], knb[:, h, :], identb[:], [knb, identb], [bkn])
            kn = knT.next()
            cp("act", kn[:], bknv.rearrange("p (h t) -> p h t", h=8), [bkn], [kn])
            dma("pool", g.KT_s[:, t, :, :], kn[:], [kn], [], wacc=[kts])
            if sa_flags in ("xkv3b", "xkv3c"):
                return
            dma("pool", g.V_s[t, :, :, :], vt[:], [vt], [], wacc=[vs])
            if sa_flags == "xkv3":
                return
            tt("dve", sq[:], knb[:], knb[:], ALU.mult, [knb], [sq])
            if sa_flags == "xkv3d":
                return
            P.op("dve", lambda e: e.tensor_reduce(out=kn2[:], in_=sq[:], axis=AX.X, op=ALU.add), B([sq]), B([kn2]))
            tt("dve", kmax2[:], kmax2[:], kn2[:], ALU.max, [kmax2, kn2], [kmax2])
            cs = tbl[:, t, 0:32]
            sn = tbl[:, t, 32:64]
            tt("dve", ra[:].rearrange("p (two d) -> p two d", two=2), mkr[:].rearrange("p (two d) -> p two d", two=2),
               cs.unsqueeze(1).to_broadcast([128, 2, 32]), ALU.mult, [mkr, tbl], [ra])
            tt("dve", rb[:, 0:32], mkr[:, 32:64], sn, ALU.mult, [mkr, tbl], [rb])
            tt("dve", rb[:, 32:64], mkr[:, 0:32], sn, ALU.mult, [mkr, tbl], [rb])
            tt("dve", krp[:, 0:32], ra[:, 0:32], rb[:, 0:32], ALU.subtract, [ra, rb], [krp])
            tt("dve", krp[:, 32:64], ra[:, 32:64], rb[:, 32:64], ALU.add, [ra, rb], [krp])
            act(ra[:], krp[:], AF.Square, [krp], [ra, krn], accum=krn[:])
            tt("dve", krmax2[:], krmax2[:], krn[:], ALU.max, [krmax2, krn], [krmax2])
            if sa_flags == "xkv4":
                return
            bk = bring.next()
            bv = bk.t[:].bitcast(BF16)
            tr(bv[0:64, 0:128], krp[:, :], identb[:], [krp, identb], [bk])
            kr = krT.next()
            cp("act", kr[:], bv[0:64, 0:128], [bk], [kr])
            dma("pool", g.KR_s[:, t * 128:(t + 1) * 128], kr[:], [kr], [], wacc=[krs])
            if not own:
                bd = [bring.next() for _ in range(2)]
                for n in range(2):
                    mm(bd[n][:], M2, fsb[:, n * 512:(n + 1) * 512], True, True, [tri, fsb], [bd[n]])
                    act(e2[:, n * 512:(n + 1) * 512], bd[n][:], AF.Exp, [bd[n]], [e2])
                bl = bring.next()
                for h in range(8):
                    mm(bl[:, h * 2:h * 2 + 2], fsb[:, h * 128:(h + 1) * 128], ind[:], True, True, [fsb, ind], [bl])
                act(dec[:].rearrange("p h j -> p (h j)"), bl[:, 0:16], AF.Exp, [bl], [dec])
                for j in range(2):
                    stt(kd[j][:], ksb[:], ind[:, j:j + 1], e2[:], ALU.mult, ALU.mult, [ksb, ind, e2], [kd[j]])
                for j in range(2):
                    bs = [bring.next() for _ in range(2)]
                    for h in range(8):
                        mm(bs[h // 4][:, (h % 4) * 128:(h % 4 + 1) * 128], kd[j][:, h * 128:(h + 1) * 128],
                           vsb[:, h * 128:(h + 1) * 128], True, True, [kd[j], vsb], [bs[h // 4]])
                    for h in range(8):
                        stt(S[:, h, :], S[:, h, :], dec[:, h, j:j + 1], bs[h // 4][:, (h % 4) * 128:(h % 4 + 1) * 128],
                            ALU.mult, ALU.add, [S, dec, bs[h // 4]], [S])
        tiles = list(range(NT)) if not sa_flags.startswith("x") else [0, NT - 1]
        gens = [tile_body(t) for t in tiles]
        next(gens[0])
        for i_, gen in enumerate(gens):
            if i_ + 1 < len(gens):
                next(gens[i_ + 1])
            for _ in gen:
                pass
        tap("S", S[:].rearrange("p h d -> p (h d)"), S)
        tap("kmax2", kmax2[:], kmax2)
        P.barrier()


def phase1b(g):
    P, sb, mm, tr, act, ts, tt, stt, cp, dma, tap = g.P, g.sb, g.mm, g.tr, g.act, g.ts, g.tt, g.stt, g.cp, g.dma, g.tap
    NT, NTP, bring, banks = g.NT, g.NTP, g.bring, g.banks
    identb, identf, tbl, lbrow, omlrow, ind, kval, causal = g.identb, g.identf, g.tbl, g.lbrow, g.omlrow, g.ind, g.kval, g.causal
    Utri, M1, M2, tri = g.Utri, g.M1, g.M2, g.tri
    w_in, S, B = g.w_in, g.S, g.B
    gmix = g.gt["g_mix"]
    oaT, obT = g.oaT, g.obT

    with ExitStack() as sa:
        hTown = sb("hTown", [128, 16, 1024], BF16, sa)
        ss = sb("ss1", [128, 4], F32, sa)
        tmp = sb("tmp1", [128, 4], F32, sa)
        rs = sb("rs1", [128, 4], F32, sa)
        with ExitStack() as sl:
            xring = Ring([sb(f"xo{i}", [128, 2048], F32, sl) for i in range(2)])
            xnring = Ring([sb(f"xno{i}", [128, 2048], BF16, sl) for i in range(2)])
            for j in range(8):
                xt = xring.next()
                dma("sp", xt[:], g.xown[j * 128:(j + 1) * 128, :], [], [xt])
                xn = xnring.next()
                act(xn[:], xt[:], AF.Square, [xt], [xn, ss], accum=ss[:, 0:1])
                g.rstd_from_ss(T(ss.t[:, 0:1], ss.b), D, T(tmp.t[:, 0:1], tmp.b), T(rs.t[:, 0:1], rs.b), None)
                act(xn[:], xt[:], AF.Copy, [xt, rs], [xn], scale=rs[:, 0:1])
                for half in range(2):
                    bk = bring.next()
                    bv = bk.t[:].bitcast(BF16)
                    for kk in range(8):
                        k = half * 8 + kk
                        tr(bv[:, kk * 128:(kk + 1) * 128], xn[:, k * 128:(k + 1) * 128], identb[:], [xn, identb], [bk])
                    cp("dve" if half == 0 else "act", hTown[:, half * 8:(half + 1) * 8, j * 128:(j + 1) * 128],
                       bv.rearrange("p (k t) -> p k t", k=8), [bk], [hTown])
            P.barrier()

        with ExitStack() as sh:
            whr = Ring([sb(f"wh{i}", [128, 16, 512], BF16, sh) for i in range(4)])
            stg = Ring([sb(f"stgh{i}", [128, 4, 128], F32, sh) for i in range(3)])
            ND = 4

            def RN(name, shape, dt):
                return Ring([sb(f"{name}{i}", shape, dt, sh) for i in range(ND)])
            f_r, k_r, q_r = RN("f_", [128, 128], F32), RN("k_", [128, 128], F32), RN("q_", [128, 128], F32)
            v_r, gate_r = RN("v_", [128, 128], BF16), RN("gate", [128, 128], F32)
            e1_r, en1_r, eb_r, e2_r = RN("e1", [128, 128], F32), RN("en1", [128, 128], F32), RN("eb", [128, 128], F32), RN("e2b", [128, 128], F32)
            dec_r = RN("decb", [128, 2], F32)
            qin_r, kin_r, qbp_r = RN("qin", [128, 128], BF16), RN("kin", [128, 128], BF16), RN("qbp", [128, 192], BF16)
            kd_r = [RN(f"kdb{i}", [128, 128], BF16) for i in range(2)]
            atm_r, sb0_r, sb1_r = RN("atm", [128, 128], BF16), RN("sb0", [128, 128], BF16), RN("sb1", [128, 128], BF16)
            on_r, onb_r = RN("on", [128, 128], F32), RN("onb", [128, 128], BF16)
            ss_r, tmp_r, rs_r = RN("ssh", [128, 1], F32), RN("tmph", [128, 1], F32), RN("rsh", [128, 1], F32)
            for qb_ in qbp_r.tiles:
                P.op("dve", lambda e, qb_=qb_: e.memset(qb_[:], 0.0), [], B([qb_]))
            Sh = [T(S.t, P.buf(f"S_h{h}")) for h in range(8)]
            w4 = w_in[:, 0:4096].rearrange("p (s c) -> p s c", s=4)

            def load_head(h):
                wh = whr.next()
                for k in range(16):
                    s = stg.next()
                    dma("sp", s[:], w4[k * 128:(k + 1) * 128, :, h * 128:(h + 1) * 128], [], [s])
                    g.wcast(wh[:, k, :], s[:].rearrange("p s c -> p (s c)"), gmix[:, k:k + 1], [s, gmix], [wh],
                            engines=("act", "dve", "pool", "dve", "act"))
                return wh

            def body(h, j, wh):
                lb_h = lbrow[:, h * 128:(h + 1) * 128]
                oml_h = omlrow[:, h * 128:(h + 1) * 128]
                S_ = Sh[h]
                f_, k_, q_, v_, gate = f_r.next(), k_r.next(), q_r.next(), v_r.next(), gate_r.next()
                e1, en1, eb, e2, dec = e1_r.next(), en1_r.next(), eb_r.next(), e2_r.next(), dec_r.next()
                qin, kin, qbp, atm = qin_r.next(), kin_r.next(), qbp_r.next(), atm_r.next()
                kd = [kd_r[0].next(), kd_r[1].next()]
                sb0, sb1, on, onb = sb0_r.next(), sb1_r.next(), on_r.next(), onb_r.next()
                ss, tmp, rs = ss_r.next(), tmp_r.next(), rs_r.next()
                bp = bring.next()
                for k in range(16):
                    mm(bp[:], hTown[:, k, j * 128:(j + 1) * 128], wh[:, k, :], k == 0, k == 15, [hTown, wh], [bp])
                yield
                act(f_[:], bp[:, 128:256], AF.Sigmoid, [bp], [f_])
                act(gate[:], bp[:, 384:512], AF.Silu, [bp], [gate])
                cp("act", v_[:], bp[:, 256:384], [bp], [v_])
                cp("act", q_[:], bp[:, 0:128], [bp], [q_])
                tt("dve", f_[:], f_[:], oml_h, ALU.mult, [f_, omlrow], [f_])
                tt("dve", f_[:], f_[:], lb_h, ALU.add, [f_, lbrow], [f_])
                ts("dve", k_[:], f_[:], -1.0, 1.0, ALU.mult, ALU.add, [f_], [k_])
                act(f_[:], f_[:], AF.Ln, [f_], [f_])
                yield
                bt = bring.next()
                P.op("pe", lambda e: e.transpose(out=bt[:, 0:128], in_=q_[:], identity=identf[:]), B([q_, identf]), B([bt]))
                P.op("pe", lambda e: e.transpose(out=bt[:, 128:256], in_=k_[:], identity=identf[:]), B([k_, identf]), B([bt]))
                bc = bring.next()
                mm(bc[:, 0:128], f_[:], M1, True, True, [f_, tri], [bc])
                mm(bc[:, 128:256], f_[:], Utri, True, True, [f_, tri], [bc])
                mm(bc[:, 256:384], M2, f_[:], True, True, [f_, tri], [bc])
                mm(bc[:, 384:386], f_[:], ind[:], True, True, [f_, ind], [bc])
                yield
                act(e1[:], bc[:, 0:128], AF.Exp, [bc], [e1])
                act(en1[:], bc[:, 0:128], AF.Exp, [bc], [en1], scale=-1.0)
                act(eb[:], bc[:, 128:256], AF.Exp, [bc], [eb])
                act(e2[:], bc[:, 256:384], AF.Exp, [bc], [e2])
                act(dec[:], bc[:, 384:386], AF.Exp, [bc], [dec])
                tt("dve", qin[:], bt[:, 0:128], e1[:], ALU.mult, [bt, e1], [qin])
                tt("dve", kin[:], bt[:, 128:256], en1[:], ALU.mult, [bt, en1], [kin])
                tt("dve", qbp[:, 0:64], bt[:, 0:64], eb[:, 0:64], ALU.mult, [bt, eb], [qbp])
                tt("dve", qbp[:, 128:192], bt[:, 64:128], eb[:, 64:128], ALU.mult, [bt, eb], [qbp])
                for jj in range(2):
                    stt(kd[jj][:], k_[:], ind[:, jj:jj + 1], e2[:], ALU.mult, ALU.mult, [k_, ind, e2], [kd[jj]])
                yield
                ba = bring.next()
                mm(ba[:, 0:128], kin[:], qin[:], True, True, [kin, qin], [ba])
                bs = bring.next()
                cp("act", sb0[:], S[:, h, :], [S_], [sb0])
                mm(bs[:, 0:128], kd[0][:], v_[:], True, True, [kd[0], v_], [bs])
                mm(bs[:, 128:256], kd[1][:], v_[:], True, True, [kd[1], v_], [bs])
                yield
                tt("dve", atm[:], ba[:, 0:128], Utri, ALU.mult, [ba, tri], [atm])
                stt(S[:, h, :], S[:, h, :], dec[:, 0:1], bs[:, 0:128], ALU.mult, ALU.add, [S_, dec, bs], [S_])
                cp("act", sb1[:], S[:, h, :], [S_], [sb1])
                stt(S[:, h, :], S[:, h, :], dec[:, 1:2], bs[:, 128:256], ALU.mult, ALU.add, [S_, dec, bs], [S_])
                yield
                bo = bring.next()
                mm(bo[:, 0:128], atm[:], v_[:], True, False, [atm, v_], [bo])
                mm(bo[:, 0:128], qbp[:, 0:128], sb0[:], False, False, [qbp, sb0], [bo])
                mm(bo[:, 0:128], qbp[:, 64:192], sb1[:], False, True, [qbp, sb1], [bo])
                yield
                act(on[:], bo[:, 0:128], AF.Square, [bo], [on, ss], accum=ss[:, 0:1])
                g.rstd_from_ss(ss, 128, tmp, rs, None)
                act(on[:], bo[:, 0:128], AF.Copy, [bo, rs], [on], scale=rs[:, 0:1])
                tt("dve", onb[:], on[:], gate[:], ALU.mult, [on, gate], [onb])
                yield
                bz = bring.next()
                bzv = bz.t[:].bitcast(BF16)
                tr(bzv[:, 0:128], onb[:], identb[:], [onb, identb], [bz])
                ts("dve", oaT[:, h, j * 128:(j + 1) * 128], bzv[:, 0:128], g.ghg[:, h:h + 1], None, ALU.mult, None,
                   [bz, g.ghg], [oaT])

            for hp in range(2):
                hs = tuple(range(4 * hp, 4 * hp + 4))
                whs = [load_head(h) for h in hs]
                for j in range(8):
                    alive = [body(h, j, wh) for h, wh in zip(hs, whs)]
                    while alive:
                        for gen in list(alive):
                            try:
                                next(gen)
                            except StopIteration:
                                alive.remove(gen)
            P.barrier()
        tap("oaT", oaT[:].rearrange("p h t -> p (h t)"), oaT)


def phase1c(g):
    P, sb, mm, tr, act, ts, tt, stt, cp, dma, tap = g.P, g.sb, g.mm, g.tr, g.act, g.ts, g.tt, g.stt, g.cp, g.dma, g.tap
    NT, NTP, bring, banks = g.NT, g.NTP, g.bring, g.banks
    identb, identf, tbl, kval, causal = g.identb, g.identf, g.tbl, g.kval, g.causal
    w_in, B = g.w_in, g.B
    gmix = g.gt["g_mix"]
    obT = g.obT
    kts, krs, vs = g.kts, g.krs, g.vs
    SCALE = float(1.0 / np.sqrt(192.0))

    with ExitStack() as sa:
        qnT = sb("qnT", [128, 8, 1024], BF16, sa)
        qra = sb("qra", [65, 8, 1024], BF16, sa)
        kmb = sb("kmb", [128, 8], F32, sa)
        ss = sb("ss2", [128, 4], F32, sa)
        tmp = sb("tmp2", [128, 4], F32, sa)
        rs = sb("rs2", [128, 4], F32, sa)
        ones = sb("ones", [128, 128], BF16, sa)
        P.op("dve", lambda e: e.memset(ones[:], 1.0), [], B([ones]))
        with ExitStack() as sl:
            km = sb("km", [128, 8], F32, sl)
            kmT = sb("kmT", [8, 128], F32, sl)
            kmc = sb("kmc", [8, 1], F32, sl)
            dg = sb("dg", [8, 8], F32, sl)
            onesf = sb("onesf", [8, 128], F32, sl)
            ts("dve", km[:], g.kmax2[:], g.krmax2[:, 0:1], None, ALU.add, None, [g.kmax2, g.krmax2], [km])
            bk = bring.next()
            P.op("pe", lambda e: e.transpose(out=bk[0:8, 0:128], in_=km[:], identity=identf[:]), B([km, identf]), B([bk]))
            cp("dve", kmT[:], bk[0:8, 0:128], [bk], [kmT])
            P.op("dve", lambda e: e.tensor_reduce(out=kmc[:], in_=kmT[:], axis=AX.X, op=ALU.max), B([kmT]), B([kmc]))
            act(kmc[:], kmc[:], AF.Sqrt, [kmc], [kmc])
            ts("dve", dg[:], identf[0:8, 0:8], kmc[:, 0:1], None, ALU.mult, None, [identf, kmc], [dg])
            P.op("dve", lambda e: e.memset(onesf[:], 1.0), [], B([onesf]))
            bk2 = bring.next()
            mm(bk2[:, 0:8], onesf[:], dg[:], True, True, [onesf, dg], [bk2])
            cp("dve", kmb[:], bk2[:, 0:8], [bk2], [kmb])
            P.barrier()
        tap("kmb", kmb[:], kmb)

        with ExitStack() as sq_:
            hTown = sb("hTown2", [128, 16, 128], BF16, sq_)
            xt = sb("xq", [128, 2048], F32, sq_)
            xn = sb("xnq", [128, 2048], BF16, sq_)
            wq = sb("wq", [128, 16, 512], BF16, sq_)
            wuq = sb("wuq", [128, 4, 1536], BF16, sq_)
            stg = Ring([sb(f"stgq{i}", [128, 1536], F32, sq_) for i in range(2)])
            cqn = sb("cqn", [128, 512], BF16, sq_)
            cqT = sb("cqT", [128, 4, 128], BF16, sq_)
            sqT = sb("sqT", [128, 8, 128], BF16, sq_)
            junk = sb("junkq", [128, 512], BF16, sq_)
            qr2 = sb("qr2", [128, 8, 64], F32, sq_)
            ra = sb("raq", [128, 8, 64], F32, sq_)
            rb = sb("rbq", [128, 8, 64], F32, sq_)
            qrp = sb("qrp", [128, 8, 64], BF16, sq_)
            qn2 = sb("qn2", [128, 8], F32, sq_)
            qn2b = sb("qn2b", [128, 8], F32, sq_)
            shT = sb("shT", [8, 128], BF16, sq_)
            for k in range(16):
                s = stg.next()
                dma("sp", s[:, 0:512], w_in[k * 128:(k + 1) * 128, 4096:4608], [], [s])
                g.wcast(wq[:, k, :], s[:, 0:512], gmix[:, k:k + 1], [s, gmix], [wq])
            for c in range(4):
                s = stg.next()
                dma("sp", s[:, :], g.w_uq[c * 128:(c + 1) * 128, :], [], [s])
                g.wcast(wuq[:, c, :], s[:, :], g.gq[:, c:c + 1], [s, g.gq], [wuq])
            wuq3 = wuq.t[:].rearrange("p c (h e) -> p c h e", h=8)
            for j in range(8):
                t = NTP + j
                dma("sp", xt[:], g.xown[j * 128:(j + 1) * 128, :], [], [xt])
                act(xn[:], xt[:], AF.Square, [xt], [xn, ss], accum=ss[:, 0:1])
                g.rstd_from_ss(T(ss.t[:, 0:1], ss.b), D, T(tmp.t[:, 0:1], tmp.b), T(rs.t[:, 0:1], rs.b), None)
                act(xn[:], xt[:], AF.Copy, [xt, rs], [xn], scale=rs[:, 0:1])
                for half in range(2):
                    bk = bring.next()
                    bv = bk.t[:].bitcast(BF16)
                    for kk in range(8):
                        k = half * 8 + kk
                        tr(bv[:, kk * 128:(kk + 1) * 128], xn[:, k * 128:(k + 1) * 128], identb[:], [xn, identb], [bk])
                    cp("dve" if half == 0 else "act", hTown[:, half * 8:(half + 1) * 8, :],
                       bv.rearrange("p (k t) -> p k t", k=8), [bk], [hTown])
                bq = bring.next()
                for k in range(16):
                    mm(bq[:], hTown[:, k, :], wq[:, k, :], k == 0, k == 15, [hTown, wq], [bq])
                act(junk[:], bq[:], AF.Square, [bq], [junk, ss], accum=ss[:, 1:2])
                g.rstd_from_ss(T(ss.t[:, 1:2], ss.b), 512, T(tmp.t[:, 1:2], tmp.b), T(rs.t[:, 1:2], rs.b), None)
                act(cqn[:], bq[:], AF.Copy, [bq, rs], [cqn], scale=rs[:, 1:2])
                bk = bring.next()
                bv = bk.t[:].bitcast(BF16)
                for c in range(4):
                    tr(bv[:, c * 128:(c + 1) * 128], cqn[:, c * 128:(c + 1) * 128], identb[:], [cqn, identb], [bk])
                cp("dve", cqT[:], bv[:, 0:512].rearrange("p (c t) -> p c t", c=4), [bk], [cqT])
                bqn = [bring.next() for _ in range(2)]
                for h in range(8):
                    for c in range(4):
                        mm(bqn[h // 4][:, (h % 4) * 128:(h % 4 + 1) * 128], wuq3[:, c, h, 0:128], cqT[:, c, :], c == 0, c == 3,
                           [wuq, cqT], [bqn[h // 4]])
                for n in range(2):
                    cp("dve", qnT[:, n * 4:(n + 1) * 4, j * 128:(j + 1) * 128], bqn[n][:].rearrange("p (h t) -> p h t", h=4),
                       [bqn[n]], [qnT])
                    act(sqT[:, n * 4:(n + 1) * 4, :], bqn[n][:].rearrange("p (h t) -> p h t", h=4), AF.Square, [bqn[n]], [sqT])
                bqr = bring.next()
                for c in range(4):
                    mm(bqr[:].rearrange("p (h e) -> p h e", h=8), cqT[:, c, :], wuq3[:, c, :, 128:192], c == 0, c == 3,
                       [cqT, wuq], [bqr])
                bqr3 = bqr[:].rearrange("p (h e) -> p h e", h=8)
                bqr4 = bqr[:].rearrange("p (h two d) -> p h two d", h=8, two=2)
                bn = bring.next()
                for h in range(8):
                    mm(bn[:, h:h + 1], sqT[:, h, :], ones[:, 0:1], True, True, [sqT, ones], [bn])
                act(qr2[:], bqr3, AF.Square, [bqr], [qr2])
                P.op("dve", lambda e: e.tensor_reduce(out=qn2[:], in_=qr2[:], axis=AX.X, op=ALU.add), B([qr2]), B([qn2]))
                tt("dve", qn2[:], qn2[:], bn[:, 0:8], ALU.add, [qn2, bn], [qn2])
                act(qn2[:], qn2[:], AF.Sqrt, [qn2], [qn2])
                stt(qn2b[:], qn2[:], -1.0, kmb[:], ALU.mult, ALU.mult, [qn2, kmb], [qn2b])
                bsh = bring.next()
                P.op("pe", lambda e, bsh=bsh: e.transpose(out=bsh[0:8, 0:128], in_=qn2b[:], identity=identf[:]),
                     B([qn2b, identf]), B([bsh]))
                cp("dve", shT[:], bsh[0:8, 0:128], [bsh], [shT])
                dma("pool", qra[64:65, :, j * 128:(j + 1) * 128], shT[:], [shT], [], wacc=[qra])
                cs = tbl[:, t, 0:32]
                sn = tbl[:, t, 32:64]
                tt("dve", ra[:].rearrange("p h (two d) -> p h two d", two=2), bqr4,
                   cs.unsqueeze(1).unsqueeze(1).to_broadcast([128, 8, 2, 32]), ALU.mult, [bqr, tbl], [ra])
                snb = sn.unsqueeze(1).to_broadcast([128, 8, 32])
                tt("dve", rb[:, :, 0:32], bqr3[:, :, 32:64], snb, ALU.mult, [bqr, tbl], [rb])
                tt("dve", rb[:, :, 32:64], bqr3[:, :, 0:32], snb, ALU.mult, [bqr, tbl], [rb])
                tt("dve", qrp[:, :, 0:32], ra[:, :, 0:32], rb[:, :, 0:32], ALU.subtract, [ra, rb], [qrp])
                tt("dve", qrp[:, :, 32:64], ra[:, :, 32:64], rb[:, :, 32:64], ALU.add, [ra, rb], [qrp])
                bk = bring.next()
                bv = bk.t[:].bitcast(BF16)
                for h in range(8):
                    tr(bv[0:64, h * 128:(h + 1) * 128], qrp[:, h, :], identb[:], [qrp, identb], [bk])
                cp("act", qra[0:64, :, j * 128:(j + 1) * 128], bv[0:64, :].rearrange("p (h t) -> p h t", h=8), [bk], [qra])
            P.barrier()
        tap("qnT", qnT[:].rearrange("p h t -> p (h t)"), qnT)
        tap("qra", qra[:].rearrange("p h t -> p (h t)"), qra, lambda d: d[0:65, :])

        with ExitStack() as st_:
            KR = sb("KR", [65, NT * 128], BF16, st_)
            KTr = Ring([sb(f"KTh{i}", [128, NT, 128], BF16, st_) for i in range(2)])
            Vr = Ring([sb(f"Vh{i}", [128, NT, 129], BF16, st_) for i in range(2)])
            PTr = Ring([sb(f"PT{i}", [128, 512], BF16, st_) for i in range(3)])
            rz = sb("rz", [128, 1], F32, st_)
            ob = sb("ob", [128, 128], BF16, st_)
            dma("sp", KR[0:64, :], g.KR_s, [krs], [KR])
            P.op("dve", lambda e: e.memset(KR[64:65, :], 1.0), [], B([KR]))
            acc = banks[0:4]
            sring = Ring(banks[4:8])
            V4 = g.V_s.rearrange("t p h d -> p t h d")
            for h in range(8):
                KTh = KTr.next()
                Vh = Vr.next()
                dma("sp", KTh[:], g.KT_s[:, :, h, :], [kts], [KTh])
                dma("sp", Vh[:, :, 0:128], V4[:, :, h, :], [vs], [Vh])
                cp("dve", Vh[:, :, 128:129], kval[:].unsqueeze(2), [kval, Vh], [Vh])
                for qt in range(2):
                    nkb = NTP + 4 * qt + 4
                    def issueS(kb, qt=qt, h=h, KTh=KTh):
                        dg_i = kb - (NTP + 4 * qt)
                        q0 = max(0, dg_i) * 128
                        c0 = qt * 512 + q0
                        c1 = qt * 512 + 512
                        st = sring.next()
                        mm(st[:, q0:512], KTh[:, kb, :], qnT[:, h, c0:c1], True, False, [KTh, qnT], [st])
                        mm(st[:, q0:512], KR[0:65, kb * 128:(kb + 1) * 128], qra[0:65, h, c0:c1], False, True, [KR, qra], [st])
                        return st, q0, dg_i

                    pend = issueS(0)
                    for kb in range(nkb):
                        st, q0, dg_i = pend
                        if kb + 1 < nkb:
                            pend = issueS(kb + 1)
                        PT = PTr.next()
                        act(PT[:, q0:512], st[:, q0:512], AF.Exp, [st], [PT], scale=SCALE)
                        if dg_i >= 0:
                            tt("dve", PT[:, q0:q0 + 128], PT[:, q0:q0 + 128], causal[:], ALU.mult, [PT, causal], [PT])
                        for jq in range(max(0, dg_i), 4):
                            last = NTP + 4 * qt + jq
                            mm(acc[jq][:, 0:129], PT[:, jq * 128:(jq + 1) * 128], Vh[:, kb, :], kb == 0, kb == last,
                               [PT, Vh], [acc[jq]])
                    for jq in range(4):
                        P.op("dve", lambda e, jq=jq: e.reciprocal(out=rz[:], in_=acc[jq][:, 128:129]), B([acc[jq]]), B([rz]))
                        act(ob[:], acc[jq][:, 0:128], AF.Copy, [acc[jq], rz], [ob], scale=rz[:, 0:1])
                        bz = sring.next()
                        bzv = bz.t[:].bitcast(BF16)
                        tr(bzv[:, 0:128], ob[:], identb[:], [ob, identb], [bz])
                        col = qt * 512 + jq * 128
                        cp("dve", obT[:, h, col:col + 128], bzv[:, 0:128], [bz], [obT])
            P.barrier()
        tap("obT", obT[:].rearrange("p h t -> p (h t)"), obT)


def prep_inputs(inp, NPREV=7, ncores=NCORES, lite=False):
    f = lambda a: np.ascontiguousarray(np.asarray(a))
    X = f(inp["x"])[0]
    pos = f(inp["positions"])[0].astype(np.int32)
    NT = NPREV * 8 + 8
    shared = {
        "w_in": f(inp["w_in"])[0],
        "g_mix": f(f(inp["norm_mix"])[0].reshape(16, 128).T),
        "g_ffn": f(f(inp["norm_ffn"])[0].reshape(16, 128).T),
        "g_ple": f(f(inp["norm_ple"])[0].reshape(16, 128).T),
        "g_final": f(inp["norm_final"]).reshape(1, D),
        "lbT": f(f(inp["lb_logits"]).reshape(2, 8, 128).transpose(2, 0, 1).reshape(128, 16)),
        "lb_logits": f(inp["lb_logits"]),
        "g_hg": f(f(inp["hg_norm"])[0].reshape(8, 128).T),
        "g_q": f(f(inp["mla_q_norm"])[0].reshape(4, 128).T),
        "g_kv": f(f(inp["mla_kv_norm"])[0].reshape(4, 128).T),
        "w_uq": f(inp["w_uq"])[0], "w_ukv": f(inp["w_ukv"])[0],
        "w_a": f(inp["w_a"])[0], "w_b": f(inp["w_b"])[0], "w_o": f(inp["w_o"])[0],
        "peer_wq": f(inp["peer_wq"])[0],
        "k1T": f(f(inp["peer_k1"])[0].T), "k2T": f(f(inp["peer_k2"])[0].T),
        "peer_uT": f(f(inp["peer_u"])[0].T), "peer_v": f(inp["peer_v"])[0],
        "w_pg": f(inp["w_pg"])[0], "w_pe": f(inp["w_pe"])[0],
    }
    if lite:
        shared["peer_uT"] = f(shared["peer_uT"][:, :128])
        shared["peer_v"] = f(shared["peer_v"][:128])
    shared.update(host_consts())
    maps = []
    for c in range(ncores):
        xprev = np.zeros((max(NPREV, 1) * 1024, D), np.float32)
        pall = np.zeros((NT * 128,), np.int32)
        valid = np.zeros((NT * 128,), np.float32)
        for s_ in range(NPREV):
            blk = c - NPREV + s_
            if blk >= 0:
                xprev[s_ * 1024:(s_ + 1) * 1024] = X[blk * 1024:(blk + 1) * 1024]
                pall[s_ * 1024:(s_ + 1) * 1024] = pos[blk * 1024:(blk + 1) * 1024]
                valid[s_ * 1024:(s_ + 1) * 1024] = 1.0
        pall[NPREV * 1024:] = pos[c * 1024:(c + 1) * 1024]
        valid[NPREV * 1024:] = 1.0
        m = dict(shared)
        m["xprev"] = xprev
        m["xown"] = f(X[c * 1024:(c + 1) * 1024])
        m["posT"] = f(pall.reshape(NT, 128).T)
        m["kvalid"] = f(valid.reshape(NT, 128).T)
        m["p_own"] = f(f(inp["p"])[0, 0, c * 1024:(c + 1) * 1024, :])
        maps.append(m)
    return maps


def own_hT(g, dst, st, src_rows, gain=None, nt=8, tag="h"):
    P, sb, tr, act, ts, cp, dma, bring, B = g.P, g.sb, g.tr, g.act, g.ts, g.cp, g.dma, g.bring, g.B
    xring = Ring([sb(f"{tag}x{i}", [128, 2048], F32, st) for i in range(2)])
    xnring = Ring([sb(f"{tag}xn{i}", [128, 2048], BF16, st) for i in range(2)])
    ss = sb(f"{tag}ss", [128, 1], F32, st)
    tmp = sb(f"{tag}tmp", [128, 1], F32, st)
    rs = sb(f"{tag}rs", [128, 1], F32, st)
    for j in range(nt):
        xt = xring.next()
        dma("sp", xt[:], src_rows(j), g.xres_dep, [xt])
        xn = xnring.next()
        act(xn[:], xt[:], AF.Square, [xt], [xn, ss], accum=ss[:, 0:1])
        g.rstd_from_ss(ss, D, tmp, rs, None)
        act(xn[:], xt[:], AF.Copy, [xt, rs], [xn], scale=rs[:, 0:1])
        for half in range(2):
            bk = bring.next()
            bv = bk.t[:].bitcast(BF16)
            for kk in range(8):
                k = half * 8 + kk
                tr(bv[:, kk * 128:(kk + 1) * 128], xn[:, k * 128:(k + 1) * 128], g.identb[:], [xn, g.identb], [bk])
            if gain is None:
                cp("dve" if half == 0 else "act", dst[:, half * 8:(half + 1) * 8, j * 128:(j + 1) * 128],
                   bv.rearrange("p (k t) -> p k t", k=8), [bk], [dst])
            else:
                for kk in range(8):
                    k = half * 8 + kk
                    ts("dve", dst[:, k, j * 128:(j + 1) * 128], bv[:, kk * 128:(kk + 1) * 128], gain[:, k:k + 1], None,
                       ALU.mult, None, [bk, gain], [dst])


def load_w(g, dst, src_fn, nk, width, gain, stg):
    for k in range(nk):
        s = stg.next()
        g.dma("sp", s[:, 0:width], src_fn(k), [], [s])
        if gain is None:
            g.wcast(dst[:, k, 0:width], s[:, 0:width], None, [s], [dst])
        else:
            g.wcast(dst[:, k, 0:width], s[:, 0:width], gain[:, k:k + 1], [s, gain], [dst])


def phase2(g):
    P, sb, mm, tr, act, ts, tt, stt, cp, dma, tap = g.P, g.sb, g.mm, g.tr, g.act, g.ts, g.tt, g.stt, g.cp, g.dma, g.tap
    bring, B, w_in = g.bring, g.B, g.w_in
    gmix = g.gt["g_mix"]
    oaT, obT = g.oaT, g.obT
    with ExitStack() as sa:
        hT = sb("hTm", [128, 16, 1024], BF16, sa)
        ysb = sb("ysb", [128, 8, 2048], BF16, sa)
        with ExitStack() as sl:
            own_hT(g, hT, sl, lambda j: g.xown[j * 128:(j + 1) * 128, :], None, 8, "m")
            P.barrier()
        with ExitStack() as sw:
            CW = 256
            wga_r = Ring([sb(f"wga{i}", [128, 16, CW], BF16, sw) for i in range(2)])
            wgb_r = Ring([sb(f"wgb{i}", [128, 16, CW], BF16, sw) for i in range(2)])
            wa_r = Ring([sb(f"wa{i}", [128, 8, CW], BF16, sw) for i in range(2)])
            wb_r = Ring([sb(f"wb{i}", [128, 8, CW], BF16, sw) for i in range(2)])
            stg = Ring([sb(f"stgm{i}", [128, CW], F32, sw) for i in range(4)])
            sga_r = Ring([sb(f"sga{i}", [128, CW], F32, sw) for i in range(2)])
            sgb_r = Ring([sb(f"sgb{i}", [128, CW], F32, sw) for i in range(2)])
            y1_r = Ring([sb(f"y1{i}", [128, CW], F32, sw) for i in range(2)])
            for n in range(D // CW):
                c0, c1 = n * CW, (n + 1) * CW
                wga, wgb, wa, wb = wga_r.next(), wgb_r.next(), wa_r.next(), wb_r.next()
                load_w(g, wga, lambda k: w_in[k * 128:(k + 1) * 128, 5184 + c0:5184 + c1], 16, CW, gmix, stg)
                load_w(g, wgb, lambda k: w_in[k * 128:(k + 1) * 128, 7232 + c0:7232 + c1], 16, CW, gmix, stg)
                load_w(g, wa, lambda k: g.w_a[k * 128:(k + 1) * 128, c0:c1], 8, CW, None, stg)
                load_w(g, wb, lambda k: g.w_b[k * 128:(k + 1) * 128, c0:c1], 8, CW, None, stg)
                for j in range(8):
                    tok = slice(j * 128, (j + 1) * 128)
                    bga, bgb = bring.next(), bring.next()
                    ba, bb = bga, bgb
                    for k in range(16):
                        mm(bga[:, 0:CW], hT[:, k, tok], wga[:, k, :], k == 0, k == 15, [hT, wga], [bga])
                    for h in range(8):
                        mm(ba[:, CW:2 * CW], oaT[:, h, tok], wa[:, h, :], h == 0, h == 7, [oaT, wa], [ba])
                    for k in range(16):
                        mm(bgb[:, 0:CW], hT[:, k, tok], wgb[:, k, :], k == 0, k == 15, [hT, wgb], [bgb])
                    for h in range(8):
                        mm(bb[:, CW:2 * CW], obT[:, h, tok], wb[:, h, :], h == 0, h == 7, [obT, wb], [bb])
                    sga, sgb, y1 = sga_r.next(), sgb_r.next(), y1_r.next()
                    act(sga[:], bga[:, 0:CW], AF.Sigmoid, [bga], [sga])
                    act(sgb[:], bgb[:, 0:CW], AF.Sigmoid, [bgb], [sgb])
                    tt("dve", y1[:], ba[:, CW:2 * CW], sga[:], ALU.mult, [ba, sga], [y1])
                    tt("dve", sgb[:], bb[:, CW:2 * CW], sgb[:], ALU.mult, [bb, sgb], [sgb])
                    tt("dve", ysb[:, j, c0:c1], y1[:], sgb[:], ALU.add, [y1, sgb], [ysb])
            P.barrier()
        tap("y", ysb[:].rearrange("p j d -> p (j d)"), ysb)
        for j in range(8):
            for half in range(2):
                bk = bring.next()
                bv = bk.t[:].bitcast(BF16)
                for kk in range(8):
                    k = half * 8 + kk
                    tr(bv[:, kk * 128:(kk + 1) * 128], ysb[:, j, k * 128:(k + 1) * 128], g.identb[:], [ysb, g.identb], [bk])
                cp("dve" if half == 0 else "act", hT[:, half * 8:(half + 1) * 8, j * 128:(j + 1) * 128],
                   bv.rearrange("p (k t) -> p k t", k=8), [bk], [hT])
        with ExitStack() as sw:
            wo_r = Ring([sb(f"wo{i}", [128, 16, 512], BF16, sw) for i in range(2)])
            stg = Ring([sb(f"stgo{i}", [128, 512], F32, sw) for i in range(4)])
            xr = Ring([sb(f"xr{i}", [128, 512], F32, sw) for i in range(3)])
            for n in range(4):
                wo = wo_r.next()
                load_w(g, wo, lambda k: g.w_o[k * 128:(k + 1) * 128, n * 512:(n + 1) * 512], 16, 512, None, stg)
                for j in range(8):
                    tok = slice(j * 128, (j + 1) * 128)
                    bo = bring.next()
                    for k in range(16):
                        mm(bo[:], hT[:, k, tok], wo[:, k, :], k == 0, k == 15, [hT, wo], [bo])
                    x = xr.next()
                    dma("sp", x[:], g.xown[tok, n * 512:(n + 1) * 512], [], [x])
                    tt("dve", x[:], bo[:], x[:], ALU.add, [bo, x], [x])
                    dma("pool", g.xres[tok, n * 512:(n + 1) * 512], x[:], [x], [], wacc=[g.xres_t])
            P.barrier()
    g.xres_dep = [g.xres_t]


def phase4(g):
    P, sb, mm, tr, act, ts, tt, stt, cp, dma, tap = g.P, g.sb, g.mm, g.tr, g.act, g.ts, g.tt, g.stt, g.cp, g.dma, g.tap
    bring, B = g.bring, g.B
    with ExitStack() as sa:
        hT = sb("hTp", [128, 16, 1024], BF16, sa)
        pT = sb("pT", [128, 2, 1024], BF16, sa)
        x3 = sb("x3", [128, 8, 2048], F32, sa)
        gfin = sb("gfin", [128, 2048], F32, sa)
        ssq = sb("ssq", [128, 8, 4], F32, sa)
        dma("sp", gfin[:], g.g_final[0:1, :].to_broadcast([128, 2048]), [], [gfin])
        with ExitStack() as sl:
            own_hT(g, hT, sl, lambda j: g.xres[j * 128:(j + 1) * 128, :], g.gt["g_ple"], 8, "p")
            pt_ = sb("pt_", [128, 256], F32, sl)
            ptb = sb("ptb", [128, 256], BF16, sl)
            for j in range(8):
                dma("sp", pt_[:], g.p_own[j * 128:(j + 1) * 128, :], [], [pt_])
                cp("dve", ptb[:], pt_[:], [pt_], [ptb])
                bk = bring.next()
                bv = bk.t[:].bitcast(BF16)
                for c in range(2):
                    tr(bv[:, c * 128:(c + 1) * 128], ptb[:, c * 128:(c + 1) * 128], g.identb[:], [ptb, g.identb], [bk])
                cp("act", pT[:, :, j * 128:(j + 1) * 128], bv[:, 0:256].rearrange("p (c t) -> p c t", c=2), [bk], [pT])
            P.barrier()
        with ExitStack() as sw:
            wpg_r = Ring([sb(f"wpg{i}", [128, 16, 512], BF16, sw) for i in range(2)])
            wpe_r = Ring([sb(f"wpe{i}", [128, 2, 512], BF16, sw) for i in range(2)])
            stg = Ring([sb(f"stgp{i}", [128, 512], F32, sw) for i in range(4)])
            sg = sb("sgp", [128, 512], F32, sw)
            junk = sb("junkp", [128, 512], F32, sw)
            xr = Ring([sb(f"xrp{i}", [128, 512], F32, sw) for i in range(3)])
            for n in range(4):
                cols = slice(n * 512, (n + 1) * 512)
                wpg, wpe = wpg_r.next(), wpe_r.next()
                load_w(g, wpg, lambda k: g.w_pg[k * 128:(k + 1) * 128, cols], 16, 512, None, stg)
                load_w(g, wpe, lambda k: g.w_pe[k * 128:(k + 1) * 128, cols], 2, 512, None, stg)
                for j in range(8):
                    tok = slice(j * 128, (j + 1) * 128)
                    bg, be = bring.next(), bring.next()
                    for k in range(16):
                        mm(bg[:], hT[:, k, tok], wpg[:, k, :], k == 0, k == 15, [hT, wpg], [bg])
                    for c in range(2):
                        mm(be[:], pT[:, c, tok], wpe[:, c, :], c == 0, c == 1, [pT, wpe], [be])
                    x = xr.next()
                    dma("sp", x[:], g.xres[tok, cols], g.xres_dep, [x])
                    act(sg[:], bg[:], AF.Sigmoid, [bg], [sg])
                    tt("dve", sg[:], be[:], sg[:], ALU.mult, [be, sg], [sg])
                    tt("dve", x3[:, j, cols], x[:], sg[:], ALU.add, [x, sg], [x3])
                    act(junk[:], x3[:, j, cols], AF.Square, [x3], [junk, ssq], accum=ssq[:, j, n:n + 1])
            P.barrier()
        tap("x3", x3[:].rearrange("p j d -> p (j d)"), x3)
        with ExitStack() as sf:
            tot = sb("tot", [128, 8], F32, sf)
            tmp = sb("tmpf", [128, 8], F32, sf)
            rs = sb("rsf", [128, 8], F32, sf)
            P.op("dve", lambda e: e.tensor_reduce(out=tot[:], in_=ssq[:], axis=AX.X, op=ALU.add), B([ssq]), B([tot]))
            g.rstd_from_ss(tot, D, tmp, rs, None)
            tap("rsf", rs[:], rs)
            tap("tot", tot[:], tot)
            tap("ssq", ssq[:].rearrange("p j n -> p (j n)"), ssq)
            tap("gfin", gfin[:], gfin)
            for j in range(8):
                stt(x3[:, j, :], x3[:, j, :], rs[:, j:j + 1], gfin[:], ALU.mult, ALU.mult, [x3, rs, gfin], [x3])
                dma("sp", g.out[j * 128:(j + 1) * 128, :], x3[:, j, :], [x3], [])
            P.barrier()


STOP_AFTER = None


def kernel(**inputs):
    maps = prep_inputs(inputs, NPREV=7, ncores=NCORES)
    nc = build(NPREV=7, stop_after=STOP_AFTER)
    res = run_bass_kernel_spmd(nc, maps, core_ids=list(range(NCORES)))
    return np.concatenate([r["out"] for r in res.results], axis=0).reshape(1, NCORES * 1024, D).astype(np.float32)


def phase3(g):
    P, sb, mm, tr, act, ts, tt, stt, cp, dma, tap = g.P, g.sb, g.mm, g.tr, g.act, g.ts, g.tt, g.stt, g.cp, g.dma, g.tap
    bring, banks, B = g.bring, g.banks, g.B
    identf, identb = g.identf, g.identb
    TP, NI = 512, 64
    NEG = -1.0e30
    uT3 = g.peer_uT.rearrange("(k p) e -> p k e", p=128)
    v3 = g.peer_v.rearrange("(i p) d -> p i d", p=128)
    with ExitStack() as s3:
        k1b = sb("k1b", [128, 128], BF16, s3)
        k2b = sb("k2b", [128, 128], BF16, s3)
        with ExitStack() as sl:
            kf = sb("kf", [128, 256], F32, sl)
            dma("sp", kf[:, 0:128], g.k1T, [], [kf])
            dma("sp", kf[:, 128:256], g.k2T, [], [kf])
            cp("dve", k1b[:], kf[:, 0:128], [kf], [k1b])
            cp("dve", k2b[:], kf[:, 128:256], [kf], [k2b])
            P.barrier()
        for ps in range(1024 // TP):
            with ExitStack() as sp_:
                hnT = sb(f"hnT{ps}", [128, 16, TP], BF16, sp_)
                qpT = sb(f"qpT{ps}", [128, 16, TP], BF16, sp_)
                statT = sb(f"statT{ps}", [128, 4, TP], F32, sp_)
                xacc = sb(f"xacc{ps}", [128, 4, 2048], F32, sp_)
                for jl in range(4):
                    dma("sp", xacc[:, jl, :], g.xres[(ps * 4 + jl) * 128:(ps * 4 + jl + 1) * 128, :], g.xres_dep, [xacc])
                with ExitStack() as sl:
                    own_hT(g, hnT, sl, lambda j: g.xres[(ps * 4 + j) * 128:(ps * 4 + j + 1) * 128, :], g.gt["g_ffn"], 4, f"f{ps}")
                    P.barrier()
                with ExitStack() as sq_:
                    wqp_r = Ring([sb(f"wqp{ps}{i}", [128, 16, 512], BF16, sq_) for i in range(2)])
                    stg = Ring([sb(f"stg3{ps}{i}", [128, 512], F32, sq_) for i in range(4)])
                    for mg in range(4):
                        wqp = wqp_r.next()
                        load_w(g, wqp, lambda k: g.peer_wq[k * 128:(k + 1) * 128, mg * 512:(mg + 1) * 512], 16, 512, None, stg)
                        for m4 in range(4):
                            bq = bring.next()
                            for k in range(16):
                                mm(bq[:], wqp[:, k, m4 * 128:(m4 + 1) * 128], hnT[:, k, :], k == 0, k == 15, [wqp, hnT], [bq])
                            cp("act" if m4 % 2 == 0 else "dve", qpT[:, mg * 4 + m4, :], bq[:], [bq], [qpT])
                    P.barrier()
                with ExitStack() as sk_:
                    m8 = sb(f"m8{ps}", [128, 16, 16], F32, sk_)
                    wk = sb(f"wk{ps}", [128, 128], F32, sk_)
                    cand = sb(f"cand{ps}", [128, 8, 256], F32, sk_)
                    wk2 = sb(f"wk2{ps}", [128, 256], F32, sk_)
                    c8 = sb(f"c8{ps}", [128, 8, 16], F32, sk_)
                    ex = sb(f"ex{ps}", [128, 8, 16], F32, sk_)
                    zz = sb(f"zz{ps}", [128, 8], F32, sk_)
                    rz = sb(f"rz3{ps}", [128, 8], F32, sk_)
                    st4 = sb(f"st4{ps}", [128, 4, 8, 16], F32, sk_)
                    for jl in range(4):
                        tok = slice(jl * 128, (jl + 1) * 128)
                        bsc = [bring.next() for _ in range(4)]
                        for m in range(16):
                            mm(bsc[m // 4][:, (m % 4) * 128:(m % 4 + 1) * 128], qpT[:, m, tok], (k1b if m % 2 == 0 else k2b)[:],
                               True, True, [qpT, k1b, k2b], [bsc[m // 4]])
                        for m in range(16):
                            sc = bsc[m // 4][:, (m % 4) * 128:(m % 4 + 1) * 128]
                            bb = bsc[m // 4]
                            P.op("dve", lambda e, sc=sc, m=m: e.max(out=m8[:, m, 0:8], in_=sc), B([bb]), B([m8]))
                            P.op("dve", lambda e, sc=sc, m=m: e.match_replace(out=wk[:], in_to_replace=m8[:, m, 0:8], in_values=sc,
                                                                              imm_value=NEG), B([bb, m8]), B([wk]))
                            P.op("dve", lambda e, m=m: e.max(out=m8[:, m, 8:16], in_=wk[:]), B([wk]), B([m8]))
                        m84 = m8[:].rearrange("p (h two) a -> p h two a", two=2)
                        v1 = m84[:, :, 0, :]
                        v2 = m84[:, :, 1, :]
                        tt("dve", cand[:].rearrange("p h (a b) -> p h a b", a=16), v1.unsqueeze(3).to_broadcast([128, 8, 16, 16]),
                           v2.unsqueeze(2).to_broadcast([128, 8, 16, 16]), ALU.add, [m8], [cand])
                        for h in range(8):
                            P.op("dve", lambda e, h=h: e.max(out=c8[:, h, 0:8], in_=cand[:, h, :]), B([cand]), B([c8]))
                            P.op("dve", lambda e, h=h: e.match_replace(out=wk2[:], in_to_replace=c8[:, h, 0:8], in_values=cand[:, h, :],
                                                                       imm_value=NEG), B([cand, c8]), B([wk2]))
                            P.op("dve", lambda e, h=h: e.max(out=c8[:, h, 8:16], in_=wk2[:]), B([wk2]), B([c8]))
                        mx = c8[:, :, 0:1]
                        tau = c8[:, :, 15:16]
                        tt("dve", ex[:], c8[:], mx.to_broadcast([128, 8, 16]), ALU.subtract, [c8], [ex])
                        act(ex[:], ex[:], AF.Exp, [ex], [ex])
                        P.op("dve", lambda e: e.tensor_reduce(out=zz[:], in_=ex[:], axis=AX.X, op=ALU.add), B([ex]), B([zz]))
                        P.op("dve", lambda e: e.reciprocal(out=rz[:], in_=zz[:]), B([zz]), B([rz]))
                        cp("dve", st4[:, 0, :, :], v1, [m8], [st4])
                        tt("dve", st4[:, 1, :, :], v1, mx.to_broadcast([128, 8, 16]), ALU.subtract, [m8, c8], [st4])
                        tt("dve", st4[:, 2, :, :], tau.to_broadcast([128, 8, 16]), v1, ALU.subtract, [m8, c8], [st4])
                        ts("dve", st4[:, 2, :, :], st4[:, 2, :, :], -2.0e-5, None, ALU.add, None, [st4], [st4])
                        cp("dve", st4[:, 3, :, :], rz[:].unsqueeze(2).to_broadcast([128, 8, 16]), [rz], [st4])
                        bt = bring.next()
                        for q in range(4):
                            P.op("pe", lambda e, q=q, bt=bt: e.transpose(out=bt[:, q * 128:(q + 1) * 128],
                                                                         in_=st4[:, q, :, :].rearrange("p h a -> p (h a)"),
                                                                         identity=identf[:]), B([st4, identf]), B([bt]))
                        cp("act", statT[:, :, tok], bt[:].rearrange("p (q t) -> p q t", q=4), [bt], [statT])
                    P.barrier()
                qp4 = qpT.t[:].rearrange("p (h two) t -> p h two t", two=2)
                for sub in range(128 // NI):
                    i0 = sub * NI
                    with ExitStack() as sg_:
                        GT = sb(f"GT{ps}{sub}", [128, NI, TP], BF16, sg_)
                        Ptr = Ring([sb(f"Pt{ps}{sub}{i}", [128, NI], BF16, sg_) for i in range(6)])
                        Er = Ring([sb(f"E{ps}{sub}{i}", [128, 128], F32, sg_) for i in range(6)])
                        Qr = Ring([sb(f"Qt{ps}{sub}{i}", [128, 128], BF16, sg_) for i in range(6)])
                        rS = Ring(banks[0:4])
                        rg = Ring(banks[4:6])
                        q1r = Ring([sb(f"q1r{ps}{sub}{i}", [128, 8, 128], BF16, sg_) for i in range(3)])
                        q2r = Ring([sb(f"q2r{ps}{sub}{i}", [128, 8, 128], BF16, sg_) for i in range(3)])
                        DEPTH = 3
                        Sof = {}
                        qrep = {}

                        def issueS(t):
                            if t % 8 == 0:
                                q1t = q1r.next()
                                q2t = q2r.next()
                                for hf_, qt_ in ((0, q1t), (1, q2t)):
                                    src = qp4[:, :, hf_, t:t + 8].rearrange("p h t -> p t h").unsqueeze(3).to_broadcast([128, 8, 8, 16])
                                    eng_ = ("pool", "dve", "pool", "act")[(t // 8 * 2 + hf_) % 4]
                                    cp(eng_, qt_[:].rearrange("p t (h a) -> p t h a", a=16), src, [qpT], [qt_])
                                qrep[t // 8] = (q1t, q2t)
                            q1t, q2t = qrep[t // 8]
                            bS = rS.next()
                            mm(bS[:, 0:NI], q1t[:, t % 8, :], k1b[:, i0:i0 + NI], True, True, [q1t, k1b], [bS])
                            mm(bS[:, 128:256], q2t[:, t % 8, :], k2b[:], True, True, [q2t, k2b], [bS])
                            Sof[t] = bS

                        for t in range(DEPTH):
                            issueS(t)
                        for t in range(TP):
                            if t + DEPTH < TP:
                                issueS(t + DEPTH)
                            bS = Sof.pop(t)
                            Pt = Ptr.next()
                            E = Er.next()
                            Qt = Qr.next()
                            act(E[:], bS[:, 128:256], AF.Exp, [bS, statT], [E], bias=statT[:, 1, t:t + 1])
                            ts("dve", Pt[:], bS[:, 0:NI], statT[:, 0, t:t + 1], statT[:, 3, t:t + 1], ALU.is_equal, ALU.mult,
                               [bS, statT], [Pt])
                            stt(Qt[:], bS[:, 128:256], statT[:, 2, t:t + 1], E[:], ALU.is_ge, ALU.mult, [bS, statT, E], [Qt])
                            if t % 4 == 0:
                                bg = rg.next()
                            mm(bg[:, (t % 4) * NI:(t % 4 + 1) * NI], Qt[:], Pt[:], True, True, [Qt, Pt], [bg])
                            if t % 4 == 3:
                                cp("act", GT[:, :, t - 3:t + 1].rearrange("p i t -> p t i"),
                                   bg[:, 0:4 * NI].rearrange("p (t i) -> p t i", t=4), [bg], [GT])
                        UTr = Ring([sb(f"UT{ps}{sub}{i}", [128, 16, 256], BF16, sg_) for i in range(4)])
                        glr = Ring([sb(f"gl{ps}{sub}{i}", [128, TP], BF16, sg_) for i in range(2)])
                        zr = Ring(banks[6:8])
                        for ig in range(NI // 2):
                            UT = UTr.next()
                            dma("pool", UT[:], uT3[:, :, (i0 + ig * 2) * 128:(i0 + ig * 2 + 2) * 128], [], [UT])
                            for il in range(2):
                                i = ig * 2 + il
                                bz = zr.next()
                                for k in range(16):
                                    mm(bz[:], UT[:, k, il * 128:(il + 1) * 128], hnT[:, k, :], k == 0, k == 15, [UT, hnT], [bz])
                                gl = glr.next()
                                act(gl[:], bz[:], AF.Gelu, [bz], [gl])
                                tt("dve", GT[:, i, :], GT[:, i, :], gl[:], ALU.mult, [GT, gl], [GT])
                        Vr = Ring([sb(f"Vg{ps}{sub}{i}", [128, 4, 512], BF16, sg_) for i in range(4)])
                        acc = banks[0:4]
                        for dc in range(4):
                            for ig in range(NI // 4):
                                Vg = Vr.next()
                                dma("pool", Vg[:], v3[:, i0 + ig * 4:i0 + ig * 4 + 4, dc * 512:(dc + 1) * 512], [], [Vg])
                                for il in range(4):
                                    i = ig * 4 + il
                                    for tq in range(4):
                                        mm(acc[tq][:], GT[:, i, tq * 128:(tq + 1) * 128], Vg[:, il, :], i == 0, i == NI - 1,
                                           [GT, Vg], [acc[tq]])
                            for tq in range(4):
                                tt("dve", xacc[:, tq, dc * 512:(dc + 1) * 512], acc[tq][:], xacc[:, tq, dc * 512:(dc + 1) * 512],
                                   ALU.add, [acc[tq], xacc], [xacc])
                        P.barrier()
                for jl in range(4):
                    dma("sp", g.xres[(ps * 4 + jl) * 128:(ps * 4 + jl + 1) * 128, :], xacc[:, jl, :], [xacc], [], wacc=[g.xres_t])
                P.barrier()
    g.xres_dep = [g.xres_t]
```

```python
import numpy as np
from contextlib import ExitStack
import concourse.bass as bass
import concourse.mybir as mybir
from concourse.bass_utils import run_bass_kernel_spmd

F32 = mybir.dt.float32
BF16 = mybir.dt.bfloat16
I32 = mybir.dt.int32
AF = mybir.ActivationFunctionType
ALU = mybir.AluOpType
AX = mybir.AxisListType

ENGS = ("pe", "act", "dve", "pool", "sp")
NCORES = 8
D = 2048
KD = 16
TOWN = 1024
EPS = 1e-6
PI = float(np.pi)


class Buf:
    __slots__ = ("name", "w", "r", "excl")

    def __init__(self, name, excl=False):
        self.name = name
        self.w = {}
        self.r = {}
        self.excl = excl


class Prog:
    SAME_ENG_SYNC = True

    def __init__(self, nc, es, dma_slots=8):
        self.nc = nc
        self.es = es
        self.q = {e: [] for e in ENGS}
        self.cnt = {e: 0 for e in ENGS}
        self.sem = {e: es.enter_context(nc.semaphore("s_" + e)) for e in ENGS}
        self.K = dma_slots
        self.dsem = {}
        self.dcnt = {}
        for e in ("sp", "pool", "act"):
            self.dsem[e] = [es.enter_context(nc.semaphore(f"d_{e}{k}")) for k in range(dma_slots)]
            self.dcnt[e] = 0
        self.seen = {e: {} for e in ENGS}
        self.nbuf = 0

    def buf(self, name=None, excl=False):
        self.nbuf += 1
        return Buf(name or f"b{self.nbuf}", excl)

    def _deps(self, eng, r, w, wacc=()):
        deps = {}

        def add(k, v):
            if deps.get(k, 0) < v:
                deps[k] = v

        for b in r:
            for k, v in b.w.items():
                add(k, v)
            if b.excl:
                for k, v in b.r.items():
                    if k != eng:
                        add(k, v)
        for b in w:
            for k, v in b.w.items():
                add(k, v)
            for k, v in b.r.items():
                add(k, v)
        for b in wacc:
            for k, v in b.r.items():
                add(k, v)
        waits = []
        seen = self.seen[eng]
        for k, v in deps.items():
            if k == eng and (eng == "pe" or not self.SAME_ENG_SYNC):
                continue
            if seen.get(k, 0) >= v:
                continue
            seen[k] = v
            waits.append((k, v))
        return waits

    def _mark(self, ev, r, w, wacc=()):
        k, v = ev
        for b in r:
            if b.r.get(k, 0) < v:
                b.r[k] = v
        for b in w:
            b.w = {k: v}
            b.r = {}
        for b in wacc:
            if b.w.get(k, 0) < v:
                b.w[k] = v

    def op(self, eng, fn, r=(), w=(), wacc=()):
        waits = self._deps(eng, r, w, wacc)
        self.cnt[eng] += 1
        ev = (eng, self.cnt[eng])
        self.q[eng].append((waits, fn, self.sem[eng], 1))
        self._mark(ev, r, w, wacc)
        return ev

    def dma(self, eng, out, in_, r=(), w=(), wacc=(), **kw):
        waits = self._deps(eng, r, w, wacc)
        j = self.dcnt[eng]
        self.dcnt[eng] = j + 1
        k = j % self.K
        val = 16 * (j // self.K + 1)
        key = ("d", eng, k)
        if j >= self.K:
            pv = val - 16
            if self.seen[eng].get(key, 0) < pv:
                self.seen[eng][key] = pv
                waits.append((key, pv))
        self.q[eng].append((waits, lambda e: e.dma_start(out=out, in_=in_, **kw), self.dsem[eng][k], 16))
        ev = (key, val)
        self._mark(ev, r, w, wacc)
        return ev

    def barrier(self):
        targets = [(e, self.cnt[e]) for e in ENGS if self.cnt[e] > 0]
        for eng in ("sp", "pool", "act"):
            j = self.dcnt[eng]
            for k in range(self.K):
                n = (j - 1 - k) // self.K + 1 if j > k else 0
                if n > 0:
                    targets.append((("d", eng, k), 16 * n))
        for e in ENGS:
            waits = []
            for k, v in targets:
                if k == e:
                    continue
                if self.seen[e].get(k, 0) >= v:
                    continue
                self.seen[e][k] = v
                waits.append((k, v))
            if waits:
                self.q[e].append((waits, None, None, 0))

    def _semh(self, k):
        if isinstance(k, tuple):
            return self.dsem[k[1]][k[2]]
        return self.sem[k]

    def emit(self):
        nc = self.nc
        with nc.Block() as block:
            def mk(ename):
                def body(e):
                    for waits, fn, sem, inc in self.q[ename]:
                        for k, v in waits:
                            e.wait_ge(self._semh(k), v)
                        if fn is not None:
                            ins = fn(e)
                            ins.then_inc(sem, inc)
                return body

            block.tensor(mk("pe"))
            block.scalar(mk("act"))
            block.vector(mk("dve"))
            block.gpsimd(mk("pool"))
            block.sync(mk("sp"))


class T:
    __slots__ = ("t", "b")

    def __init__(self, t, b):
        self.t = t
        self.b = b

    def __getitem__(self, idx):
        return self.t[idx]


class NS:
    pass


class Acc:
    __slots__ = ("t",)

    def __init__(self, t):
        self.t = t


class Ring:
    def __init__(self, tiles):
        self.tiles = tiles
        self.i = 0

    def next(self):
        t = self.tiles[self.i % len(self.tiles)]
        self.i += 1
        return t


def host_consts():
    c = {}
    c["c_ident"] = np.eye(128, dtype=np.float32)
    s = np.arange(128)[:, None]
    t = np.arange(128)[None, :]
    same = (s // 64) == (t // 64)
    U = (same & (s <= t)).astype(np.float32)
    R = (same & ((s % 64) <= 31)).astype(np.float32)
    L = same.astype(np.float32)
    c["c_tri"] = np.ascontiguousarray(np.stack([U, U - R, L - U], axis=1))
    c["c_causal"] = (s <= t).astype(np.float32)
    ind = np.zeros((128, 2), np.float32)
    ind[:64, 0] = 1.0
    ind[64:, 1] = 1.0
    c["c_ind"] = ind
    half = 32
    freqs = (10000.0 ** (-np.arange(half, dtype=np.float32) / half)).astype(np.float32)
    c["c_freq"] = np.ascontiguousarray(np.tile(freqs[None, :], (128, 2)))
    ph = np.concatenate([np.full(32, PI / 2), np.zeros(32)]).astype(np.float32)
    c["c_phase"] = np.ascontiguousarray(np.tile(ph[None, :], (128, 1)))
    return c


def build(NPREV=7, taps=(), stop_after=None, lite=False):
    NTP = NPREV * 8
    NT = NTP + 8
    nc = bass.Bass("TRN2", target_bir_lowering=False)

    def din(name, shape, dt=F32):
        return nc.dram_tensor(name, list(shape), dt, kind="ExternalInput").ap()

    xprev = din("xprev", [max(NPREV, 1) * 1024, D])
    xown = din("xown", [1024, D])
    posT = din("posT", [128, NT], I32)
    kvalid = din("kvalid", [128, NT])
    p_own = din("p_own", [1024, 256])
    w_in = din("w_in", [D, 9280])
    gT = {n: din(n, [128, 16]) for n in ("g_mix", "g_ffn", "g_ple")}
    g_final = din("g_final", [1, D])
    lbT = din("lbT", [128, 16])
    lb_logits = din("lb_logits", [2, 1024])
    g_hg = din("g_hg", [128, 8])
    g_q = din("g_q", [128, 4])
    g_kv = din("g_kv", [128, 4])
    w_uq = din("w_uq", [512, 1536])
    w_ukv = din("w_ukv", [512, 2048])
    w_a = din("w_a", [1024, D])
    w_b = din("w_b", [1024, D])
    w_o = din("w_o", [D, D])
    peer_wq = din("peer_wq", [D, D])
    k1T = din("k1T", [128, 128])
    k2T = din("k2T", [128, 128])
    peer_uT = din("peer_uT", [D, 128 if lite else 16384])
    peer_v = din("peer_v", [128 if lite else 16384, D])
    w_pg = din("w_pg", [D, D])
    w_pe = din("w_pe", [256, D])
    consts = {k: din(k, v.shape) for k, v in host_consts().items()}
    out = nc.dram_tensor("out", [1024, D], F32, kind="ExternalOutput").ap()
    tap_out = {}
    for name, shape in taps:
        tap_out[name] = nc.dram_tensor("tap_" + name, list(shape), F32, kind="ExternalOutput").ap()
    KT_s = nc.dram_tensor("KT_s", [128, NT, 8, 128], BF16).ap()
    KR_s = nc.dram_tensor("KR_s", [64, NT * 128], BF16).ap()
    V_s = nc.dram_tensor("V_s", [NT, 128, 8, 128], BF16).ap()
    xres = nc.dram_tensor("xres", [1024, D], F32).ap()

    with ExitStack() as es:
        P = Prog(nc, es)

        def sb(name, shape, dt, st=None):
            t = (st or es).enter_context(nc.sbuf_tensor(name, list(shape), dt))
            return T(t, P.buf(name))

        banks = [T(es.enter_context(nc.psum_tensor(f"bank{i}", [128, 512], F32)), P.buf(f"bank{i}", excl=True)) for i in range(8)]
        bring = Ring(banks)

        def B(l):
            return [x.b for x in l]

        def BW(l):
            return [x.b for x in l if not isinstance(x, Acc)], [x.t.b for x in l if isinstance(x, Acc)]

        def mm(o, lhsT, rhs, st, sp, r, w):
            P.op("pe", lambda e: e.matmul(o, lhsT=lhsT, rhs=rhs, start=st, stop=sp), B(r), *BW(w))

        def tr(o, i, ident, r, w):
            P.op("pe", lambda e: e.transpose(out=o, in_=i, identity=ident), B(r), *BW(w))

        def act(o, i, func, r, w, bias=None, scale=None, accum=None):
            kw = {}
            if bias is not None:
                kw["bias"] = bias
            if scale is not None:
                kw["scale"] = scale
            if accum is not None:
                kw["accum_out"] = accum
            P.op("act", lambda e: e.activation(out=o, in_=i, func=func, **kw), B(r), *BW(w))

        def ts(eng, o, i, s1, s2, op0, op1, r, w):
            if op1 is None:
                P.op(eng, lambda e: e.tensor_scalar(out=o, in0=i, scalar1=s1, scalar2=None, op0=op0), B(r), *BW(w))
            else:
                P.op(eng, lambda e: e.tensor_scalar(out=o, in0=i, scalar1=s1, scalar2=s2, op0=op0, op1=op1), B(r), *BW(w))

        def tt(eng, o, a, b, op, r, w):
            P.op(eng, lambda e: e.tensor_tensor(out=o, in0=a, in1=b, op=op), B(r), *BW(w))

        def stt(o, a, s, b, op0, op1, r, w):
            P.op("dve", lambda e: e.scalar_tensor_tensor(out=o, in0=a, scalar=s, in1=b, op0=op0, op1=op1), B(r), *BW(w))

        def cp(eng, o, i, r, w):
            if eng == "act":
                P.op("act", lambda e: e.copy(out=o, in_=i), B(r), *BW(w))
            else:
                P.op(eng, lambda e: e.tensor_copy(out=o, in_=i), B(r), *BW(w))

        wc_state = [0]

        def wcast(o, i, gain, r, w, engines=("act", "dve", "act", "dve", "pool")):
            eng = engines[wc_state[0] % len(engines)]
            wc_state[0] += 1
            if eng == "act":
                if gain is None:
                    cp("act", o, i, r, w)
                else:
                    act(o, i, AF.Copy, r, w, scale=gain)
            else:
                if gain is None:
                    cp(eng, o, i, r, w)
                else:
                    ts(eng, o, i, gain, None, ALU.mult, None, r, w)

        def dma(eng, o, i, r, w, wacc=(), **kw):
            P.dma(eng, o, i, B(r), B(w), B(wacc), **kw)

        def tap(name, src_ap, src_t, dst_slice=None):
            if name in tap_out:
                dst = tap_out[name] if dst_slice is None else dst_slice(tap_out[name])
                dma("pool", dst, src_ap, [src_t], [], max_dma_last_dim=2048)

        def rstd_from_ss(ss, n, tmp, rs, st_r):
            ts("dve", tmp[:], ss[:], 1.0 / n, EPS, ALU.mult, ALU.add, [ss], [tmp])
            act(tmp[:], tmp[:], AF.Sqrt, [tmp], [tmp])
            P.op("dve", lambda e: e.reciprocal(out=rs[:], in_=tmp[:]), B([tmp]), B([rs]))

        identf = sb("identf", [128, 128], F32)
        identb = sb("identb", [128, 128], BF16)
        tri = sb("tri", [128, 3, 128], F32)
        causal = sb("causal", [128, 128], F32)
        ind = sb("ind", [128, 2], F32)
        kval = sb("kval", [128, NT], F32)
        gt = {n: sb("sb_" + n, [128, 16], F32) for n in gT}
        lbc = sb("lbc", [128, 16], F32)
        ghg = sb("ghg", [128, 8], F32)
        gq = sb("gq", [128, 4], F32)
        gkv = sb("gkv", [128, 4], F32)
        dma("sp", identf[:], consts["c_ident"], [], [identf])
        dma("sp", tri[:], consts["c_tri"], [], [tri])
        dma("sp", causal[:], consts["c_causal"], [], [causal])
        dma("sp", ind[:], consts["c_ind"], [], [ind])
        dma("sp", kval[:], kvalid, [], [kval])
        for n in gT:
            dma("sp", gt[n][:], gT[n], [], [gt[n]])
        dma("sp", lbc[:], lbT, [], [lbc])
        dma("sp", ghg[:], g_hg, [], [ghg])
        dma("sp", gq[:], g_q, [], [gq])
        dma("sp", gkv[:], g_kv, [], [gkv])
        cp("dve", identb[:], identf[:], [identf], [identb])
        Utri = tri[:, 0, :]
        M1 = tri[:, 1, :]
        M2 = tri[:, 2, :]

        s1 = es.enter_context(ExitStack())
        tbl = sb("tbl", [128, NT, 64], F32, s1)
        with ExitStack() as s0:
            posi = sb("posi", [128, NT], I32, s0)
            posf = sb("posf", [128, NT], F32, s0)
            frq = sb("frq", [128, 64], F32, s0)
            phs = sb("phs", [128, 64], F32, s0)
            kk = sb("kk", [128, NT, 64], F32, s0)
            ki = sb("ki", [128, NT, 64], I32, s0)
            dma("sp", posi[:], posT, [], [posi])
            dma("sp", frq[:], consts["c_freq"], [], [frq])
            dma("sp", phs[:], consts["c_phase"], [], [phs])
            cp("dve", posf[:], posi[:], [posi], [posf])
            tt("dve", tbl[:], posf[:].unsqueeze(2).to_broadcast([128, NT, 64]),
               frq[:].unsqueeze(1).to_broadcast([128, NT, 64]), ALU.mult, [posf, frq], [tbl])
            tt("dve", tbl[:], tbl[:], phs[:].unsqueeze(1).to_broadcast([128, NT, 64]), ALU.add, [tbl, phs], [tbl])
            ts("dve", kk[:], tbl[:], 1.0 / (2 * PI), None, ALU.mult, None, [tbl], [kk])
            cp("dve", ki[:], kk[:], [kk], [ki])
            cp("dve", kk[:], ki[:], [ki], [kk])
            C1 = 6.28125
            C2 = float(2 * np.pi - 6.28125)
            stt(tbl[:], kk[:], -C1, tbl[:], ALU.mult, ALU.add, [kk, tbl], [tbl])
            stt(tbl[:], kk[:], -C2, tbl[:], ALU.mult, ALU.add, [kk, tbl], [tbl])
            ts("dve", kk[:], tbl[:], PI, -2 * PI, ALU.is_gt, ALU.mult, [tbl], [kk])
            tt("dve", tbl[:], tbl[:], kk[:], ALU.add, [tbl, kk], [tbl])
            ts("dve", kk[:], tbl[:], -PI, 2 * PI, ALU.is_lt, ALU.mult, [tbl], [kk])
            tt("dve", tbl[:], tbl[:], kk[:], ALU.add, [tbl, kk], [tbl])
            ts("dve", tbl[:], tbl[:], PI, -PI, ALU.min, ALU.max, [tbl], [tbl])
            act(tbl[:], tbl[:], AF.Sin, [tbl], [tbl])
            P.barrier()
        tap("tbl", tbl[:, NT - 1, :], tbl)

        lbrow = sb("lbrow", [128, 1024], F32, s1)
        omlrow = sb("omlrow", [128, 1024], F32, s1)
        with ExitStack() as s0:
            l1 = sb("l1", [128, 1024], F32, s0)
            dma("sp", lbrow[:], lb_logits[0:1, :].to_broadcast([128, 1024]), [], [lbrow])
            dma("sp", l1[:], lb_logits[1:2, :].to_broadcast([128, 1024]), [], [l1])
            tt("dve", lbrow[:], lbrow[:], l1[:], ALU.subtract, [lbrow, l1], [lbrow])
            act(lbrow[:], lbrow[:], AF.Sigmoid, [lbrow], [lbrow])
            ts("dve", omlrow[:], lbrow[:], -1.0, 1.0, ALU.mult, ALU.add, [lbrow], [omlrow])
            P.barrier()

        g = NS()
        g.__dict__.update(locals())
        if stop_after != "p0":
            phase1(g)
        sO = es.enter_context(ExitStack())
        g.oaT = oaT = sb("oaT", [128, 8, 1024], BF16, sO)
        g.obT = obT = sb("obT", [128, 8, 1024], BF16, sO)
        g.xres_t = T(None, P.buf("xres"))
        g.xres_dep = []
        early = stop_after is not None and (stop_after in ("p0", "p1a") or stop_after.startswith("x"))
        if not early:
            phase1b(g)
        if not early and stop_after != "p1b":
            phase1c(g)
        if not early and stop_after not in ("p1b", "p1c"):
            phase2(g)
        P.barrier()
        sO.close()
        s1.close()
        if not early and stop_after not in ("p1b", "p1c", "p2"):
            if stop_after != "nopeer":
                phase3(g)
            phase4(g)
        P.barrier()
        P.emit()
    return nc


def phase1(g):
    P, sb, mm, tr, act, ts, tt, stt, cp, dma, tap = g.P, g.sb, g.mm, g.tr, g.act, g.ts, g.tt, g.stt, g.cp, g.dma, g.tap
    NT, NTP, bring, s1 = g.NT, g.NTP, g.bring, g.s1
    identb, identf, tbl, lbrow, omlrow, ind, kval = g.identb, g.identf, g.tbl, g.lbrow, g.omlrow, g.ind, g.kval
    Utri, M1, M2, tri = g.Utri, g.M1, g.M2, g.tri
    w_in = g.w_in
    B = g.B

    es = g.es
    S = sb("S", [128, 8, 128], F32, s1)
    kmax2 = sb("kmax2", [128, 8], F32, s1)
    krmax2 = sb("krmax2", [128, 1], F32, s1)
    g.S, g.kmax2, g.krmax2 = S, kmax2, krmax2
    Sh = [T(S.t, P.buf(f"S1a_h{h}")) for h in range(8)]
    g.Sh1a = Sh
    P.op("dve", lambda e: e.memset(S[:], 0.0), [], B([S] + Sh))
    P.op("dve", lambda e: e.memset(kmax2[:], 0.0), [], B([kmax2]))
    P.op("dve", lambda e: e.memset(krmax2[:], 0.0), [], B([krmax2]))
    kts = T(None, P.buf("KT_s"))
    krs = T(None, P.buf("KR_s"))
    vs = T(None, P.buf("V_s"))
    g.kts, g.krs, g.vs = kts, krs, vs

    with ExitStack() as sa:
        wA = sb("wA", [128, 16, 2048], BF16, sa)
        wB = sb("wB", [128, 16, 576], BF16, sa)
        wkv = sb("wkv", [128, 4, 2048], BF16, sa)
        gmix = g.gt["g_mix"]
        with ExitStack() as sl:
            stg = Ring([sb(f"stg{i}", [128, 2048], F32, sl) for i in range(2)])
            for k in range(16):
                s = stg.next()
                dma("sp", s[:, :], w_in[k * 128:(k + 1) * 128, 1024:3072], [], [s])
                g.wcast(wA[:, k, :], s[:, :], gmix[:, k:k + 1], [s, gmix], [wA])
                s = stg.next()
                dma("sp", s[:, 0:576], w_in[k * 128:(k + 1) * 128, 4608:5184], [], [s])
                g.wcast(wB[:, k, :], s[:, 0:576], gmix[:, k:k + 1], [s, gmix], [wB])
            for c in range(4):
                s = stg.next()
                dma("sp", s[:, :], g.w_ukv[c * 128:(c + 1) * 128, :], [], [s])
                g.wcast(wkv[:, c, :], s[:, :], g.gkv[:, c:c + 1], [s, g.gkv], [wkv])
            P.barrier()

        xring = Ring([sb(f"xt{i}", [128, 2048], F32, sa) for i in range(2)])
        xnring = Ring([sb(f"xn{i}", [128, 2048], BF16, sa) for i in range(2)])
        hTring = Ring([sb(f"hT{i}", [128, 16, 128], BF16, sa) for i in range(2)])
        ss = sb("ss", [128, 4], F32, sa)
        tmp = sb("tmp", [128, 4], F32, sa)
        rs = sb("rs", [128, 4], F32, sa)
        fsb = sb("fsb", [128, 1024], F32, sa)
        ksb = sb("ksb", [128, 1024], F32, sa)
        vsb = sb("vsb", [128, 1024], BF16, sa)
        e2 = sb("e2", [128, 1024], F32, sa)
        kd = [sb(f"kd{j}", [128, 1024], BF16, sa) for j in range(2)]
        dec = sb("dec", [128, 8, 2], F32, sa)
        junk = sb("junk", [128, 512], BF16, sa)
        ckvn = sb("ckvn", [128, 512], BF16, sa)
        ckvT = sb("ckvT", [128, 4, 128], BF16, sa)
        knT = Ring([sb(f"knT{i}", [128, 8, 128], BF16, sa) for i in range(2)])
        Vt = Ring([sb(f"Vt{i}", [128, 8, 128], BF16, sa) for i in range(2)])
        sq = sb("sq", [128, 8, 128], F32, sa)
        knb = sb("knb", [128, 8, 128], BF16, sa)
        kn2 = sb("kn2", [128, 8], F32, sa)
        ra = sb("ra", [128, 64], F32, sa)
        rb = sb("rb", [128, 64], F32, sa)
        krp = sb("krp", [128, 64], BF16, sa)
        mkr = sb("mkr", [128, 64], F32, sa)
        krn = sb("krn", [128, 1], F32, sa)
        krT = Ring([sb(f"krT{i}", [64, 128], BF16, sa) for i in range(2)])

        sa_flags = g.stop_after or ""
        if sa_flags == "xw":
            return
        def tile_body(t):
            own = t >= NTP
            xt = xring.next()
            src = g.xown[(t - NTP) * 128:(t - NTP + 1) * 128, :] if own else g.xprev[t * 128:(t + 1) * 128, :]
            dma("sp", xt[:], src, [], [xt])
            xn = xnring.next()
            act(xn[:], xt[:], AF.Square, [xt], [xn, ss], accum=ss[:, 0:1])
            g.rstd_from_ss(T(ss.t[:, 0:1], ss.b), D, T(tmp.t[:, 0:1], tmp.b), T(rs.t[:, 0:1], rs.b), None)
            act(xn[:], xt[:], AF.Copy, [xt, rs], [xn], scale=rs[:, 0:1])
            hT = hTring.next()
            for half in range(2):
                bk = bring.next()
                bv = bk.t[:].bitcast(BF16)
                for kk in range(8):
                    k = half * 8 + kk
                    tr(bv[:, kk * 128:(kk + 1) * 128], xn[:, k * 128:(k + 1) * 128], identb[:], [xn, identb], [bk])
                cp("dve" if half == 0 else "act", hT[:, half * 8:(half + 1) * 8, :],
                   bv.rearrange("p (k t) -> p k t", k=8), [bk], [Acc(hT)])
            yield
            if not own:
                bA = [bring.next() for _ in range(4)]
                for n in range(4):
                    for k in range(16):
                        mm(bA[n][:], hT[:, k, :], wA[:, k, n * 512:(n + 1) * 512], k == 0, k == 15, [hT, wA], [bA[n]])
            bkv = bring.next()
            bkr = bring.next()
            for k in range(16):
                mm(bkv[:], hT[:, k, :], wB[:, k, 0:512], k == 0, k == 15, [hT, wB], [bkv])
            for k in range(16):
                mm(bkr[:, 0:64], hT[:, k, :], wB[:, k, 512:576], k == 0, k == 15, [hT, wB], [bkr])
            act(junk[:], bkv[:], AF.Square, [bkv], [junk, ss], accum=ss[:, 1:2])
            g.rstd_from_ss(T(ss.t[:, 1:2], ss.b), 512, T(tmp.t[:, 1:2], tmp.b), T(rs.t[:, 1:2], rs.b), None)
            act(ckvn[:], bkv[:], AF.Copy, [bkv, rs], [ckvn], scale=rs[:, 1:2])
            cp("dve", mkr[:], bkr[:, 0:64], [bkr], [mkr])
            if sa_flags == "xproj":
                return
            if not own:
                for n in range(2):
                    act(fsb[:, n * 512:(n + 1) * 512], bA[n][:], AF.Sigmoid, [bA[n]], [Acc(fsb)])
                for n in range(2):
                    cp("act", vsb[:, n * 512:(n + 1) * 512], bA[2 + n][:], [bA[2 + n]], [Acc(vsb)])
                tt("dve", fsb[:], fsb[:], omlrow[:], ALU.mult, [fsb, omlrow], [fsb])
                tt("dve", fsb[:], fsb[:], lbrow[:], ALU.add, [fsb, lbrow], [fsb])
                ts("dve", ksb[:], fsb[:], -1.0, 1.0, ALU.mult, ALU.add, [fsb], [ksb])
                act(fsb[:], fsb[:], AF.Ln, [fsb], [fsb])
            if sa_flags == "xhg":
                return
            bk = bring.next()
            bv = bk.t[:].bitcast(BF16)
            for c in range(4):
                tr(bv[:, c * 128:(c + 1) * 128], ckvn[:, c * 128:(c + 1) * 128], identb[:], [ckvn, identb], [bk])
            cp("dve", ckvT[:], bv[:, 0:512].rearrange("p (c t) -> p c t", c=4), [bk], [ckvT])
            if sa_flags == "xkv1":
                return
            wkv4 = wkv.t[:].rearrange("p c (h two d) -> p c h two d", h=8, two=2)
            btk = [bring.next() for _ in range(4)]
            for n in range(4):
                for c in range(4):
                    mm(btk[n][:], ckvT[:, c, :], wkv[:, c, n * 512:(n + 1) * 512], c == 0, c == 3, [ckvT, wkv], [btk[n]])
            if sa_flags == "xkv3a":
                return
            vt = Vt.next()
            for n in range(4):
                bview = btk[n][:].rearrange("p (h two d) -> p h two d", h=2, two=2)
                cp("dve" if n % 2 == 0 else "act", vt[:, 2 * n:2 * n + 2, :], bview[:, :, 1, :], [btk[n]], [Acc(vt)])
                cp("act" if n % 2 == 0 else "dve", knb[:, 2 * n:2 * n + 2, :], bview[:, :, 0, :], [btk[n]], [Acc(knb)])
            bkn = bring.next()
            bknv = bkn.t[:].bitcast(BF16)
            for h in range(8):
                tr(bknv[:, h * 128:(h + 1) * 128], knb[:, h, :], identb[:], [knb, identb], [bkn])
            kn = knT.next()
            cp("act", kn[:], bknv.rearrange("p (h t) -> p h t", h=8), [bkn], [kn])
            dma("pool", g.KT_s[:, t, :, :], kn[:], [kn], [], wacc=[kts])
            if sa_flags in ("xkv3b", "xkv3c"):
                return
            dma("pool", g.V_s[t, :, :, :], vt[:], [vt], [], wacc=[vs])
            if sa_flags == "xkv3":
                return
            tt("dve", sq[:], knb[:], knb[:], ALU.mult, [knb], [sq])
            if sa_flags == "xkv3d":
                return
            P.op("dve", lambda e: e.tensor_reduce(out=kn2[:], in_=sq[:], axis=AX.X, op=ALU.add), B([sq]), B([kn2]))
            tt("dve", kmax2[:], kmax2[:], kn2[:], ALU.max, [kmax2, kn2], [kmax2])
            cs = tbl[:, t, 0:32]
            sn = tbl[:, t, 32:64]
            tt("dve", ra[:].rearrange("p (two d) -> p two d", two=2), mkr[:].rearrange("p (two d) -> p two d", two=2),
               cs.unsqueeze(1).to_broadcast([128, 2, 32]), ALU.mult, [mkr, tbl], [ra])
            tt("dve", rb[:, 0:32], mkr[:, 32:64], sn, ALU.mult, [mkr, tbl], [Acc(rb)])
            tt("dve", rb[:, 32:64], mkr[:, 0:32], sn, ALU.mult, [mkr, tbl], [Acc(rb)])
            tt("dve", krp[:, 0:32], ra[:, 0:32], rb[:, 0:32], ALU.subtract, [ra, rb], [Acc(krp)])
            tt("dve", krp[:, 32:64], ra[:, 32:64], rb[:, 32:64], ALU.add, [ra, rb], [Acc(krp)])
            act(ra[:], krp[:], AF.Square, [krp], [ra, krn], accum=krn[:])
            tt("dve", krmax2[:], krmax2[:], krn[:], ALU.max, [krmax2, krn], [krmax2])
            if sa_flags == "xkv4":
                return
            bk = bring.next()
            bv = bk.t[:].bitcast(BF16)
            tr(bv[0:64, 0:128], krp[:, :], identb[:], [krp, identb], [bk])
            kr = krT.next()
            cp("act", kr[:], bv[0:64, 0:128], [bk], [kr])
            dma("pool", g.KR_s[:, t * 128:(t + 1) * 128], kr[:], [kr], [], wacc=[krs])
            if not own:
                bd = [bring.next() for _ in range(2)]
                for n in range(2):
                    mm(bd[n][:], M2, fsb[:, n * 512:(n + 1) * 512], True, True, [tri, fsb], [bd[n]])
                    act(e2[:, n * 512:(n + 1) * 512], bd[n][:], AF.Exp, [bd[n]], [Acc(e2)])
                bl = bring.next()
                for h in range(8):
                    mm(bl[:, h * 2:h * 2 + 2], fsb[:, h * 128:(h + 1) * 128], ind[:], True, True, [fsb, ind], [bl])
                act(dec[:].rearrange("p h j -> p (h j)"), bl[:, 0:16], AF.Exp, [bl], [dec])
                for j in range(2):
                    stt(kd[j][:], ksb[:], ind[:, j:j + 1], e2[:], ALU.mult, ALU.mult, [ksb, ind, e2], [kd[j]])
                for j in range(2):
                    bs = [bring.next() for _ in range(2)]
                    for h in range(8):
                        mm(bs[h // 4][:, (h % 4) * 128:(h % 4 + 1) * 128], kd[j][:, h * 128:(h + 1) * 128],
                           vsb[:, h * 128:(h + 1) * 128], True, True, [kd[j], vsb], [bs[h // 4]])
                    for h in range(8):
                        stt(S[:, h, :], S[:, h, :], dec[:, h, j:j + 1], bs[h // 4][:, (h % 4) * 128:(h % 4 + 1) * 128],
                            ALU.mult, ALU.add, [Sh[h], dec, bs[h // 4]], [Sh[h]])
        tiles = list(range(NT)) if not sa_flags.startswith("x") else [0, NT - 1]
        gens = [tile_body(t) for t in tiles]
        next(gens[0])
        for i_, gen in enumerate(gens):
            if i_ + 1 < len(gens):
                next(gens[i_ + 1])
            for _ in gen:
                pass
        if "S" in g.tap_out:
            g.dma("pool", g.tap_out["S"], S[:].rearrange("p h d -> p (h d)"), Sh, [])
        tap("kmax2", kmax2[:], kmax2)
        P.barrier()


def phase1b(g):
    P, sb, mm, tr, act, ts, tt, stt, cp, dma, tap = g.P, g.sb, g.mm, g.tr, g.act, g.ts, g.tt, g.stt, g.cp, g.dma, g.tap
    NT, NTP, bring, banks = g.NT, g.NTP, g.bring, g.banks
    identb, identf, tbl, lbrow, omlrow, ind, kval, causal = g.identb, g.identf, g.tbl, g.lbrow, g.omlrow, g.ind, g.kval, g.causal
    Utri, M1, M2, tri = g.Utri, g.M1, g.M2, g.tri
    w_in, S, B = g.w_in, g.S, g.B
    gmix = g.gt["g_mix"]
    oaT, obT = g.oaT, g.obT

    with ExitStack() as sa:
        hTown = sb("hTown", [128, 16, 1024], BF16, sa)
        ss = sb("ss1", [128, 4], F32, sa)
        tmp = sb("tmp1", [128, 4], F32, sa)
        rs = sb("rs1", [128, 4], F32, sa)
        with ExitStack() as sl:
            xring = Ring([sb(f"xo{i}", [128, 2048], F32, sl) for i in range(2)])
            xnring = Ring([sb(f"xno{i}", [128, 2048], BF16, sl) for i in range(2)])
            for j in range(8):
                xt = xring.next()
                dma("sp", xt[:], g.xown[j * 128:(j + 1) * 128, :], [], [xt])
                xn = xnring.next()
                act(xn[:], xt[:], AF.Square, [xt], [xn, ss], accum=ss[:, 0:1])
                g.rstd_from_ss(T(ss.t[:, 0:1], ss.b), D, T(tmp.t[:, 0:1], tmp.b), T(rs.t[:, 0:1], rs.b), None)
                act(xn[:], xt[:], AF.Copy, [xt, rs], [xn], scale=rs[:, 0:1])
                for half in range(2):
                    bk = bring.next()
                    bv = bk.t[:].bitcast(BF16)
                    for kk in range(8):
                        k = half * 8 + kk
                        tr(bv[:, kk * 128:(kk + 1) * 128], xn[:, k * 128:(k + 1) * 128], identb[:], [xn, identb], [bk])
                    cp("dve" if half == 0 else "act", hTown[:, half * 8:(half + 1) * 8, j * 128:(j + 1) * 128],
                       bv.rearrange("p (k t) -> p k t", k=8), [bk], [hTown])
            P.barrier()

        with ExitStack() as sh:
            whr = Ring([sb(f"wh{i}", [128, 16, 512], BF16, sh) for i in range(4)])
            stg = Ring([sb(f"stgh{i}", [128, 4, 128], F32, sh) for i in range(3)])
            ND = 4

            def RN(name, shape, dt):
                return Ring([sb(f"{name}{i}", shape, dt, sh) for i in range(ND)])
            f_r, k_r, q_r = RN("f_", [128, 128], F32), RN("k_", [128, 128], F32), RN("q_", [128, 128], F32)
            v_r, gate_r = RN("v_", [128, 128], BF16), RN("gate", [128, 128], F32)
            e1_r, en1_r, eb_r, e2_r = RN("e1", [128, 128], F32), RN("en1", [128, 128], F32), RN("eb", [128, 128], F32), RN("e2b", [128, 128], F32)
            dec_r = RN("decb", [128, 2], F32)
            qin_r, kin_r, qbp_r = RN("qin", [128, 128], BF16), RN("kin", [128, 128], BF16), RN("qbp", [128, 192], BF16)
            kd_r = [RN(f"kdb{i}", [128, 128], BF16) for i in range(2)]
            atm_r, sb0_r, sb1_r = RN("atm", [128, 128], BF16), RN("sb0", [128, 128], BF16), RN("sb1", [128, 128], BF16)
            on_r, onb_r = RN("on", [128, 128], F32), RN("onb", [128, 128], BF16)
            ss_r, tmp_r, rs_r = RN("ssh", [128, 1], F32), RN("tmph", [128, 1], F32), RN("rsh", [128, 1], F32)
            for qb_ in qbp_r.tiles:
                P.op("dve", lambda e, qb_=qb_: e.memset(qb_[:], 0.0), [], B([qb_]))
            Sh = [T(S.t, P.buf(f"S_h{h}")) for h in range(8)]
            w4 = w_in[:, 0:4096].rearrange("p (s c) -> p s c", s=4)

            def load_head(h):
                wh = whr.next()
                for k in range(16):
                    s = stg.next()
                    dma("sp", s[:], w4[k * 128:(k + 1) * 128, :, h * 128:(h + 1) * 128], [], [s])
                    g.wcast(wh[:, k, :], s[:].rearrange("p s c -> p (s c)"), gmix[:, k:k + 1], [s, gmix], [wh],
                            engines=("act", "dve", "pool", "dve", "act"))
                return wh

            def body(h, j, wh):
                lb_h = lbrow[:, h * 128:(h + 1) * 128]
                oml_h = omlrow[:, h * 128:(h + 1) * 128]
                S_ = Sh[h]
                f_, k_, q_, v_, gate = f_r.next(), k_r.next(), q_r.next(), v_r.next(), gate_r.next()
                e1, en1, eb, e2, dec = e1_r.next(), en1_r.next(), eb_r.next(), e2_r.next(), dec_r.next()
                qin, kin, qbp, atm = qin_r.next(), kin_r.next(), qbp_r.next(), atm_r.next()
                kd = [kd_r[0].next(), kd_r[1].next()]
                sb0, sb1, on, onb = sb0_r.next(), sb1_r.next(), on_r.next(), onb_r.next()
                ss, tmp, rs = ss_r.next(), tmp_r.next(), rs_r.next()
                bp = bring.next()
                for k in range(16):
                    mm(bp[:], hTown[:, k, j * 128:(j + 1) * 128], wh[:, k, :], k == 0, k == 15, [hTown, wh], [bp])
                yield
                act(f_[:], bp[:, 128:256], AF.Sigmoid, [bp], [f_])
                act(gate[:], bp[:, 384:512], AF.Silu, [bp], [gate])
                cp("act", v_[:], bp[:, 256:384], [bp], [v_])
                cp("act", q_[:], bp[:, 0:128], [bp], [q_])
                tt("dve", f_[:], f_[:], oml_h, ALU.mult, [f_, omlrow], [f_])
                tt("dve", f_[:], f_[:], lb_h, ALU.add, [f_, lbrow], [f_])
                ts("dve", k_[:], f_[:], -1.0, 1.0, ALU.mult, ALU.add, [f_], [k_])
                act(f_[:], f_[:], AF.Ln, [f_], [f_])
                yield
                bt = bring.next()
                P.op("pe", lambda e: e.transpose(out=bt[:, 0:128], in_=q_[:], identity=identf[:]), B([q_, identf]), B([bt]))
                P.op("pe", lambda e: e.transpose(out=bt[:, 128:256], in_=k_[:], identity=identf[:]), B([k_, identf]), B([bt]))
                bc = bring.next()
                mm(bc[:, 0:128], f_[:], M1, True, True, [f_, tri], [bc])
                mm(bc[:, 128:256], f_[:], Utri, True, True, [f_, tri], [bc])
                mm(bc[:, 256:384], M2, f_[:], True, True, [f_, tri], [bc])
                mm(bc[:, 384:386], f_[:], ind[:], True, True, [f_, ind], [bc])
                yield
                act(e1[:], bc[:, 0:128], AF.Exp, [bc], [e1])
                act(en1[:], bc[:, 0:128], AF.Exp, [bc], [en1], scale=-1.0)
                act(eb[:], bc[:, 128:256], AF.Exp, [bc], [eb])
                act(e2[:], bc[:, 256:384], AF.Exp, [bc], [e2])
                act(dec[:], bc[:, 384:386], AF.Exp, [bc], [dec])
                tt("dve", qin[:], bt[:, 0:128], e1[:], ALU.mult, [bt, e1], [qin])
                tt("dve", kin[:], bt[:, 128:256], en1[:], ALU.mult, [bt, en1], [kin])
                tt("dve", qbp[:, 0:64], bt[:, 0:64], eb[:, 0:64], ALU.mult, [bt, eb], [qbp])
                tt("dve", qbp[:, 128:192], bt[:, 64:128], eb[:, 64:128], ALU.mult, [bt, eb], [qbp])
                for jj in range(2):
                    stt(kd[jj][:], k_[:], ind[:, jj:jj + 1], e2[:], ALU.mult, ALU.mult, [k_, ind, e2], [kd[jj]])
                yield
                ba = bring.next()
                mm(ba[:, 0:128], kin[:], qin[:], True, True, [kin, qin], [ba])
                bs = bring.next()
                cp("act", sb0[:], S[:, h, :], [S_], [sb0])
                mm(bs[:, 0:128], kd[0][:], v_[:], True, True, [kd[0], v_], [bs])
                mm(bs[:, 128:256], kd[1][:], v_[:], True, True, [kd[1], v_], [bs])
                yield
                tt("dve", atm[:], ba[:, 0:128], Utri, ALU.mult, [ba, tri], [atm])
                stt(S[:, h, :], S[:, h, :], dec[:, 0:1], bs[:, 0:128], ALU.mult, ALU.add, [S_, dec, bs], [S_])
                cp("act", sb1[:], S[:, h, :], [S_], [sb1])
                stt(S[:, h, :], S[:, h, :], dec[:, 1:2], bs[:, 128:256], ALU.mult, ALU.add, [S_, dec, bs], [S_])
                yield
                bo = bring.next()
                mm(bo[:, 0:128], atm[:], v_[:], True, False, [atm, v_], [bo])
                mm(bo[:, 0:128], qbp[:, 0:128], sb0[:], False, False, [qbp, sb0], [bo])
                mm(bo[:, 0:128], qbp[:, 64:192], sb1[:], False, True, [qbp, sb1], [bo])
                yield
                act(on[:], bo[:, 0:128], AF.Square, [bo], [on, ss], accum=ss[:, 0:1])
                g.rstd_from_ss(ss, 128, tmp, rs, None)
                act(on[:], bo[:, 0:128], AF.Copy, [bo, rs], [on], scale=rs[:, 0:1])
                tt("dve", onb[:], on[:], gate[:], ALU.mult, [on, gate], [onb])
                yield
                bz = bring.next()
                bzv = bz.t[:].bitcast(BF16)
                tr(bzv[:, 0:128], onb[:], identb[:], [onb, identb], [bz])
                ts("dve", oaT[:, h, j * 128:(j + 1) * 128], bzv[:, 0:128], g.ghg[:, h:h + 1], None, ALU.mult, None,
                   [bz, g.ghg], [oaT])

            for hp in range(2):
                hs = tuple(range(4 * hp, 4 * hp + 4))
                whs = [load_head(h) for h in hs]
                for j in range(8):
                    alive = [body(h, j, wh) for h, wh in zip(hs, whs)]
                    while alive:
                        for gen in list(alive):
                            try:
                                next(gen)
                            except StopIteration:
                                alive.remove(gen)
            P.barrier()
        tap("oaT", oaT[:].rearrange("p h t -> p (h t)"), oaT)


def phase1c(g):
    P, sb, mm, tr, act, ts, tt, stt, cp, dma, tap = g.P, g.sb, g.mm, g.tr, g.act, g.ts, g.tt, g.stt, g.cp, g.dma, g.tap
    NT, NTP, bring, banks = g.NT, g.NTP, g.bring, g.banks
    identb, identf, tbl, kval, causal = g.identb, g.identf, g.tbl, g.kval, g.causal
    w_in, B = g.w_in, g.B
    gmix = g.gt["g_mix"]
    obT = g.obT
    kts, krs, vs = g.kts, g.krs, g.vs
    SCALE = float(1.0 / np.sqrt(192.0))

    with ExitStack() as sa:
        qnT = sb("qnT", [128, 8, 1024], BF16, sa)
        qra = sb("qra", [65, 8, 1024], BF16, sa)
        kmb = sb("kmb", [128, 8], F32, sa)
        ss = sb("ss2", [128, 4], F32, sa)
        tmp = sb("tmp2", [128, 4], F32, sa)
        rs = sb("rs2", [128, 4], F32, sa)
        ones = sb("ones", [128, 128], BF16, sa)
        P.op("dve", lambda e: e.memset(ones[:], 1.0), [], B([ones]))
        with ExitStack() as sl:
            km = sb("km", [128, 8], F32, sl)
            kmT = sb("kmT", [8, 128], F32, sl)
            kmc = sb("kmc", [8, 1], F32, sl)
            dg = sb("dg", [8, 8], F32, sl)
            onesf = sb("onesf", [8, 128], F32, sl)
            ts("dve", km[:], g.kmax2[:], g.krmax2[:, 0:1], None, ALU.add, None, [g.kmax2, g.krmax2], [km])
            bk = bring.next()
            P.op("pe", lambda e: e.transpose(out=bk[0:8, 0:128], in_=km[:], identity=identf[:]), B([km, identf]), B([bk]))
            cp("dve", kmT[:], bk[0:8, 0:128], [bk], [kmT])
            P.op("dve", lambda e: e.tensor_reduce(out=kmc[:], in_=kmT[:], axis=AX.X, op=ALU.max), B([kmT]), B([kmc]))
            act(kmc[:], kmc[:], AF.Sqrt, [kmc], [kmc])
            ts("dve", dg[:], identf[0:8, 0:8], kmc[:, 0:1], None, ALU.mult, None, [identf, kmc], [dg])
            P.op("dve", lambda e: e.memset(onesf[:], 1.0), [], B([onesf]))
            bk2 = bring.next()
            mm(bk2[:, 0:8], onesf[:], dg[:], True, True, [onesf, dg], [bk2])
            cp("dve", kmb[:], bk2[:, 0:8], [bk2], [kmb])
            P.barrier()
        tap("kmb", kmb[:], kmb)

        with ExitStack() as sq_:
            hTown = sb("hTown2", [128, 16, 128], BF16, sq_)
            xt = sb("xq", [128, 2048], F32, sq_)
            xn = sb("xnq", [128, 2048], BF16, sq_)
            wq = sb("wq", [128, 16, 512], BF16, sq_)
            wuq = sb("wuq", [128, 4, 1536], BF16, sq_)
            stg = Ring([sb(f"stgq{i}", [128, 1536], F32, sq_) for i in range(2)])
            cqn = sb("cqn", [128, 512], BF16, sq_)
            cqT = sb("cqT", [128, 4, 128], BF16, sq_)
            sqT = sb("sqT", [128, 8, 128], BF16, sq_)
            junk = sb("junkq", [128, 512], BF16, sq_)
            qr2 = sb("qr2", [128, 8, 64], F32, sq_)
            ra = sb("raq", [128, 8, 64], F32, sq_)
            rb = sb("rbq", [128, 8, 64], F32, sq_)
            qrp = sb("qrp", [128, 8, 64], BF16, sq_)
            qn2 = sb("qn2", [128, 8], F32, sq_)
            qn2b = sb("qn2b", [128, 8], F32, sq_)
            shT = sb("shT", [8, 128], BF16, sq_)
            for k in range(16):
                s = stg.next()
                dma("sp", s[:, 0:512], w_in[k * 128:(k + 1) * 128, 4096:4608], [], [s])
                g.wcast(wq[:, k, :], s[:, 0:512], gmix[:, k:k + 1], [s, gmix], [wq])
            for c in range(4):
                s = stg.next()
                dma("sp", s[:, :], g.w_uq[c * 128:(c + 1) * 128, :], [], [s])
                g.wcast(wuq[:, c, :], s[:, :], g.gq[:, c:c + 1], [s, g.gq], [wuq])
            wuq3 = wuq.t[:].rearrange("p c (h e) -> p c h e", h=8)
            for j in range(8):
                t = NTP + j
                dma("sp", xt[:], g.xown[j * 128:(j + 1) * 128, :], [], [xt])
                act(xn[:], xt[:], AF.Square, [xt], [xn, ss], accum=ss[:, 0:1])
                g.rstd_from_ss(T(ss.t[:, 0:1], ss.b), D, T(tmp.t[:, 0:1], tmp.b), T(rs.t[:, 0:1], rs.b), None)
                act(xn[:], xt[:], AF.Copy, [xt, rs], [xn], scale=rs[:, 0:1])
                for half in range(2):
                    bk = bring.next()
                    bv = bk.t[:].bitcast(BF16)
                    for kk in range(8):
                        k = half * 8 + kk
                        tr(bv[:, kk * 128:(kk + 1) * 128], xn[:, k * 128:(k + 1) * 128], identb[:], [xn, identb], [bk])
                    cp("dve" if half == 0 else "act", hTown[:, half * 8:(half + 1) * 8, :],
                       bv.rearrange("p (k t) -> p k t", k=8), [bk], [hTown])
                bq = bring.next()
                for k in range(16):
                    mm(bq[:], hTown[:, k, :], wq[:, k, :], k == 0, k == 15, [hTown, wq], [bq])
                act(junk[:], bq[:], AF.Square, [bq], [junk, ss], accum=ss[:, 1:2])
                g.rstd_from_ss(T(ss.t[:, 1:2], ss.b), 512, T(tmp.t[:, 1:2], tmp.b), T(rs.t[:, 1:2], rs.b), None)
                act(cqn[:], bq[:], AF.Copy, [bq, rs], [cqn], scale=rs[:, 1:2])
                bk = bring.next()
                bv = bk.t[:].bitcast(BF16)
                for c in range(4):
                    tr(bv[:, c * 128:(c + 1) * 128], cqn[:, c * 128:(c + 1) * 128], identb[:], [cqn, identb], [bk])
                cp("dve", cqT[:], bv[:, 0:512].rearrange("p (c t) -> p c t", c=4), [bk], [cqT])
                bqn = [bring.next() for _ in range(2)]
                for h in range(8):
                    for c in range(4):
                        mm(bqn[h // 4][:, (h % 4) * 128:(h % 4 + 1) * 128], wuq3[:, c, h, 0:128], cqT[:, c, :], c == 0, c == 3,
                           [wuq, cqT], [bqn[h // 4]])
                for n in range(2):
                    cp("dve", qnT[:, n * 4:(n + 1) * 4, j * 128:(j + 1) * 128], bqn[n][:].rearrange("p (h t) -> p h t", h=4),
                       [bqn[n]], [qnT])
                    act(sqT[:, n * 4:(n + 1) * 4, :], bqn[n][:].rearrange("p (h t) -> p h t", h=4), AF.Square, [bqn[n]], [sqT])
                bqr = bring.next()
                for c in range(4):
                    mm(bqr[:].rearrange("p (h e) -> p h e", h=8), cqT[:, c, :], wuq3[:, c, :, 128:192], c == 0, c == 3,
                       [cqT, wuq], [bqr])
                bqr3 = bqr[:].rearrange("p (h e) -> p h e", h=8)
                bqr4 = bqr[:].rearrange("p (h two d) -> p h two d", h=8, two=2)
                bn = bring.next()
                for h in range(8):
                    mm(bn[:, h:h + 1], sqT[:, h, :], ones[:, 0:1], True, True, [sqT, ones], [bn])
                act(qr2[:], bqr3, AF.Square, [bqr], [qr2])
                P.op("dve", lambda e: e.tensor_reduce(out=qn2[:], in_=qr2[:], axis=AX.X, op=ALU.add), B([qr2]), B([qn2]))
                tt("dve", qn2[:], qn2[:], bn[:, 0:8], ALU.add, [qn2, bn], [qn2])
                act(qn2[:], qn2[:], AF.Sqrt, [qn2], [qn2])
                stt(qn2b[:], qn2[:], -1.0, kmb[:], ALU.mult, ALU.mult, [qn2, kmb], [qn2b])
                bsh = bring.next()
                P.op("pe", lambda e, bsh=bsh: e.transpose(out=bsh[0:8, 0:128], in_=qn2b[:], identity=identf[:]),
                     B([qn2b, identf]), B([bsh]))
                cp("dve", shT[:], bsh[0:8, 0:128], [bsh], [shT])
                dma("pool", qra[64:65, :, j * 128:(j + 1) * 128], shT[:], [shT], [], wacc=[qra])
                cs = tbl[:, t, 0:32]
                sn = tbl[:, t, 32:64]
                tt("dve", ra[:].rearrange("p h (two d) -> p h two d", two=2), bqr4,
                   cs.unsqueeze(1).unsqueeze(1).to_broadcast([128, 8, 2, 32]), ALU.mult, [bqr, tbl], [ra])
                snb = sn.unsqueeze(1).to_broadcast([128, 8, 32])
                tt("dve", rb[:, :, 0:32], bqr3[:, :, 32:64], snb, ALU.mult, [bqr, tbl], [rb])
                tt("dve", rb[:, :, 32:64], bqr3[:, :, 0:32], snb, ALU.mult, [bqr, tbl], [rb])
                tt("dve", qrp[:, :, 0:32], ra[:, :, 0:32], rb[:, :, 0:32], ALU.subtract, [ra, rb], [qrp])
                tt("dve", qrp[:, :, 32:64], ra[:, :, 32:64], rb[:, :, 32:64], ALU.add, [ra, rb], [qrp])
                bk = bring.next()
                bv = bk.t[:].bitcast(BF16)
                for h in range(8):
                    tr(bv[0:64, h * 128:(h + 1) * 128], qrp[:, h, :], identb[:], [qrp, identb], [bk])
                cp("act", qra[0:64, :, j * 128:(j + 1) * 128], bv[0:64, :].rearrange("p (h t) -> p h t", h=8), [bk], [qra])
            P.barrier()
        tap("qnT", qnT[:].rearrange("p h t -> p (h t)"), qnT)
        tap("qra", qra[:].rearrange("p h t -> p (h t)"), qra, lambda d: d[0:65, :])

        with ExitStack() as st_:
            KR = sb("KR", [65, NT * 128], BF16, st_)
            KTr = Ring([sb(f"KTh{i}", [128, NT, 128], BF16, st_) for i in range(2)])
            Vr = Ring([sb(f"Vh{i}", [128, NT, 129], BF16, st_) for i in range(2)])
            PTr = Ring([sb(f"PT{i}", [128, 512], BF16, st_) for i in range(3)])
            rz = sb("rz", [128, 1], F32, st_)
            ob = sb("ob", [128, 128], BF16, st_)
            dma("sp", KR[0:64, :], g.KR_s, [krs], [KR])
            P.op("dve", lambda e: e.memset(KR[64:65, :], 1.0), [], B([KR]))
            acc = banks[0:4]
            sring = Ring(banks[4:8])
            V4 = g.V_s.rearrange("t p h d -> p t h d")
            for h in range(8):
                KTh = KTr.next()
                Vh = Vr.next()
                dma("sp", KTh[:], g.KT_s[:, :, h, :], [kts], [KTh])
                dma("sp", Vh[:, :, 0:128], V4[:, :, h, :], [vs], [Vh])
                cp("dve", Vh[:, :, 128:129], kval[:].unsqueeze(2), [kval, Vh], [Vh])
                for qt in range(2):
                    nkb = NTP + 4 * qt + 4
                    def issueS(kb, qt=qt, h=h, KTh=KTh):
                        dg_i = kb - (NTP + 4 * qt)
                        q0 = max(0, dg_i) * 128
                        c0 = qt * 512 + q0
                        c1 = qt * 512 + 512
                        st = sring.next()
                        mm(st[:, q0:512], KTh[:, kb, :], qnT[:, h, c0:c1], True, False, [KTh, qnT], [st])
                        mm(st[:, q0:512], KR[0:65, kb * 128:(kb + 1) * 128], qra[0:65, h, c0:c1], False, True, [KR, qra], [st])
                        return st, q0, dg_i

                    pend = issueS(0)
                    for kb in range(nkb):
                        st, q0, dg_i = pend
                        if kb + 1 < nkb:
                            pend = issueS(kb + 1)
                        PT = PTr.next()
                        act(PT[:, q0:512], st[:, q0:512], AF.Exp, [st], [PT], scale=SCALE)
                        if dg_i >= 0:
                            tt("dve", PT[:, q0:q0 + 128], PT[:, q0:q0 + 128], causal[:], ALU.mult, [PT, causal], [PT])
                        for jq in range(max(0, dg_i), 4):
                            last = NTP + 4 * qt + jq
                            mm(acc[jq][:, 0:129], PT[:, jq * 128:(jq + 1) * 128], Vh[:, kb, :], kb == 0, kb == last,
                               [PT, Vh], [acc[jq]])
                    for jq in range(4):
                        P.op("dve", lambda e, jq=jq: e.reciprocal(out=rz[:], in_=acc[jq][:, 128:129]), B([acc[jq]]), B([rz]))
                        act(ob[:], acc[jq][:, 0:128], AF.Copy, [acc[jq], rz], [ob], scale=rz[:, 0:1])
                        bz = sring.next()
                        bzv = bz.t[:].bitcast(BF16)
                        tr(bzv[:, 0:128], ob[:], identb[:], [ob, identb], [bz])
                        col = qt * 512 + jq * 128
                        cp("dve", obT[:, h, col:col + 128], bzv[:, 0:128], [bz], [obT])
            P.barrier()
        tap("obT", obT[:].rearrange("p h t -> p (h t)"), obT)


def prep_inputs(inp, NPREV=7, ncores=NCORES, lite=False):
    f = lambda a: np.ascontiguousarray(np.asarray(a))
    X = f(inp["x"])[0]
    pos = f(inp["positions"])[0].astype(np.int32)
    NT = NPREV * 8 + 8
    shared = {
        "w_in": f(inp["w_in"])[0],
        "g_mix": f(f(inp["norm_mix"])[0].reshape(16, 128).T),
        "g_ffn": f(f(inp["norm_ffn"])[0].reshape(16, 128).T),
        "g_ple": f(f(inp["norm_ple"])[0].reshape(16, 128).T),
        "g_final": f(inp["norm_final"]).reshape(1, D),
        "lbT": f(f(inp["lb_logits"]).reshape(2, 8, 128).transpose(2, 0, 1).reshape(128, 16)),
        "lb_logits": f(inp["lb_logits"]),
        "g_hg": f(f(inp["hg_norm"])[0].reshape(8, 128).T),
        "g_q": f(f(inp["mla_q_norm"])[0].reshape(4, 128).T),
        "g_kv": f(f(inp["mla_kv_norm"])[0].reshape(4, 128).T),
        "w_uq": f(inp["w_uq"])[0], "w_ukv": f(inp["w_ukv"])[0],
        "w_a": f(inp["w_a"])[0], "w_b": f(inp["w_b"])[0], "w_o": f(inp["w_o"])[0],
        "peer_wq": f(inp["peer_wq"])[0],
        "k1T": f(f(inp["peer_k1"])[0].T), "k2T": f(f(inp["peer_k2"])[0].T),
        "peer_uT": f(f(inp["peer_u"])[0].T), "peer_v": f(inp["peer_v"])[0],
        "w_pg": f(inp["w_pg"])[0], "w_pe": f(inp["w_pe"])[0],
    }
    if lite:
        shared["peer_uT"] = f(shared["peer_uT"][:, :128])
        shared["peer_v"] = f(shared["peer_v"][:128])
    shared.update(host_consts())
    maps = []
    for c in range(ncores):
        xprev = np.zeros((max(NPREV, 1) * 1024, D), np.float32)
        pall = np.zeros((NT * 128,), np.int32)
        valid = np.zeros((NT * 128,), np.float32)
        for s_ in range(NPREV):
            blk = c - NPREV + s_
            if blk >= 0:
                xprev[s_ * 1024:(s_ + 1) * 1024] = X[blk * 1024:(blk + 1) * 1024]
                pall[s_ * 1024:(s_ + 1) * 1024] = pos[blk * 1024:(blk + 1) * 1024]
                valid[s_ * 1024:(s_ + 1) * 1024] = 1.0
        pall[NPREV * 1024:] = pos[c * 1024:(c + 1) * 1024]
        valid[NPREV * 1024:] = 1.0
        m = dict(shared)
        m["xprev"] = xprev
        m["xown"] = f(X[c * 1024:(c + 1) * 1024])
        m["posT"] = f(pall.reshape(NT, 128).T)
        m["kvalid"] = f(valid.reshape(NT, 128).T)
        m["p_own"] = f(f(inp["p"])[0, 0, c * 1024:(c + 1) * 1024, :])
        maps.append(m)
    return maps


def own_hT(g, dst, st, src_rows, gain=None, nt=8, tag="h"):
    P, sb, tr, act, ts, cp, dma, bring, B = g.P, g.sb, g.tr, g.act, g.ts, g.cp, g.dma, g.bring, g.B
    xring = Ring([sb(f"{tag}x{i}", [128, 2048], F32, st) for i in range(2)])
    xnring = Ring([sb(f"{tag}xn{i}", [128, 2048], BF16, st) for i in range(2)])
    ss = sb(f"{tag}ss", [128, 1], F32, st)
    tmp = sb(f"{tag}tmp", [128, 1], F32, st)
    rs = sb(f"{tag}rs", [128, 1], F32, st)
    for j in range(nt):
        xt = xring.next()
        dma("sp", xt[:], src_rows(j), g.xres_dep, [xt])
        xn = xnring.next()
        act(xn[:], xt[:], AF.Square, [xt], [xn, ss], accum=ss[:, 0:1])
        g.rstd_from_ss(ss, D, tmp, rs, None)
        act(xn[:], xt[:], AF.Copy, [xt, rs], [xn], scale=rs[:, 0:1])
        for half in range(2):
            bk = bring.next()
            bv = bk.t[:].bitcast(BF16)
            for kk in range(8):
                k = half * 8 + kk
                tr(bv[:, kk * 128:(kk + 1) * 128], xn[:, k * 128:(k + 1) * 128], g.identb[:], [xn, g.identb], [bk])
            if gain is None:
                cp("dve" if half == 0 else "act", dst[:, half * 8:(half + 1) * 8, j * 128:(j + 1) * 128],
                   bv.rearrange("p (k t) -> p k t", k=8), [bk], [Acc(dst)])
            else:
                for kk in range(8):
                    k = half * 8 + kk
                    ts("dve", dst[:, k, j * 128:(j + 1) * 128], bv[:, kk * 128:(kk + 1) * 128], gain[:, k:k + 1], None,
                       ALU.mult, None, [bk, gain], [Acc(dst)])


def load_w(g, dst, src_fn, nk, width, gain, stg):
    for k in range(nk):
        s = stg.next()
        g.dma("sp", s[:, 0:width], src_fn(k), [], [s])
        if gain is None:
            g.wcast(dst[:, k, 0:width], s[:, 0:width], None, [s], [dst])
        else:
            g.wcast(dst[:, k, 0:width], s[:, 0:width], gain[:, k:k + 1], [s, gain], [dst])


def phase2(g):
    P, sb, mm, tr, act, ts, tt, stt, cp, dma, tap = g.P, g.sb, g.mm, g.tr, g.act, g.ts, g.tt, g.stt, g.cp, g.dma, g.tap
    bring, B, w_in = g.bring, g.B, g.w_in
    gmix = g.gt["g_mix"]
    oaT, obT = g.oaT, g.obT
    with ExitStack() as sa:
        hT = sb("hTm", [128, 16, 1024], BF16, sa)
        ysb = sb("ysb", [128, 8, 2048], BF16, sa)
        with ExitStack() as sl:
            own_hT(g, hT, sl, lambda j: g.xown[j * 128:(j + 1) * 128, :], None, 8, "m")
            P.barrier()
        with ExitStack() as sw:
            CW = 256
            wga_r = Ring([sb(f"wga{i}", [128, 16, CW], BF16, sw) for i in range(2)])
            wgb_r = Ring([sb(f"wgb{i}", [128, 16, CW], BF16, sw) for i in range(2)])
            wa_r = Ring([sb(f"wa{i}", [128, 8, CW], BF16, sw) for i in range(2)])
            wb_r = Ring([sb(f"wb{i}", [128, 8, CW], BF16, sw) for i in range(2)])
            stg = Ring([sb(f"stgm{i}", [128, CW], F32, sw) for i in range(4)])
            sga_r = Ring([sb(f"sga{i}", [128, CW], F32, sw) for i in range(2)])
            sgb_r = Ring([sb(f"sgb{i}", [128, CW], F32, sw) for i in range(2)])
            y1_r = Ring([sb(f"y1{i}", [128, CW], F32, sw) for i in range(2)])
            for n in range(D // CW):
                c0, c1 = n * CW, (n + 1) * CW
                wga, wgb, wa, wb = wga_r.next(), wgb_r.next(), wa_r.next(), wb_r.next()
                load_w(g, wga, lambda k: w_in[k * 128:(k + 1) * 128, 5184 + c0:5184 + c1], 16, CW, gmix, stg)
                load_w(g, wgb, lambda k: w_in[k * 128:(k + 1) * 128, 7232 + c0:7232 + c1], 16, CW, gmix, stg)
                load_w(g, wa, lambda k: g.w_a[k * 128:(k + 1) * 128, c0:c1], 8, CW, None, stg)
                load_w(g, wb, lambda k: g.w_b[k * 128:(k + 1) * 128, c0:c1], 8, CW, None, stg)
                for j in range(8):
                    tok = slice(j * 128, (j + 1) * 128)
                    bga, bgb = bring.next(), bring.next()
                    ba, bb = bga, bgb
                    for k in range(16):
                        mm(bga[:, 0:CW], hT[:, k, tok], wga[:, k, :], k == 0, k == 15, [hT, wga], [bga])
                    for h in range(8):
                        mm(ba[:, CW:2 * CW], oaT[:, h, tok], wa[:, h, :], h == 0, h == 7, [oaT, wa], [ba])
                    for k in range(16):
                        mm(bgb[:, 0:CW], hT[:, k, tok], wgb[:, k, :], k == 0, k == 15, [hT, wgb], [bgb])
                    for h in range(8):
                        mm(bb[:, CW:2 * CW], obT[:, h, tok], wb[:, h, :], h == 0, h == 7, [obT, wb], [bb])
                    sga, sgb, y1 = sga_r.next(), sgb_r.next(), y1_r.next()
                    act(sga[:], bga[:, 0:CW], AF.Sigmoid, [bga], [sga])
                    act(sgb[:], bgb[:, 0:CW], AF.Sigmoid, [bgb], [sgb])
                    tt("dve", y1[:], ba[:, CW:2 * CW], sga[:], ALU.mult, [ba, sga], [y1])
                    tt("dve", sgb[:], bb[:, CW:2 * CW], sgb[:], ALU.mult, [bb, sgb], [sgb])
                    tt("dve", ysb[:, j, c0:c1], y1[:], sgb[:], ALU.add, [y1, sgb], [Acc(ysb)])
            P.barrier()
        tap("y", ysb[:].rearrange("p j d -> p (j d)"), ysb)
        for j in range(8):
            for half in range(2):
                bk = bring.next()
                bv = bk.t[:].bitcast(BF16)
                for kk in range(8):
                    k = half * 8 + kk
                    tr(bv[:, kk * 128:(kk + 1) * 128], ysb[:, j, k * 128:(k + 1) * 128], g.identb[:], [ysb, g.identb], [bk])
                cp("dve" if half == 0 else "act", hT[:, half * 8:(half + 1) * 8, j * 128:(j + 1) * 128],
                   bv.rearrange("p (k t) -> p k t", k=8), [bk], [hT])
        with ExitStack() as sw:
            wo_r = Ring([sb(f"wo{i}", [128, 16, 512], BF16, sw) for i in range(2)])
            stg = Ring([sb(f"stgo{i}", [128, 512], F32, sw) for i in range(4)])
            xr = Ring([sb(f"xr{i}", [128, 512], F32, sw) for i in range(3)])
            for n in range(4):
                wo = wo_r.next()
                load_w(g, wo, lambda k: g.w_o[k * 128:(k + 1) * 128, n * 512:(n + 1) * 512], 16, 512, None, stg)
                for j in range(8):
                    tok = slice(j * 128, (j + 1) * 128)
                    bo = bring.next()
                    for k in range(16):
                        mm(bo[:], hT[:, k, tok], wo[:, k, :], k == 0, k == 15, [hT, wo], [bo])
                    x = xr.next()
                    dma("sp", x[:], g.xown[tok, n * 512:(n + 1) * 512], [], [x])
                    tt("dve", x[:], bo[:], x[:], ALU.add, [bo, x], [x])
                    dma("pool", g.xres[tok, n * 512:(n + 1) * 512], x[:], [x], [], wacc=[g.xres_t])
            P.barrier()
    g.xres_dep = [g.xres_t]


def phase4(g):
    P, sb, mm, tr, act, ts, tt, stt, cp, dma, tap = g.P, g.sb, g.mm, g.tr, g.act, g.ts, g.tt, g.stt, g.cp, g.dma, g.tap
    bring, B = g.bring, g.B
    with ExitStack() as sa:
        hT = sb("hTp", [128, 16, 1024], BF16, sa)
        pT = sb("pT", [128, 2, 1024], BF16, sa)
        x3 = sb("x3", [128, 8, 2048], F32, sa)
        gfin = sb("gfin", [128, 2048], F32, sa)
        ssq = sb("ssq", [128, 8, 4], F32, sa)
        dma("sp", gfin[:], g.g_final[0:1, :].to_broadcast([128, 2048]), [], [gfin])
        with ExitStack() as sl:
            own_hT(g, hT, sl, lambda j: g.xres[j * 128:(j + 1) * 128, :], g.gt["g_ple"], 8, "p")
            pt_ = sb("pt_", [128, 256], F32, sl)
            ptb = sb("ptb", [128, 256], BF16, sl)
            for j in range(8):
                dma("sp", pt_[:], g.p_own[j * 128:(j + 1) * 128, :], [], [pt_])
                cp("dve", ptb[:], pt_[:], [pt_], [ptb])
                bk = bring.next()
                bv = bk.t[:].bitcast(BF16)
                for c in range(2):
                    tr(bv[:, c * 128:(c + 1) * 128], ptb[:, c * 128:(c + 1) * 128], g.identb[:], [ptb, g.identb], [bk])
                cp("act", pT[:, :, j * 128:(j + 1) * 128], bv[:, 0:256].rearrange("p (c t) -> p c t", c=2), [bk], [pT])
            P.barrier()
        with ExitStack() as sw:
            wpg_r = Ring([sb(f"wpg{i}", [128, 16, 512], BF16, sw) for i in range(2)])
            wpe_r = Ring([sb(f"wpe{i}", [128, 2, 512], BF16, sw) for i in range(2)])
            stg = Ring([sb(f"stgp{i}", [128, 512], F32, sw) for i in range(4)])
            sg = sb("sgp", [128, 512], F32, sw)
            junk = sb("junkp", [128, 512], F32, sw)
            xr = Ring([sb(f"xrp{i}", [128, 512], F32, sw) for i in range(3)])
            for n in range(4):
                cols = slice(n * 512, (n + 1) * 512)
                wpg, wpe = wpg_r.next(), wpe_r.next()
                load_w(g, wpg, lambda k: g.w_pg[k * 128:(k + 1) * 128, cols], 16, 512, None, stg)
                load_w(g, wpe, lambda k: g.w_pe[k * 128:(k + 1) * 128, cols], 2, 512, None, stg)
                for j in range(8):
                    tok = slice(j * 128, (j + 1) * 128)
                    bg, be = bring.next(), bring.next()
                    for k in range(16):
                        mm(bg[:], hT[:, k, tok], wpg[:, k, :], k == 0, k == 15, [hT, wpg], [bg])
                    for c in range(2):
                        mm(be[:], pT[:, c, tok], wpe[:, c, :], c == 0, c == 1, [pT, wpe], [be])
                    x = xr.next()
                    dma("sp", x[:], g.xres[tok, cols], g.xres_dep, [x])
                    act(sg[:], bg[:], AF.Sigmoid, [bg], [sg])
                    tt("dve", sg[:], be[:], sg[:], ALU.mult, [be, sg], [sg])
                    tt("dve", x3[:, j, cols], x[:], sg[:], ALU.add, [x, sg], [Acc(x3)])
                    act(junk[:], x3[:, j, cols], AF.Square, [x3], [junk, ssq], accum=ssq[:, j, n:n + 1])
            P.barrier()
        tap("x3", x3[:].rearrange("p j d -> p (j d)"), x3)
        with ExitStack() as sf:
            tot = sb("tot", [128, 8], F32, sf)
            tmp = sb("tmpf", [128, 8], F32, sf)
            rs = sb("rsf", [128, 8], F32, sf)
            P.op("dve", lambda e: e.tensor_reduce(out=tot[:], in_=ssq[:], axis=AX.X, op=ALU.add), B([ssq]), B([tot]))
            g.rstd_from_ss(tot, D, tmp, rs, None)
            tap("rsf", rs[:], rs)
            tap("tot", tot[:], tot)
            tap("ssq", ssq[:].rearrange("p j n -> p (j n)"), ssq)
            tap("gfin", gfin[:], gfin)
            for j in range(8):
                stt(x3[:, j, :], x3[:, j, :], rs[:, j:j + 1], gfin[:], ALU.mult, ALU.mult, [x3, rs, gfin], [x3])
                dma("sp", g.out[j * 128:(j + 1) * 128, :], x3[:, j, :], [x3], [])
            P.barrier()


STOP_AFTER = None


def kernel(**inputs):
    maps = prep_inputs(inputs, NPREV=7, ncores=NCORES)
    nc = build(NPREV=7, stop_after=STOP_AFTER)
    res = run_bass_kernel_spmd(nc, maps, core_ids=list(range(NCORES)))
    return np.concatenate([r["out"] for r in res.results], axis=0).reshape(1, NCORES * 1024, D).astype(np.float32)


def phase3(g):
    P, sb, mm, tr, act, ts, tt, stt, cp, dma, tap = g.P, g.sb, g.mm, g.tr, g.act, g.ts, g.tt, g.stt, g.cp, g.dma, g.tap
    bring, banks, B = g.bring, g.banks, g.B
    identf, identb = g.identf, g.identb
    TP, NI = 512, 64
    NEG = -1.0e30
    uT3 = g.peer_uT.rearrange("(k p) e -> p k e", p=128)
    v3 = g.peer_v.rearrange("(i p) d -> p i d", p=128)
    with ExitStack() as s3:
        k1b = sb("k1b", [128, 128], BF16, s3)
        k2b = sb("k2b", [128, 128], BF16, s3)
        with ExitStack() as sl:
            kf = sb("kf", [128, 256], F32, sl)
            dma("sp", kf[:, 0:128], g.k1T, [], [kf])
            dma("sp", kf[:, 128:256], g.k2T, [], [kf])
            cp("dve", k1b[:], kf[:, 0:128], [kf], [k1b])
            cp("dve", k2b[:], kf[:, 128:256], [kf], [k2b])
            P.barrier()
        for ps in range(1024 // TP):
            with ExitStack() as sp_:
                hnT = sb(f"hnT{ps}", [128, 16, TP], BF16, sp_)
                qpT = sb(f"qpT{ps}", [128, 16, TP], BF16, sp_)
                statT = sb(f"statT{ps}", [128, 4, TP], F32, sp_)
                xacc = sb(f"xacc{ps}", [128, 4, 2048], F32, sp_)
                for jl in range(4):
                    dma("sp", xacc[:, jl, :], g.xres[(ps * 4 + jl) * 128:(ps * 4 + jl + 1) * 128, :], g.xres_dep, [xacc])
                with ExitStack() as sl:
                    own_hT(g, hnT, sl, lambda j: g.xres[(ps * 4 + j) * 128:(ps * 4 + j + 1) * 128, :], g.gt["g_ffn"], 4, f"f{ps}")
                    P.barrier()
                with ExitStack() as sq_:
                    wqp_r = Ring([sb(f"wqp{ps}{i}", [128, 16, 512], BF16, sq_) for i in range(2)])
                    stg = Ring([sb(f"stg3{ps}{i}", [128, 512], F32, sq_) for i in range(4)])
                    for mg in range(4):
                        wqp = wqp_r.next()
                        load_w(g, wqp, lambda k: g.peer_wq[k * 128:(k + 1) * 128, mg * 512:(mg + 1) * 512], 16, 512, None, stg)
                        for m4 in range(4):
                            bq = bring.next()
                            for k in range(16):
                                mm(bq[:], wqp[:, k, m4 * 128:(m4 + 1) * 128], hnT[:, k, :], k == 0, k == 15, [wqp, hnT], [bq])
                            cp("act" if m4 % 2 == 0 else "dve", qpT[:, mg * 4 + m4, :], bq[:], [bq], [Acc(qpT)])
                    P.barrier()
                with ExitStack() as sk_:
                    m8 = sb(f"m8{ps}", [128, 16, 16], F32, sk_)
                    wk = sb(f"wk{ps}", [128, 128], F32, sk_)
                    cand = sb(f"cand{ps}", [128, 8, 256], F32, sk_)
                    wk2 = sb(f"wk2{ps}", [128, 256], F32, sk_)
                    c8 = sb(f"c8{ps}", [128, 8, 16], F32, sk_)
                    ex = sb(f"ex{ps}", [128, 8, 16], F32, sk_)
                    zz = sb(f"zz{ps}", [128, 8], F32, sk_)
                    rz = sb(f"rz3{ps}", [128, 8], F32, sk_)
                    st4 = sb(f"st4{ps}", [128, 4, 8, 16], F32, sk_)
                    for jl in range(4):
                        tok = slice(jl * 128, (jl + 1) * 128)
                        bsc = [bring.next() for _ in range(4)]
                        for m in range(16):
                            mm(bsc[m // 4][:, (m % 4) * 128:(m % 4 + 1) * 128], qpT[:, m, tok], (k1b if m % 2 == 0 else k2b)[:],
                               True, True, [qpT, k1b, k2b], [bsc[m // 4]])
                        for m in range(16):
                            sc = bsc[m // 4][:, (m % 4) * 128:(m % 4 + 1) * 128]
                            bb = bsc[m // 4]
                            P.op("dve", lambda e, sc=sc, m=m: e.max(out=m8[:, m, 0:8], in_=sc), B([bb]), B([m8]))
                            P.op("dve", lambda e, sc=sc, m=m: e.match_replace(out=wk[:], in_to_replace=m8[:, m, 0:8], in_values=sc,
                                                                              imm_value=NEG), B([bb, m8]), B([wk]))
                            P.op("dve", lambda e, m=m: e.max(out=m8[:, m, 8:16], in_=wk[:]), B([wk]), B([m8]))
                        m84 = m8[:].rearrange("p (h two) a -> p h two a", two=2)
                        v1 = m84[:, :, 0, :]
                        v2 = m84[:, :, 1, :]
                        tt("dve", cand[:].rearrange("p h (a b) -> p h a b", a=16), v1.unsqueeze(3).to_broadcast([128, 8, 16, 16]),
                           v2.unsqueeze(2).to_broadcast([128, 8, 16, 16]), ALU.add, [m8], [cand])
                        for h in range(8):
                            P.op("dve", lambda e, h=h: e.max(out=c8[:, h, 0:8], in_=cand[:, h, :]), B([cand]), B([c8]))
                            P.op("dve", lambda e, h=h: e.match_replace(out=wk2[:], in_to_replace=c8[:, h, 0:8], in_values=cand[:, h, :],
                                                                       imm_value=NEG), B([cand, c8]), B([wk2]))
                            P.op("dve", lambda e, h=h: e.max(out=c8[:, h, 8:16], in_=wk2[:]), B([wk2]), B([c8]))
                        mx = c8[:, :, 0:1]
                        tau = c8[:, :, 15:16]
                        tt("dve", ex[:], c8[:], mx.to_broadcast([128, 8, 16]), ALU.subtract, [c8], [ex])
                        act(ex[:], ex[:], AF.Exp, [ex], [ex])
                        P.op("dve", lambda e: e.tensor_reduce(out=zz[:], in_=ex[:], axis=AX.X, op=ALU.add), B([ex]), B([zz]))
                        P.op("dve", lambda e: e.reciprocal(out=rz[:], in_=zz[:]), B([zz]), B([rz]))
                        cp("dve", st4[:, 0, :, :], v1, [m8], [st4])
                        tt("dve", st4[:, 1, :, :], v1, mx.to_broadcast([128, 8, 16]), ALU.subtract, [m8, c8], [st4])
                        tt("dve", st4[:, 2, :, :], tau.to_broadcast([128, 8, 16]), v1, ALU.subtract, [m8, c8], [st4])
                        ts("dve", st4[:, 2, :, :], st4[:, 2, :, :], -2.0e-5, None, ALU.add, None, [st4], [st4])
                        cp("dve", st4[:, 3, :, :], rz[:].unsqueeze(2).to_broadcast([128, 8, 16]), [rz], [st4])
                        bt = bring.next()
                        for q in range(4):
                            P.op("pe", lambda e, q=q, bt=bt: e.transpose(out=bt[:, q * 128:(q + 1) * 128],
                                                                         in_=st4[:, q, :, :].rearrange("p h a -> p (h a)"),
                                                                         identity=identf[:]), B([st4, identf]), B([bt]))
                        cp("act", statT[:, :, tok], bt[:].rearrange("p (q t) -> p q t", q=4), [bt], [statT])
                    P.barrier()
                qp4 = qpT.t[:].rearrange("p (h two) t -> p h two t", two=2)
                for sub in range(128 // NI):
                    i0 = sub * NI
                    with ExitStack() as sg_:
                        GT = sb(f"GT{ps}{sub}", [128, NI, TP], BF16, sg_)
                        Ptr = Ring([sb(f"Pt{ps}{sub}{i}", [128, NI], BF16, sg_) for i in range(6)])
                        Er = Ring([sb(f"E{ps}{sub}{i}", [128, 128], F32, sg_) for i in range(6)])
                        Qr = Ring([sb(f"Qt{ps}{sub}{i}", [128, 128], BF16, sg_) for i in range(6)])
                        rS = Ring(banks[0:4])
                        rg = Ring(banks[4:6])
                        q1r = Ring([sb(f"q1r{ps}{sub}{i}", [128, 8, 128], BF16, sg_) for i in range(3)])
                        q2r = Ring([sb(f"q2r{ps}{sub}{i}", [128, 8, 128], BF16, sg_) for i in range(3)])
                        DEPTH = 3
                        Sof = {}
                        qrep = {}

                        def issueS(t):
                            if t % 8 == 0:
                                q1t = q1r.next()
                                q2t = q2r.next()
                                for hf_, qt_ in ((0, q1t), (1, q2t)):
                                    src = qp4[:, :, hf_, t:t + 8].rearrange("p h t -> p t h").unsqueeze(3).to_broadcast([128, 8, 8, 16])
                                    eng_ = ("pool", "dve", "pool", "act")[(t // 8 * 2 + hf_) % 4]
                                    cp(eng_, qt_[:].rearrange("p t (h a) -> p t h a", a=16), src, [qpT], [qt_])
                                qrep[t // 8] = (q1t, q2t)
                            q1t, q2t = qrep[t // 8]
                            bS = rS.next()
                            mm(bS[:, 0:NI], q1t[:, t % 8, :], k1b[:, i0:i0 + NI], True, True, [q1t, k1b], [bS])
                            mm(bS[:, 128:256], q2t[:, t % 8, :], k2b[:], True, True, [q2t, k2b], [bS])
                            Sof[t] = bS

                        for t in range(DEPTH):
                            issueS(t)
                        for t in range(TP):
                            if t + DEPTH < TP:
                                issueS(t + DEPTH)
                            bS = Sof.pop(t)
                            Pt = Ptr.next()
                            E = Er.next()
                            Qt = Qr.next()
                            act(E[:], bS[:, 128:256], AF.Exp, [bS, statT], [E], bias=statT[:, 1, t:t + 1])
                            ts("dve", Pt[:], bS[:, 0:NI], statT[:, 0, t:t + 1], statT[:, 3, t:t + 1], ALU.is_equal, ALU.mult,
                               [bS, statT], [Pt])
                            stt(Qt[:], bS[:, 128:256], statT[:, 2, t:t + 1], E[:], ALU.is_ge, ALU.mult, [bS, statT, E], [Qt])
                            if t % 4 == 0:
                                bg = rg.next()
                            mm(bg[:, (t % 4) * NI:(t % 4 + 1) * NI], Qt[:], Pt[:], True, True, [Qt, Pt], [bg])
                            if t % 4 == 3:
                                cp("act", GT[:, :, t - 3:t + 1].rearrange("p i t -> p t i"),
                                   bg[:, 0:4 * NI].rearrange("p (t i) -> p t i", t=4), [bg], [Acc(GT)])
                        UTr = Ring([sb(f"UT{ps}{sub}{i}", [128, 16, 256], BF16, sg_) for i in range(4)])
                        glr = Ring([sb(f"gl{ps}{sub}{i}", [128, TP], BF16, sg_) for i in range(2)])
                        zr = Ring(banks[6:8])
                        for ig in range(NI // 2):
                            UT = UTr.next()
                            dma("pool", UT[:], uT3[:, :, (i0 + ig * 2) * 128:(i0 + ig * 2 + 2) * 128], [], [UT])
                            for il in range(2):
                                i = ig * 2 + il
                                bz = zr.next()
                                for k in range(16):
                                    mm(bz[:], UT[:, k, il * 128:(il + 1) * 128], hnT[:, k, :], k == 0, k == 15, [UT, hnT], [bz])
                                gl = glr.next()
                                act(gl[:], bz[:], AF.Gelu, [bz], [gl])
                                tt("dve", GT[:, i, :], GT[:, i, :], gl[:], ALU.mult, [GT, gl], [GT])
                        Vr = Ring([sb(f"Vg{ps}{sub}{i}", [128, 4, 512], BF16, sg_) for i in range(4)])
                        acc = banks[0:4]
                        for dc in range(4):
                            for ig in range(NI // 4):
                                Vg = Vr.next()
                                dma("pool", Vg[:], v3[:, i0 + ig * 4:i0 + ig * 4 + 4, dc * 512:(dc + 1) * 512], [], [Vg])
                                for il in range(4):
                                    i = ig * 4 + il
                                    for tq in range(4):
                                        mm(acc[tq][:], GT[:, i, tq * 128:(tq + 1) * 128], Vg[:, il, :], i == 0, i == NI - 1,
                                           [GT, Vg], [acc[tq]])
                            for tq in range(4):
                                tt("dve", xacc[:, tq, dc * 512:(dc + 1) * 512], acc[tq][:], xacc[:, tq, dc * 512:(dc + 1) * 512],
                                   ALU.add, [acc[tq], xacc], [xacc])
                        P.barrier()
                for jl in range(4):
                    dma("sp", g.xres[(ps * 4 + jl) * 128:(ps * 4 + jl + 1) * 128, :], xacc[:, jl, :], [xacc], [], wacc=[g.xres_t])
                P.barrier()
    g.xres_dep = [g.xres_t]
```

```python
import numpy as np
from contextlib import ExitStack
import concourse.bass as bass
import concourse.mybir as mybir
from concourse.bass_utils import run_bass_kernel_spmd

F32 = mybir.dt.float32
BF16 = mybir.dt.bfloat16
I32 = mybir.dt.int32
AF = mybir.ActivationFunctionType
ALU = mybir.AluOpType
AX = mybir.AxisListType

ENGS = ("pe", "act", "dve", "pool", "sp")
NCORES = 8
D = 2048
KD = 16
TOWN = 1024
EPS = 1e-6
PI = float(np.pi)


class Buf:
    __slots__ = ("name", "w", "r", "excl")

    def __init__(self, name, excl=False):
        self.name = name
        self.w = {}
        self.r = {}
        self.excl = excl


class Prog:
    SAME_ENG_SYNC = True

    def __init__(self, nc, es, dma_slots=8):
        self.nc = nc
        self.es = es
        self.q = {e: [] for e in ENGS}
        self.cnt = {e: 0 for e in ENGS}
        self.sem = {e: es.enter_context(nc.semaphore("s_" + e)) for e in ENGS}
        self.K = dma_slots
        self.dsem = {}
        self.dcnt = {}
        for e in ("sp", "pool", "act"):
            self.dsem[e] = [es.enter_context(nc.semaphore(f"d_{e}{k}")) for k in range(dma_slots)]
            self.dcnt[e] = 0
        self.seen = {e: {} for e in ENGS}
        self.nbuf = 0

    def buf(self, name=None, excl=False):
        self.nbuf += 1
        return Buf(name or f"b{self.nbuf}", excl)

    def _deps(self, eng, r, w, wacc=()):
        deps = {}

        def add(k, v):
            if deps.get(k, 0) < v:
                deps[k] = v

        for b in r:
            for k, v in b.w.items():
                add(k, v)
            if b.excl:
                for k, v in b.r.items():
                    if k != eng:
                        add(k, v)
        for b in w:
            for k, v in b.w.items():
                add(k, v)
            for k, v in b.r.items():
                add(k, v)
        for b in wacc:
            for k, v in b.r.items():
                add(k, v)
        waits = []
        seen = self.seen[eng]
        for k, v in deps.items():
            if k == eng and (eng == "pe" or not self.SAME_ENG_SYNC):
                continue
            if seen.get(k, 0) >= v:
                continue
            seen[k] = v
            waits.append((k, v))
        return waits

    def _mark(self, ev, r, w, wacc=()):
        k, v = ev
        for b in r:
            if b.r.get(k, 0) < v:
                b.r[k] = v
        for b in w:
            b.w = {k: v}
            b.r = {}
        for b in wacc:
            if b.w.get(k, 0) < v:
                b.w[k] = v

    def op(self, eng, fn, r=(), w=(), wacc=()):
        waits = self._deps(eng, r, w, wacc)
        self.cnt[eng] += 1
        ev = (eng, self.cnt[eng])
        self.q[eng].append((waits, fn, self.sem[eng], 1))
        self._mark(ev, r, w, wacc)
        return ev

    def dma(self, eng, out, in_, r=(), w=(), wacc=(), **kw):
        waits = self._deps(eng, r, w, wacc)
        j = self.dcnt[eng]
        self.dcnt[eng] = j + 1
        k = j % self.K
        val = 16 * (j // self.K + 1)
        key = ("d", eng, k)
        if j >= self.K:
            pv = val - 16
            if self.seen[eng].get(key, 0) < pv:
                self.seen[eng][key] = pv
                waits.append((key, pv))
        self.q[eng].append((waits, lambda e: e.dma_start(out=out, in_=in_, **kw), self.dsem[eng][k], 16))
        ev = (key, val)
        self._mark(ev, r, w, wacc)
        return ev

    def barrier(self):
        targets = [(e, self.cnt[e]) for e in ENGS if self.cnt[e] > 0]
        for eng in ("sp", "pool", "act"):
            j = self.dcnt[eng]
            for k in range(self.K):
                n = (j - 1 - k) // self.K + 1 if j > k else 0
                if n > 0:
                    targets.append((("d", eng, k), 16 * n))
        for e in ENGS:
            waits = []
            for k, v in targets:
                if k == e:
                    continue
                if self.seen[e].get(k, 0) >= v:
                    continue
                self.seen[e][k] = v
                waits.append((k, v))
            if waits:
                self.q[e].append((waits, None, None, 0))

    def _semh(self, k):
        if isinstance(k, tuple):
            return self.dsem[k[1]][k[2]]
        return self.sem[k]

    def emit(self):
        nc = self.nc
        with nc.Block() as block:
            def mk(ename):
                def body(e):
                    for waits, fn, sem, inc in self.q[ename]:
                        for k, v in waits:
                            e.wait_ge(self._semh(k), v)
                        if fn is not None:
                            ins = fn(e)
                            ins.then_inc(sem, inc)
                return body

            block.tensor(mk("pe"))
            block.scalar(mk("act"))
            block.vector(mk("dve"))
            block.gpsimd(mk("pool"))
            block.sync(mk("sp"))


class T:
    __slots__ = ("t", "b")

    def __init__(self, t, b):
        self.t = t
        self.b = b

    def __getitem__(self, idx):
        return self.t[idx]


class NS:
    pass


class Acc:
    __slots__ = ("t",)

    def __init__(self, t):
        self.t = t


class Ring:
    def __init__(self, tiles):
        self.tiles = tiles
        self.i = 0

    def next(self):
        t = self.tiles[self.i % len(self.tiles)]
        self.i += 1
        return t


def host_consts():
    c = {}
    c["c_ident"] = np.eye(128, dtype=np.float32)
    s = np.arange(128)[:, None]
    t = np.arange(128)[None, :]
    same = (s // 64) == (t // 64)
    U = (same & (s <= t)).astype(np.float32)
    R = (same & ((s % 64) <= 31)).astype(np.float32)
    L = same.astype(np.float32)
    c["c_tri"] = np.ascontiguousarray(np.stack([U, U - R, L - U], axis=1))
    c["c_causal"] = (s <= t).astype(np.float32)
    ind = np.zeros((128, 2), np.float32)
    ind[:64, 0] = 1.0
    ind[64:, 1] = 1.0
    c["c_ind"] = ind
    half = 32
    freqs = (10000.0 ** (-np.arange(half, dtype=np.float32) / half)).astype(np.float32)
    c["c_freq"] = np.ascontiguousarray(np.tile(freqs[None, :], (128, 2)))
    ph = np.concatenate([np.full(32, PI / 2), np.zeros(32)]).astype(np.float32)
    c["c_phase"] = np.ascontiguousarray(np.tile(ph[None, :], (128, 1)))
    return c


def build(NPREV=7, taps=(), stop_after=None, lite=False):
    NTP = NPREV * 8
    NT = NTP + 8
    nc = bass.Bass("TRN2", target_bir_lowering=False)

    def din(name, shape, dt=F32):
        return nc.dram_tensor(name, list(shape), dt, kind="ExternalInput").ap()

    xprev = din("xprev", [max(NPREV, 1) * 1024, D])
    xown = din("xown", [1024, D])
    posT = din("posT", [128, NT], I32)
    kvalid = din("kvalid", [128, NT])
    p_own = din("p_own", [1024, 256])
    w_in = din("w_in", [D, 9280])
    gT = {n: din(n, [128, 16]) for n in ("g_mix", "g_ffn", "g_ple")}
    g_final = din("g_final", [1, D])
    lbT = din("lbT", [128, 16])
    lb_logits = din("lb_logits", [2, 1024])
    g_hg = din("g_hg", [128, 8])
    g_q = din("g_q", [128, 4])
    g_kv = din("g_kv", [128, 4])
    w_uq = din("w_uq", [512, 1536])
    w_ukv = din("w_ukv", [512, 2048])
    w_a = din("w_a", [1024, D])
    w_b = din("w_b", [1024, D])
    w_o = din("w_o", [D, D])
    peer_wq = din("peer_wq", [D, D])
    k1T = din("k1T", [128, 128])
    k2T = din("k2T", [128, 128])
    peer_uT = din("peer_uT", [D, 128 if lite else 16384])
    peer_v = din("peer_v", [128 if lite else 16384, D])
    w_pg = din("w_pg", [D, D])
    w_pe = din("w_pe", [256, D])
    consts = {k: din(k, v.shape) for k, v in host_consts().items()}
    out = nc.dram_tensor("out", [1024, D], F32, kind="ExternalOutput").ap()
    tap_out = {}
    for name, shape in taps:
        tap_out[name] = nc.dram_tensor("tap_" + name, list(shape), F32, kind="ExternalOutput").ap()
    KT_s = nc.dram_tensor("KT_s", [128, NT, 8, 128], BF16).ap()
    KR_s = nc.dram_tensor("KR_s", [64, NT * 128], BF16).ap()
    V_s = nc.dram_tensor("V_s", [NT, 128, 8, 128], BF16).ap()
    xres = nc.dram_tensor("xres", [1024, D], F32).ap()

    with ExitStack() as es:
        P = Prog(nc, es)

        def sb(name, shape, dt, st=None):
            t = (st or es).enter_context(nc.sbuf_tensor(name, list(shape), dt))
            return T(t, P.buf(name))

        banks = [T(es.enter_context(nc.psum_tensor(f"bank{i}", [128, 512], F32)), P.buf(f"bank{i}", excl=True)) for i in range(8)]
        bring = Ring(banks)

        def B(l):
            return [x.b for x in l]

        def BW(l):
            return [x.b for x in l if not isinstance(x, Acc)], [x.t.b for x in l if isinstance(x, Acc)]

        def mm(o, lhsT, rhs, st, sp, r, w):
            P.op("pe", lambda e: e.matmul(o, lhsT=lhsT, rhs=rhs, start=st, stop=sp), B(r), *BW(w))

        def tr(o, i, ident, r, w):
            P.op("pe", lambda e: e.transpose(out=o, in_=i, identity=ident), B(r), *BW(w))

        def act(o, i, func, r, w, bias=None, scale=None, accum=None):
            kw = {}
            if bias is not None:
                kw["bias"] = bias
            if scale is not None:
                kw["scale"] = scale
            if accum is not None:
                kw["accum_out"] = accum
            P.op("act", lambda e: e.activation(out=o, in_=i, func=func, **kw), B(r), *BW(w))

        def ts(eng, o, i, s1, s2, op0, op1, r, w):
            if op1 is None:
                P.op(eng, lambda e: e.tensor_scalar(out=o, in0=i, scalar1=s1, scalar2=None, op0=op0), B(r), *BW(w))
            else:
                P.op(eng, lambda e: e.tensor_scalar(out=o, in0=i, scalar1=s1, scalar2=s2, op0=op0, op1=op1), B(r), *BW(w))

        def tt(eng, o, a, b, op, r, w):
            P.op(eng, lambda e: e.tensor_tensor(out=o, in0=a, in1=b, op=op), B(r), *BW(w))

        def stt(o, a, s, b, op0, op1, r, w):
            P.op("dve", lambda e: e.scalar_tensor_tensor(out=o, in0=a, scalar=s, in1=b, op0=op0, op1=op1), B(r), *BW(w))

        def cp(eng, o, i, r, w):
            if eng == "act":
                P.op("act", lambda e: e.copy(out=o, in_=i), B(r), *BW(w))
            else:
                P.op(eng, lambda e: e.tensor_copy(out=o, in_=i), B(r), *BW(w))

        wc_state = [0]

        def wcast(o, i, gain, r, w, engines=("act", "dve", "act", "dve", "pool")):
            eng = engines[wc_state[0] % len(engines)]
            wc_state[0] += 1
            if eng == "act":
                if gain is None:
                    cp("act", o, i, r, w)
                else:
                    act(o, i, AF.Copy, r, w, scale=gain)
            else:
                if gain is None:
                    cp(eng, o, i, r, w)
                else:
                    ts(eng, o, i, gain, None, ALU.mult, None, r, w)

        def dma(eng, o, i, r, w, wacc=(), **kw):
            P.dma(eng, o, i, B(r), B(w), B(wacc), **kw)

        def tap(name, src_ap, src_t, dst_slice=None):
            if name in tap_out:
                dst = tap_out[name] if dst_slice is None else dst_slice(tap_out[name])
                dma("pool", dst, src_ap, [src_t], [], max_dma_last_dim=2048)

        def rstd_from_ss(ss, n, tmp, rs, st_r):
            ts("dve", tmp[:], ss[:], 1.0 / n, EPS, ALU.mult, ALU.add, [ss], [tmp])
            act(tmp[:], tmp[:], AF.Sqrt, [tmp], [tmp])
            P.op("dve", lambda e: e.reciprocal(out=rs[:], in_=tmp[:]), B([tmp]), B([rs]))

        identf = sb("identf", [128, 128], F32)
        identb = sb("identb", [128, 128], BF16)
        tri = sb("tri", [128, 3, 128], F32)
        causal = sb("causal", [128, 128], F32)
        ind = sb("ind", [128, 2], F32)
        kval = sb("kval", [128, NT], F32)
        gt = {n: sb("sb_" + n, [128, 16], F32) for n in gT}
        lbc = sb("lbc", [128, 16], F32)
        ghg = sb("ghg", [128, 8], F32)
        gq = sb("gq", [128, 4], F32)
        gkv = sb("gkv", [128, 4], F32)
        dma("sp", identf[:], consts["c_ident"], [], [identf])
        dma("sp", tri[:], consts["c_tri"], [], [tri])
        dma("sp", causal[:], consts["c_causal"], [], [causal])
        dma("sp", ind[:], consts["c_ind"], [], [ind])
        dma("sp", kval[:], kvalid, [], [kval])
        for n in gT:
            dma("sp", gt[n][:], gT[n], [], [gt[n]])
        dma("sp", lbc[:], lbT, [], [lbc])
        dma("sp", ghg[:], g_hg, [], [ghg])
        dma("sp", gq[:], g_q, [], [gq])
        dma("sp", gkv[:], g_kv, [], [gkv])
        cp("dve", identb[:], identf[:], [identf], [identb])
        Utri = tri[:, 0, :]
        M1 = tri[:, 1, :]
        M2 = tri[:, 2, :]

        s1 = es.enter_context(ExitStack())
        tbl = sb("tbl", [128, NT, 64], F32, s1)
        with ExitStack() as s0:
            posi = sb("posi", [128, NT], I32, s0)
            posf = sb("posf", [128, NT], F32, s0)
            frq = sb("frq", [128, 64], F32, s0)
            phs = sb("phs", [128, 64], F32, s0)
            kk = sb("kk", [128, NT, 64], F32, s0)
            ki = sb("ki", [128, NT, 64], I32, s0)
            dma("sp", posi[:], posT, [], [posi])
            dma("sp", frq[:], consts["c_freq"], [], [frq])
            dma("sp", phs[:], consts["c_phase"], [], [phs])
            cp("dve", posf[:], posi[:], [posi], [posf])
            tt("dve", tbl[:], posf[:].unsqueeze(2).to_broadcast([128, NT, 64]),
               frq[:].unsqueeze(1).to_broadcast([128, NT, 64]), ALU.mult, [posf, frq], [tbl])
            tt("dve", tbl[:], tbl[:], phs[:].unsqueeze(1).to_broadcast([128, NT, 64]), ALU.add, [tbl, phs], [tbl])
            ts("dve", kk[:], tbl[:], 1.0 / (2 * PI), None, ALU.mult, None, [tbl], [kk])
            cp("dve", ki[:], kk[:], [kk], [ki])
            cp("dve", kk[:], ki[:], [ki], [kk])
            C1 = 6.28125
            C2 = float(2 * np.pi - 6.28125)
            stt(tbl[:], kk[:], -C1, tbl[:], ALU.mult, ALU.add, [kk, tbl], [tbl])
            stt(tbl[:], kk[:], -C2, tbl[:], ALU.mult, ALU.add, [kk, tbl], [tbl])
            ts("dve", kk[:], tbl[:], PI, -2 * PI, ALU.is_gt, ALU.mult, [tbl], [kk])
            tt("dve", tbl[:], tbl[:], kk[:], ALU.add, [tbl, kk], [tbl])
            ts("dve", kk[:], tbl[:], -PI, 2 * PI, ALU.is_lt, ALU.mult, [tbl], [kk])
            tt("dve", tbl[:], tbl[:], kk[:], ALU.add, [tbl, kk], [tbl])
            ts("dve", tbl[:], tbl[:], PI, -PI, ALU.min, ALU.max, [tbl], [tbl])
            act(tbl[:], tbl[:], AF.Sin, [tbl], [tbl])
            P.barrier()
        tap("tbl", tbl[:, NT - 1, :], tbl)

        lbrow = sb("lbrow", [128, 1024], F32, s1)
        omlrow = sb("omlrow", [128, 1024], F32, s1)
        with ExitStack() as s0:
            l1 = sb("l1", [128, 1024], F32, s0)
            dma("sp", lbrow[:], lb_logits[0:1, :].to_broadcast([128, 1024]), [], [lbrow])
            dma("sp", l1[:], lb_logits[1:2, :].to_broadcast([128, 1024]), [], [l1])
            tt("dve", lbrow[:], lbrow[:], l1[:], ALU.subtract, [lbrow, l1], [lbrow])
            act(lbrow[:], lbrow[:], AF.Sigmoid, [lbrow], [lbrow])
            ts("dve", omlrow[:], lbrow[:], -1.0, 1.0, ALU.mult, ALU.add, [lbrow], [omlrow])
            P.barrier()

        g = NS()
        g.__dict__.update(locals())
        if stop_after != "p0":
            phase1(g)
        sO = es.enter_context(ExitStack())
        g.oaT = oaT = sb("oaT", [128, 8, 1024], BF16, sO)
        g.obT = obT = sb("obT", [128, 8, 1024], BF16, sO)
        g.xres_t = T(None, P.buf("xres"))
        g.xres_dep = []
        early = stop_after is not None and (stop_after in ("p0", "p1a") or stop_after.startswith("x"))
        if not early:
            phase1b(g)
        if not early and stop_after != "p1b":
            phase1c(g)
        if not early and stop_after not in ("p1b", "p1c"):
            phase2(g)
        P.barrier()
        sO.close()
        s1.close()
        if not early and stop_after not in ("p1b", "p1c", "p2"):
            if stop_after != "nopeer":
                phase3(g)
            phase4(g)
        P.barrier()
        P.emit()
    return nc


def phase1(g):
    P, sb, mm, tr, act, ts, tt, stt, cp, dma, tap = g.P, g.sb, g.mm, g.tr, g.act, g.ts, g.tt, g.stt, g.cp, g.dma, g.tap
    NT, NTP, bring, s1 = g.NT, g.NTP, g.bring, g.s1
    identb, identf, tbl, lbrow, omlrow, ind, kval = g.identb, g.identf, g.tbl, g.lbrow, g.omlrow, g.ind, g.kval
    Utri, M1, M2, tri = g.Utri, g.M1, g.M2, g.tri
    w_in = g.w_in
    B = g.B

    es = g.es
    S = sb("S", [128, 8, 128], F32, s1)
    kmax2 = sb("kmax2", [128, 8], F32, s1)
    krmax2 = sb("krmax2", [128, 1], F32, s1)
    g.S, g.kmax2, g.krmax2 = S, kmax2, krmax2
    Sh = [T(S.t, P.buf(f"S1a_h{h}")) for h in range(8)]
    g.Sh1a = Sh
    P.op("dve", lambda e: e.memset(S[:], 0.0), [], B([S] + Sh))
    P.op("dve", lambda e: e.memset(kmax2[:], 0.0), [], B([kmax2]))
    P.op("dve", lambda e: e.memset(krmax2[:], 0.0), [], B([krmax2]))
    kts = T(None, P.buf("KT_s"))
    krs = T(None, P.buf("KR_s"))
    vs = T(None, P.buf("V_s"))
    g.kts, g.krs, g.vs = kts, krs, vs

    with ExitStack() as sa:
        wA = sb("wA", [128, 16, 2048], BF16, sa)
        wB = sb("wB", [128, 16, 576], BF16, sa)
        wkv = sb("wkv", [128, 4, 2048], BF16, sa)
        gmix = g.gt["g_mix"]
        with ExitStack() as sl:
            stg = Ring([sb(f"stg{i}", [128, 2048], F32, sl) for i in range(2)])
            for k in range(16):
                s = stg.next()
                dma("sp", s[:, :], w_in[k * 128:(k + 1) * 128, 1024:3072], [], [s])
                g.wcast(wA[:, k, :], s[:, :], gmix[:, k:k + 1], [s, gmix], [wA])
                s = stg.next()
                dma("sp", s[:, 0:576], w_in[k * 128:(k + 1) * 128, 4608:5184], [], [s])
                g.wcast(wB[:, k, :], s[:, 0:576], gmix[:, k:k + 1], [s, gmix], [wB])
            for c in range(4):
                s = stg.next()
                dma("sp", s[:, :], g.w_ukv[c * 128:(c + 1) * 128, :], [], [s])
                g.wcast(wkv[:, c, :], s[:, :], g.gkv[:, c:c + 1], [s, g.gkv], [wkv])
            P.barrier()

        xring = Ring([sb(f"xt{i}", [128, 2048], F32, sa) for i in range(2)])
        xnring = Ring([sb(f"xn{i}", [128, 2048], BF16, sa) for i in range(2)])
        hTring = Ring([sb(f"hT{i}", [128, 16, 128], BF16, sa) for i in range(2)])
        ss = sb("ss", [128, 4], F32, sa)
        tmp = sb("tmp", [128, 4], F32, sa)
        rs = sb("rs", [128, 4], F32, sa)
        fsb = sb("fsb", [128, 1024], F32, sa)
        ksb = sb("ksb", [128, 1024], F32, sa)
        vsb = sb("vsb", [128, 1024], BF16, sa)
        e2 = sb("e2", [128, 1024], F32, sa)
        kd = [sb(f"kd{j}", [128, 1024], BF16, sa) for j in range(2)]
        dec = sb("dec", [128, 8, 2], F32, sa)
        junk = sb("junk", [128, 512], BF16, sa)
        ckvn = sb("ckvn", [128, 512], BF16, sa)
        ckvT = sb("ckvT", [128, 4, 128], BF16, sa)
        knT = Ring([sb(f"knT{i}", [128, 8, 128], BF16, sa) for i in range(2)])
        Vt = Ring([sb(f"Vt{i}", [128, 8, 128], BF16, sa) for i in range(2)])
        sq = sb("sq", [128, 8, 128], F32, sa)
        knb = sb("knb", [128, 8, 128], BF16, sa)
        kn2 = sb("kn2", [128, 8], F32, sa)
        ra = sb("ra", [128, 64], F32, sa)
        rb = sb("rb", [128, 64], F32, sa)
        krp = sb("krp", [128, 64], BF16, sa)
        mkr = sb("mkr", [128, 64], F32, sa)
        krn = sb("krn", [128, 1], F32, sa)
        krT = Ring([sb(f"krT{i}", [64, 128], BF16, sa) for i in range(2)])

        sa_flags = g.stop_after or ""
        if sa_flags == "xw":
            return
        def tile_body(t):
            own = t >= NTP
            xt = xring.next()
            src = g.xown[(t - NTP) * 128:(t - NTP + 1) * 128, :] if own else g.xprev[t * 128:(t + 1) * 128, :]
            dma("sp", xt[:], src, [], [xt])
            xn = xnring.next()
            act(xn[:], xt[:], AF.Square, [xt], [xn, ss], accum=ss[:, 0:1])
            g.rstd_from_ss(T(ss.t[:, 0:1], ss.b), D, T(tmp.t[:, 0:1], tmp.b), T(rs.t[:, 0:1], rs.b), None)
            act(xn[:], xt[:], AF.Copy, [xt, rs], [xn], scale=rs[:, 0:1])
            hT = hTring.next()
            for half in range(2):
                bk = bring.next()
                bv = bk.t[:].bitcast(BF16)
                for kk in range(8):
                    k = half * 8 + kk
                    tr(bv[:, kk * 128:(kk + 1) * 128], xn[:, k * 128:(k + 1) * 128], identb[:], [xn, identb], [bk])
                cp("dve" if half == 0 else "act", hT[:, half * 8:(half + 1) * 8, :],
                   bv.rearrange("p (k t) -> p k t", k=8), [bk], [Acc(hT)])
            yield
            if not own:
                bA = [bring.next() for _ in range(4)]
                for n in range(4):
                    for k in range(16):
                        mm(bA[n][:], hT[:, k, :], wA[:, k, n * 512:(n + 1) * 512], k == 0, k == 15, [hT, wA], [bA[n]])
            bkv = bring.next()
            bkr = bring.next()
            for k in range(16):
                mm(bkv[:], hT[:, k, :], wB[:, k, 0:512], k == 0, k == 15, [hT, wB], [bkv])
            for k in range(16):
                mm(bkr[:, 0:64], hT[:, k, :], wB[:, k, 512:576], k == 0, k == 15, [hT, wB], [bkr])
            act(junk[:], bkv[:], AF.Square, [bkv], [junk, ss], accum=ss[:, 1:2])
            g.rstd_from_ss(T(ss.t[:, 1:2], ss.b), 512, T(tmp.t[:, 1:2], tmp.b), T(rs.t[:, 1:2], rs.b), None)
            act(ckvn[:], bkv[:], AF.Copy, [bkv, rs], [ckvn], scale=rs[:, 1:2])
            cp("dve", mkr[:], bkr[:, 0:64], [bkr], [mkr])
            if sa_flags == "xproj":
                return
            if not own:
                for n in range(2):
                    act(fsb[:, n * 512:(n + 1) * 512], bA[n][:], AF.Sigmoid, [bA[n]], [Acc(fsb)])
                for n in range(2):
                    cp("act", vsb[:, n * 512:(n + 1) * 512], bA[2 + n][:], [bA[2 + n]], [Acc(vsb)])
                tt("dve", fsb[:], fsb[:], omlrow[:], ALU.mult, [fsb, omlrow], [fsb])
                tt("dve", fsb[:], fsb[:], lbrow[:], ALU.add, [fsb, lbrow], [fsb])
                ts("dve", ksb[:], fsb[:], -1.0, 1.0, ALU.mult, ALU.add, [fsb], [ksb])
                act(fsb[:], fsb[:], AF.Ln, [fsb], [fsb])
            if sa_flags == "xhg":
                return
            bk = bring.next()
            bv = bk.t[:].bitcast(BF16)
            for c in range(4):
                tr(bv[:, c * 128:(c + 1) * 128], ckvn[:, c * 128:(c + 1) * 128], identb[:], [ckvn, identb], [bk])
            cp("dve", ckvT[:], bv[:, 0:512].rearrange("p (c t) -> p c t", c=4), [bk], [ckvT])
            if sa_flags == "xkv1":
                return
            wkv4 = wkv.t[:].rearrange("p c (h two d) -> p c h two d", h=8, two=2)
            btk = [bring.next() for _ in range(4)]
            for n in range(4):
                for c in range(4):
                    mm(btk[n][:], ckvT[:, c, :], wkv[:, c, n * 512:(n + 1) * 512], c == 0, c == 3, [ckvT, wkv], [btk[n]])
            if sa_flags == "xkv3a":
                return
            vt = Vt.next()
            for n in range(4):
                bview = btk[n][:].rearrange("p (h two d) -> p h two d", h=2, two=2)
                cp("dve" if n % 2 == 0 else "act", vt[:, 2 * n:2 * n + 2, :], bview[:, :, 1, :], [btk[n]], [Acc(vt)])
                cp("act" if n % 2 == 0 else "dve", knb[:, 2 * n:2 * n + 2, :], bview[:, :, 0, :], [btk[n]], [Acc(knb)])
            bkn = bring.next()
            bknv = bkn.t[:].bitcast(BF16)
            for h in range(8):
                tr(bknv[:, h * 128:(h + 1) * 128], knb[:, h, :], identb[:], [knb, identb], [bkn])
            kn = knT.next()
            cp("act", kn[:], bknv.rearrange("p (h t) -> p h t", h=8), [bkn], [kn])
            dma("pool", g.KT_s[:, t, :, :], kn[:], [kn], [], wacc=[kts])
            if sa_flags in ("xkv3b", "xkv3c"):
                return
            dma("pool", g.V_s[t, :, :, :], vt[:], [vt], [], wacc=[vs])
            if sa_flags == "xkv3":
                return
            tt("dve", sq[:], knb[:], knb[:], ALU.mult, [knb], [sq])
            if sa_flags == "xkv3d":
                return
            P.op("dve", lambda e: e.tensor_reduce(out=kn2[:], in_=sq[:], axis=AX.X, op=ALU.add), B([sq]), B([kn2]))
            tt("dve", kmax2[:], kmax2[:], kn2[:], ALU.max, [kmax2, kn2], [kmax2])
            cs = tbl[:, t, 0:32]
            sn = tbl[:, t, 32:64]
            tt("dve", ra[:].rearrange("p (two d) -> p two d", two=2), mkr[:].rearrange("p (two d) -> p two d", two=2),
               cs.unsqueeze(1).to_broadcast([128, 2, 32]), ALU.mult, [mkr, tbl], [ra])
            tt("dve", rb[:, 0:32], mkr[:, 32:64], sn, ALU.mult, [mkr, tbl], [Acc(rb)])
            tt("dve", rb[:, 32:64], mkr[:, 0:32], sn, ALU.mult, [mkr, tbl], [Acc(rb)])
            tt("dve", krp[:, 0:32], ra[:, 0:32], rb[:, 0:32], ALU.subtract, [ra, rb], [Acc(krp)])
            tt("dve", krp[:, 32:64], ra[:, 32:64], rb[:, 32:64], ALU.add, [ra, rb], [Acc(krp)])
            act(ra[:], krp[:], AF.Square, [krp], [ra, krn], accum=krn[:])
            tt("dve", krmax2[:], krmax2[:], krn[:], ALU.max, [krmax2, krn], [krmax2])
            if sa_flags == "xkv4":
                return
            bk = bring.next()
            bv = bk.t[:].bitcast(BF16)
            tr(bv[0:64, 0:128], krp[:, :], identb[:], [krp, identb], [bk])
            kr = krT.next()
            cp("act", kr[:], bv[0:64, 0:128], [bk], [kr])
            dma("pool", g.KR_s[:, t * 128:(t + 1) * 128], kr[:], [kr], [], wacc=[krs])
            if not own:
                bd = [bring.next() for _ in range(2)]
                for n in range(2):
                    mm(bd[n][:], M2, fsb[:, n * 512:(n + 1) * 512], True, True, [tri, fsb], [bd[n]])
                    act(e2[:, n * 512:(n + 1) * 512], bd[n][:], AF.Exp, [bd[n]], [Acc(e2)])
                bl = bring.next()
                for h in range(8):
                    mm(bl[:, h * 2:h * 2 + 2], fsb[:, h * 128:(h + 1) * 128], ind[:], True, True, [fsb, ind], [bl])
                act(dec[:].rearrange("p h j -> p (h j)"), bl[:, 0:16], AF.Exp, [bl], [dec])
                for j in range(2):
                    stt(kd[j][:], ksb[:], ind[:, j:j + 1], e2[:], ALU.mult, ALU.mult, [ksb, ind, e2], [kd[j]])
                for j in range(2):
                    bs = [bring.next() for _ in range(2)]
                    for h in range(8):
                        mm(bs[h // 4][:, (h % 4) * 128:(h % 4 + 1) * 128], kd[j][:, h * 128:(h + 1) * 128],
                           vsb[:, h * 128:(h + 1) * 128], True, True, [kd[j], vsb], [bs[h // 4]])
                    for h in range(8):
                        stt(S[:, h, :], S[:, h, :], dec[:, h, j:j + 1], bs[h // 4][:, (h % 4) * 128:(h % 4 + 1) * 128],
                            ALU.mult, ALU.add, [Sh[h], dec, bs[h // 4]], [Sh[h]])
        tiles = list(range(NT)) if not sa_flags.startswith("x") else [0, NT - 1]
        gens = [tile_body(t) for t in tiles]
        next(gens[0])
        for i_, gen in enumerate(gens):
            if i_ + 1 < len(gens):
                next(gens[i_ + 1])
            for _ in gen:
                pass
        if "S" in g.tap_out:
            g.dma("pool", g.tap_out["S"], S[:].rearrange("p h d -> p (h d)"), Sh, [])
        tap("kmax2", kmax2[:], kmax2)
        P.barrier()


def phase1b(g):
    P, sb, mm, tr, act, ts, tt, stt, cp, dma, tap = g.P, g.sb, g.mm, g.tr, g.act, g.ts, g.tt, g.stt, g.cp, g.dma, g.tap
    NT, NTP, bring, banks = g.NT, g.NTP, g.bring, g.banks
    identb, identf, tbl, lbrow, omlrow, ind, kval, causal = g.identb, g.identf, g.tbl, g.lbrow, g.omlrow, g.ind, g.kval, g.causal
    Utri, M1, M2, tri = g.Utri, g.M1, g.M2, g.tri
    w_in, S, B = g.w_in, g.S, g.B
    gmix = g.gt["g_mix"]
    oaT, obT = g.oaT, g.obT

    with ExitStack() as sa:
        hTown = sb("hTown", [128, 16, 1024], BF16, sa)
        ss = sb("ss1", [128, 4], F32, sa)
        tmp = sb("tmp1", [128, 4], F32, sa)
        rs = sb("rs1", [128, 4], F32, sa)
        with ExitStack() as sl:
            xring = Ring([sb(f"xo{i}", [128, 2048], F32, sl) for i in range(2)])
            xnring = Ring([sb(f"xno{i}", [128, 2048], BF16, sl) for i in range(2)])
            for j in range(8):
                xt = xring.next()
                dma("sp", xt[:], g.xown[j * 128:(j + 1) * 128, :], [], [xt])
                xn = xnring.next()
                act(xn[:], xt[:], AF.Square, [xt], [xn, ss], accum=ss[:, 0:1])
                g.rstd_from_ss(T(ss.t[:, 0:1], ss.b), D, T(tmp.t[:, 0:1], tmp.b), T(rs.t[:, 0:1], rs.b), None)
                act(xn[:], xt[:], AF.Copy, [xt, rs], [xn], scale=rs[:, 0:1])
                for half in range(2):
                    bk = bring.next()
                    bv = bk.t[:].bitcast(BF16)
                    for kk in range(8):
                        k = half * 8 + kk
                        tr(bv[:, kk * 128:(kk + 1) * 128], xn[:, k * 128:(k + 1) * 128], identb[:], [xn, identb], [bk])
                    cp("dve" if half == 0 else "act", hTown[:, half * 8:(half + 1) * 8, j * 128:(j + 1) * 128],
                       bv.rearrange("p (k t) -> p k t", k=8), [bk], [hTown])
            P.barrier()

        with ExitStack() as sh:
            whr = Ring([sb(f"wh{i}", [128, 16, 512], BF16, sh) for i in range(4)])
            stg = Ring([sb(f"stgh{i}", [128, 4, 128], F32, sh) for i in range(3)])
            ND = 4

            def RN(name, shape, dt):
                return Ring([sb(f"{name}{i}", shape, dt, sh) for i in range(ND)])
            f_r, k_r, q_r = RN("f_", [128, 128], F32), RN("k_", [128, 128], F32), RN("q_", [128, 128], F32)
            v_r, gate_r = RN("v_", [128, 128], BF16), RN("gate", [128, 128], F32)
            e1_r, en1_r, eb_r, e2_r = RN("e1", [128, 128], F32), RN("en1", [128, 128], F32), RN("eb", [128, 128], F32), RN("e2b", [128, 128], F32)
            dec_r = RN("decb", [128, 2], F32)
            qin_r, kin_r, qbp_r = RN("qin", [128, 128], BF16), RN("kin", [128, 128], BF16), RN("qbp", [128, 192], BF16)
            kd_r = [RN(f"kdb{i}", [128, 128], BF16) for i in range(2)]
            atm_r, sb0_r, sb1_r = RN("atm", [128, 128], BF16), RN("sb0", [128, 128], BF16), RN("sb1", [128, 128], BF16)
            on_r, onb_r = RN("on", [128, 128], F32), RN("onb", [128, 128], BF16)
            ss_r, tmp_r, rs_r = RN("ssh", [128, 1], F32), RN("tmph", [128, 1], F32), RN("rsh", [128, 1], F32)
            for qb_ in qbp_r.tiles:
                P.op("dve", lambda e, qb_=qb_: e.memset(qb_[:], 0.0), [], B([qb_]))
            Sh = [T(S.t, P.buf(f"S_h{h}")) for h in range(8)]
            w4 = w_in[:, 0:4096].rearrange("p (s c) -> p s c", s=4)

            def load_head(h):
                wh = whr.next()
                for k in range(16):
                    s = stg.next()
                    dma("sp", s[:], w4[k * 128:(k + 1) * 128, :, h * 128:(h + 1) * 128], [], [s])
                    g.wcast(wh[:, k, :], s[:].rearrange("p s c -> p (s c)"), gmix[:, k:k + 1], [s, gmix], [wh],
                            engines=("act", "dve", "pool", "dve", "act"))
                return wh

            def body(h, j, wh):
                lb_h = lbrow[:, h * 128:(h + 1) * 128]
                oml_h = omlrow[:, h * 128:(h + 1) * 128]
                S_ = Sh[h]
                f_, k_, q_, v_, gate = f_r.next(), k_r.next(), q_r.next(), v_r.next(), gate_r.next()
                e1, en1, eb, e2, dec = e1_r.next(), en1_r.next(), eb_r.next(), e2_r.next(), dec_r.next()
                qin, kin, qbp, atm = qin_r.next(), kin_r.next(), qbp_r.next(), atm_r.next()
                kd = [kd_r[0].next(), kd_r[1].next()]
                sb0, sb1, on, onb = sb0_r.next(), sb1_r.next(), on_r.next(), onb_r.next()
                ss, tmp, rs = ss_r.next(), tmp_r.next(), rs_r.next()
                bp = bring.next()
                for k in range(16):
                    mm(bp[:], hTown[:, k, j * 128:(j + 1) * 128], wh[:, k, :], k == 0, k == 15, [hTown, wh], [bp])
                yield
                act(f_[:], bp[:, 128:256], AF.Sigmoid, [bp], [f_])
                act(gate[:], bp[:, 384:512], AF.Silu, [bp], [gate])
                cp("act", v_[:], bp[:, 256:384], [bp], [v_])
                cp("act", q_[:], bp[:, 0:128], [bp], [q_])
                tt("dve", f_[:], f_[:], oml_h, ALU.mult, [f_, omlrow], [f_])
                tt("dve", f_[:], f_[:], lb_h, ALU.add, [f_, lbrow], [f_])
                ts("dve", k_[:], f_[:], -1.0, 1.0, ALU.mult, ALU.add, [f_], [k_])
                act(f_[:], f_[:], AF.Ln, [f_], [f_])
                yield
                bt = bring.next()
                P.op("pe", lambda e: e.transpose(out=bt[:, 0:128], in_=q_[:], identity=identf[:]), B([q_, identf]), B([bt]))
                P.op("pe", lambda e: e.transpose(out=bt[:, 128:256], in_=k_[:], identity=identf[:]), B([k_, identf]), B([bt]))
                bc = bring.next()
                mm(bc[:, 0:128], f_[:], M1, True, True, [f_, tri], [bc])
                mm(bc[:, 128:256], f_[:], Utri, True, True, [f_, tri], [bc])
                mm(bc[:, 256:384], M2, f_[:], True, True, [f_, tri], [bc])
                mm(bc[:, 384:386], f_[:], ind[:], True, True, [f_, ind], [bc])
                yield
                act(e1[:], bc[:, 0:128], AF.Exp, [bc], [e1])
                act(en1[:], bc[:, 0:128], AF.Exp, [bc], [en1], scale=-1.0)
                act(eb[:], bc[:, 128:256], AF.Exp, [bc], [eb])
                act(e2[:], bc[:, 256:384], AF.Exp, [bc], [e2])
                act(dec[:], bc[:, 384:386], AF.Exp, [bc], [dec])
                tt("dve", qin[:], bt[:, 0:128], e1[:], ALU.mult, [bt, e1], [qin])
                tt("dve", kin[:], bt[:, 128:256], en1[:], ALU.mult, [bt, en1], [kin])
                tt("dve", qbp[:, 0:64], bt[:, 0:64], eb[:, 0:64], ALU.mult, [bt, eb], [qbp])
                tt("dve", qbp[:, 128:192], bt[:, 64:128], eb[:, 64:128], ALU.mult, [bt, eb], [qbp])
                for jj in range(2):
                    stt(kd[jj][:], k_[:], ind[:, jj:jj + 1], e2[:], ALU.mult, ALU.mult, [k_, ind, e2], [kd[jj]])
                yield
                ba = bring.next()
                mm(ba[:, 0:128], kin[:], qin[:], True, True, [kin, qin], [ba])
                bs = bring.next()
                cp("act", sb0[:], S[:, h, :], [S_], [sb0])
                mm(bs[:, 0:128], kd[0][:], v_[:], True, True, [kd[0], v_], [bs])
                mm(bs[:, 128:256], kd[1][:], v_[:], True, True, [kd[1], v_], [bs])
                yield
                tt("dve", atm[:], ba[:, 0:128], Utri, ALU.mult, [ba, tri], [atm])
                stt(S[:, h, :], S[:, h, :], dec[:, 0:1], bs[:, 0:128], ALU.mult, ALU.add, [S_, dec, bs], [S_])
                cp("act", sb1[:], S[:, h, :], [S_], [sb1])
                stt(S[:, h, :], S[:, h, :], dec[:, 1:2], bs[:, 128:256], ALU.mult, ALU.add, [S_, dec, bs], [S_])
                yield
                bo = bring.next()
                mm(bo[:, 0:128], atm[:], v_[:], True, False, [atm, v_], [bo])
                mm(bo[:, 0:128], qbp[:, 0:128], sb0[:], False, False, [qbp, sb0], [bo])
                mm(bo[:, 0:128], qbp[:, 64:192], sb1[:], False, True, [qbp, sb1], [bo])
                yield
                act(on[:], bo[:, 0:128], AF.Square, [bo], [on, ss], accum=ss[:, 0:1])
                g.rstd_from_ss(ss, 128, tmp, rs, None)
                act(on[:], bo[:, 0:128], AF.Copy, [bo, rs], [on], scale=rs[:, 0:1])
                tt("dve", onb[:], on[:], gate[:], ALU.mult, [on, gate], [onb])
                yield
                bz = bring.next()
                bzv = bz.t[:].bitcast(BF16)
                tr(bzv[:, 0:128], onb[:], identb[:], [onb, identb], [bz])
                ts("dve", oaT[:, h, j * 128:(j + 1) * 128], bzv[:, 0:128], g.ghg[:, h:h + 1], None, ALU.mult, None,
                   [bz, g.ghg], [oaT])

            for hp in range(2):
                hs = tuple(range(4 * hp, 4 * hp + 4))
                whs = [load_head(h) for h in hs]
                for j in range(8):
                    alive = [body(h, j, wh) for h, wh in zip(hs, whs)]
                    while alive:
                        for gen in list(alive):
                            try:
                                next(gen)
                            except StopIteration:
                                alive.remove(gen)
            P.barrier()
        tap("oaT", oaT[:].rearrange("p h t -> p (h t)"), oaT)


def phase1c(g):
    P, sb, mm, tr, act, ts, tt, stt, cp, dma, tap = g.P, g.sb, g.mm, g.tr, g.act, g.ts, g.tt, g.stt, g.cp, g.dma, g.tap
    NT, NTP, bring, banks = g.NT, g.NTP, g.bring, g.banks
    identb, identf, tbl, kval, causal = g.identb, g.identf, g.tbl, g.kval, g.causal
    w_in, B = g.w_in, g.B
    gmix = g.gt["g_mix"]
    obT = g.obT
    kts, krs, vs = g.kts, g.krs, g.vs
    SCALE = float(1.0 / np.sqrt(192.0))

    with ExitStack() as sa:
        qnT = sb("qnT", [128, 8, 1024], BF16, sa)
        qra = sb("qra", [65, 8, 1024], BF16, sa)
        kmb = sb("kmb", [128, 8], F32, sa)
        ss = sb("ss2", [128, 4], F32, sa)
        tmp = sb("tmp2", [128, 4], F32, sa)
        rs = sb("rs2", [128, 4], F32, sa)
        ones = sb("ones", [128, 128], BF16, sa)
        P.op("dve", lambda e: e.memset(ones[:], 1.0), [], B([ones]))
        with ExitStack() as sl:
            km = sb("km", [128, 128], F32, sl)
            P.op("dve", lambda e: e.memset(km[:], 0.0), [], B([km]))
            kmT = sb("kmT", [8, 128], F32, sl)
            kmc = sb("kmc", [8, 1], F32, sl)
            dg = sb("dg", [8, 8], F32, sl)
            onesf = sb("onesf", [8, 128], F32, sl)
            ts("dve", km[:, 0:8], g.kmax2[:], g.krmax2[:, 0:1], None, ALU.add, None, [g.kmax2, g.krmax2, km], [km])
            bk = bring.next()
            P.op("pe", lambda e, bk=bk: e.transpose(out=bk[:, 0:128], in_=km[:], identity=identf[:]), B([km, identf]), B([bk]))
            cp("dve", kmT[:], bk[0:8, 0:128], [bk], [kmT])
            tap("km", km[:, 0:8], km)
            tap("kmT", kmT[:], kmT)
            P.op("dve", lambda e: e.tensor_reduce(out=kmc[:], in_=kmT[:], axis=AX.X, op=ALU.max), B([kmT]), B([kmc]))
            act(kmc[:], kmc[:], AF.Sqrt, [kmc], [kmc])
            ts("dve", dg[:], identf[0:8, 0:8], kmc[:, 0:1], None, ALU.mult, None, [identf, kmc], [dg])
            P.op("dve", lambda e: e.memset(onesf[:], 1.0), [], B([onesf]))
            bk2 = bring.next()
            mm(bk2[:, 0:8], onesf[:], dg[:], True, True, [onesf, dg], [bk2])
            cp("dve", kmb[:], bk2[:, 0:8], [bk2], [kmb])
            P.barrier()
        tap("kmb", kmb[:], kmb)

        with ExitStack() as sq_:
            hTown = sb("hTown2", [128, 16, 128], BF16, sq_)
            xt = sb("xq", [128, 2048], F32, sq_)
            xn = sb("xnq", [128, 2048], BF16, sq_)
            wq = sb("wq", [128, 16, 512], BF16, sq_)
            wuq = sb("wuq", [128, 4, 1536], BF16, sq_)
            stg = Ring([sb(f"stgq{i}", [128, 1536], F32, sq_) for i in range(2)])
            cqn = sb("cqn", [128, 512], BF16, sq_)
            cqT = sb("cqT", [128, 4, 128], BF16, sq_)
            sqT = sb("sqT", [128, 8, 128], BF16, sq_)
            junk = sb("junkq", [128, 512], BF16, sq_)
            qr2 = sb("qr2", [128, 8, 64], F32, sq_)
            ra = sb("raq", [128, 8, 64], F32, sq_)
            rb = sb("rbq", [128, 8, 64], F32, sq_)
            qrp = sb("qrp", [128, 8, 64], BF16, sq_)
            qn2 = sb("qn2", [128, 8], F32, sq_)
            qn2b = sb("qn2b", [128, 128], F32, sq_)
            P.op("dve", lambda e: e.memset(qn2b[:], 0.0), [], B([qn2b]))
            shT = sb("shT", [8, 128], BF16, sq_)
            for k in range(16):
                s = stg.next()
                dma("sp", s[:, 0:512], w_in[k * 128:(k + 1) * 128, 4096:4608], [], [s])
                g.wcast(wq[:, k, :], s[:, 0:512], gmix[:, k:k + 1], [s, gmix], [wq])
            for c in range(4):
                s = stg.next()
                dma("sp", s[:, :], g.w_uq[c * 128:(c + 1) * 128, :], [], [s])
                g.wcast(wuq[:, c, :], s[:, :], g.gq[:, c:c + 1], [s, g.gq], [wuq])
            wuq3 = wuq.t[:].rearrange("p c (h e) -> p c h e", h=8)
            for j in range(8):
                t = NTP + j
                dma("sp", xt[:], g.xown[j * 128:(j + 1) * 128, :], [], [xt])
                act(xn[:], xt[:], AF.Square, [xt], [xn, ss], accum=ss[:, 0:1])
                g.rstd_from_ss(T(ss.t[:, 0:1], ss.b), D, T(tmp.t[:, 0:1], tmp.b), T(rs.t[:, 0:1], rs.b), None)
                act(xn[:], xt[:], AF.Copy, [xt, rs], [xn], scale=rs[:, 0:1])
                for half in range(2):
                    bk = bring.next()
                    bv = bk.t[:].bitcast(BF16)
                    for kk in range(8):
                        k = half * 8 + kk
                        tr(bv[:, kk * 128:(kk + 1) * 128], xn[:, k * 128:(k + 1) * 128], identb[:], [xn, identb], [bk])
                    cp("dve" if half == 0 else "act", hTown[:, half * 8:(half + 1) * 8, :],
                       bv.rearrange("p (k t) -> p k t", k=8), [bk], [hTown])
                bq = bring.next()
                for k in range(16):
                    mm(bq[:], hTown[:, k, :], wq[:, k, :], k == 0, k == 15, [hTown, wq], [bq])
                act(junk[:], bq[:], AF.Square, [bq], [junk, ss], accum=ss[:, 1:2])
                g.rstd_from_ss(T(ss.t[:, 1:2], ss.b), 512, T(tmp.t[:, 1:2], tmp.b), T(rs.t[:, 1:2], rs.b), None)
                act(cqn[:], bq[:], AF.Copy, [bq, rs], [cqn], scale=rs[:, 1:2])
                bk = bring.next()
                bv = bk.t[:].bitcast(BF16)
                for c in range(4):
                    tr(bv[:, c * 128:(c + 1) * 128], cqn[:, c * 128:(c + 1) * 128], identb[:], [cqn, identb], [bk])
                cp("dve", cqT[:], bv[:, 0:512].rearrange("p (c t) -> p c t", c=4), [bk], [cqT])
                bqn = [bring.next() for _ in range(2)]
                for h in range(8):
                    for c in range(4):
                        mm(bqn[h // 4][:, (h % 4) * 128:(h % 4 + 1) * 128], wuq3[:, c, h, 0:128], cqT[:, c, :], c == 0, c == 3,
                           [wuq, cqT], [bqn[h // 4]])
                for n in range(2):
                    cp("dve", qnT[:, n * 4:(n + 1) * 4, j * 128:(j + 1) * 128], bqn[n][:].rearrange("p (h t) -> p h t", h=4),
                       [bqn[n]], [qnT])
                    act(sqT[:, n * 4:(n + 1) * 4, :], bqn[n][:].rearrange("p (h t) -> p h t", h=4), AF.Square, [bqn[n]], [sqT])
                bqr = bring.next()
                for c in range(4):
                    mm(bqr[:].rearrange("p (h e) -> p h e", h=8), cqT[:, c, :], wuq3[:, c, :, 128:192], c == 0, c == 3,
                       [cqT, wuq], [bqr])
                bqr3 = bqr[:].rearrange("p (h e) -> p h e", h=8)
                bqr4 = bqr[:].rearrange("p (h two d) -> p h two d", h=8, two=2)
                bn = bring.next()
                for h in range(8):
                    mm(bn[:, h:h + 1], sqT[:, h, :], ones[:, 0:1], True, True, [sqT, ones], [bn])
                act(qr2[:], bqr3, AF.Square, [bqr], [qr2])
                P.op("dve", lambda e: e.tensor_reduce(out=qn2[:], in_=qr2[:], axis=AX.X, op=ALU.add), B([qr2]), B([qn2]))
                tt("dve", qn2[:], qn2[:], bn[:, 0:8], ALU.add, [qn2, bn], [qn2])
                act(qn2[:], qn2[:], AF.Sqrt, [qn2], [qn2])
                stt(qn2b[:, 0:8], qn2[:], -1.0, kmb[:], ALU.mult, ALU.mult, [qn2, kmb, qn2b], [qn2b])
                bsh = bring.next()
                P.op("pe", lambda e, bsh=bsh: e.transpose(out=bsh[:, 0:128], in_=qn2b[:], identity=identf[:]),
                     B([qn2b, identf]), B([bsh]))
                cp("dve", shT[:], bsh[0:8, 0:128], [bsh], [shT])
                dma("pool", qra[64:65, :, j * 128:(j + 1) * 128], shT[:], [shT], [], wacc=[qra])
                cs = tbl[:, t, 0:32]
                sn = tbl[:, t, 32:64]
                tt("dve", ra[:].rearrange("p h (two d) -> p h two d", two=2), bqr4,
                   cs.unsqueeze(1).unsqueeze(1).to_broadcast([128, 8, 2, 32]), ALU.mult, [bqr, tbl], [ra])
                snb = sn.unsqueeze(1).to_broadcast([128, 8, 32])
                tt("dve", rb[:, :, 0:32], bqr3[:, :, 32:64], snb, ALU.mult, [bqr, tbl], [rb])
                tt("dve", rb[:, :, 32:64], bqr3[:, :, 0:32], snb, ALU.mult, [bqr, tbl], [rb])
                tt("dve", qrp[:, :, 0:32], ra[:, :, 0:32], rb[:, :, 0:32], ALU.subtract, [ra, rb], [qrp])
                tt("dve", qrp[:, :, 32:64], ra[:, :, 32:64], rb[:, :, 32:64], ALU.add, [ra, rb], [qrp])
                bk = bring.next()
                bv = bk.t[:].bitcast(BF16)
                for h in range(8):
                    tr(bv[0:64, h * 128:(h + 1) * 128], qrp[:, h, :], identb[:], [qrp, identb], [bk])
                cp("act", qra[0:64, :, j * 128:(j + 1) * 128], bv[0:64, :].rearrange("p (h t) -> p h t", h=8), [bk], [qra])
            P.barrier()
        tap("qnT", qnT[:].rearrange("p h t -> p (h t)"), qnT)
        tap("qra", qra[:].rearrange("p h t -> p (h t)"), qra, lambda d: d[0:65, :])

        with ExitStack() as st_:
            KR = sb("KR", [65, NT * 128], BF16, st_)
            KTr = Ring([sb(f"KTh{i}", [128, NT, 128], BF16, st_) for i in range(2)])
            Vr = Ring([sb(f"Vh{i}", [128, NT, 129], BF16, st_) for i in range(2)])
            PTr = Ring([sb(f"PT{i}", [128, 512], BF16, st_) for i in range(3)])
            rz = sb("rz", [128, 1], F32, st_)
            ob = sb("ob", [128, 128], BF16, st_)
            dma("sp", KR[0:64, :], g.KR_s, [krs], [KR])
            P.op("dve", lambda e: e.memset(KR[64:65, :], 1.0), [], B([KR]))
            acc = banks[0:4]
            sring = Ring(banks[4:8])
            V4 = g.V_s.rearrange("t p h d -> p t h d")
            for h in range(8):
                KTh = KTr.next()
                Vh = Vr.next()
                dma("sp", KTh[:], g.KT_s[:, :, h, :], [kts], [KTh])
                dma("sp", Vh[:, :, 0:128], V4[:, :, h, :], [vs], [Vh])
                cp("dve", Vh[:, :, 128:129], kval[:].unsqueeze(2), [kval, Vh], [Vh])
                for qt in range(2):
                    nkb = NTP + 4 * qt + 4
                    def issueS(kb, qt=qt, h=h, KTh=KTh):
                        dg_i = kb - (NTP + 4 * qt)
                        q0 = max(0, dg_i) * 128
                        c0 = qt * 512 + q0
                        c1 = qt * 512 + 512
                        st = sring.next()
                        mm(st[:, q0:512], KTh[:, kb, :], qnT[:, h, c0:c1], True, False, [KTh, qnT], [st])
                        mm(st[:, q0:512], KR[0:65, kb * 128:(kb + 1) * 128], qra[0:65, h, c0:c1], False, True, [KR, qra], [st])
                        return st, q0, dg_i

                    pend = issueS(0)
                    for kb in range(nkb):
                        st, q0, dg_i = pend
                        if kb + 1 < nkb:
                            pend = issueS(kb + 1)
                        PT = PTr.next()
                        act(PT[:, q0:512], st[:, q0:512], AF.Exp, [st], [PT], scale=SCALE)
                        if dg_i >= 0:
                            tt("dve", PT[:, q0:q0 + 128], PT[:, q0:q0 + 128], causal[:], ALU.mult, [PT, causal], [PT])
                        for jq in range(max(0, dg_i), 4):
                            last = NTP + 4 * qt + jq
                            mm(acc[jq][:, 0:129], PT[:, jq * 128:(jq + 1) * 128], Vh[:, kb, :], kb == 0, kb == last,
                               [PT, Vh], [acc[jq]])
                    for jq in range(4):
                        P.op("dve", lambda e, jq=jq: e.reciprocal(out=rz[:], in_=acc[jq][:, 128:129]), B([acc[jq]]), B([rz]))
                        act(ob[:], acc[jq][:, 0:128], AF.Copy, [acc[jq], rz], [ob], scale=rz[:, 0:1])
                        bz = sring.next()
                        bzv = bz.t[:].bitcast(BF16)
                        tr(bzv[:, 0:128], ob[:], identb[:], [ob, identb], [bz])
                        col = qt * 512 + jq * 128
                        cp("dve", obT[:, h, col:col + 128], bzv[:, 0:128], [bz], [obT])
            P.barrier()
        tap("obT", obT[:].rearrange("p h t -> p (h t)"), obT)


def prep_inputs(inp, NPREV=7, ncores=NCORES, lite=False):
    f = lambda a: np.ascontiguousarray(np.asarray(a))
    X = f(inp["x"])[0]
    pos = f(inp["positions"])[0].astype(np.int32)
    NT = NPREV * 8 + 8
    shared = {
        "w_in": f(inp["w_in"])[0],
        "g_mix": f(f(inp["norm_mix"])[0].reshape(16, 128).T),
        "g_ffn": f(f(inp["norm_ffn"])[0].reshape(16, 128).T),
        "g_ple": f(f(inp["norm_ple"])[0].reshape(16, 128).T),
        "g_final": f(inp["norm_final"]).reshape(1, D),
        "lbT": f(f(inp["lb_logits"]).reshape(2, 8, 128).transpose(2, 0, 1).reshape(128, 16)),
        "lb_logits": f(inp["lb_logits"]),
        "g_hg": f(f(inp["hg_norm"])[0].reshape(8, 128).T),
        "g_q": f(f(inp["mla_q_norm"])[0].reshape(4, 128).T),
        "g_kv": f(f(inp["mla_kv_norm"])[0].reshape(4, 128).T),
        "w_uq": f(inp["w_uq"])[0], "w_ukv": f(inp["w_ukv"])[0],
        "w_a": f(inp["w_a"])[0], "w_b": f(inp["w_b"])[0], "w_o": f(inp["w_o"])[0],
        "peer_wq": f(inp["peer_wq"])[0],
        "k1T": f(f(inp["peer_k1"])[0].T), "k2T": f(f(inp["peer_k2"])[0].T),
        "peer_uT": f(f(inp["peer_u"])[0].T), "peer_v": f(inp["peer_v"])[0],
        "w_pg": f(inp["w_pg"])[0], "w_pe": f(inp["w_pe"])[0],
    }
    if lite:
        shared["peer_uT"] = f(shared["peer_uT"][:, :128])
        shared["peer_v"] = f(shared["peer_v"][:128])
    shared.update(host_consts())
    maps = []
    for c in range(ncores):
        xprev = np.zeros((max(NPREV, 1) * 1024, D), np.float32)
        pall = np.zeros((NT * 128,), np.int32)
        valid = np.zeros((NT * 128,), np.float32)
        for s_ in range(NPREV):
            blk = c - NPREV + s_
            if blk >= 0:
                xprev[s_ * 1024:(s_ + 1) * 1024] = X[blk * 1024:(blk + 1) * 1024]
                pall[s_ * 1024:(s_ + 1) * 1024] = pos[blk * 1024:(blk + 1) * 1024]
                valid[s_ * 1024:(s_ + 1) * 1024] = 1.0
        pall[NPREV * 1024:] = pos[c * 1024:(c + 1) * 1024]
        valid[NPREV * 1024:] = 1.0
        m = dict(shared)
        m["xprev"] = xprev
        m["xown"] = f(X[c * 1024:(c + 1) * 1024])
        m["posT"] = f(pall.reshape(NT, 128).T)
        m["kvalid"] = f(valid.reshape(NT, 128).T)
        m["p_own"] = f(f(inp["p"])[0, 0, c * 1024:(c + 1) * 1024, :])
        maps.append(m)
    return maps


def own_hT(g, dst, st, src_rows, gain=None, nt=8, tag="h"):
    P, sb, tr, act, ts, cp, dma, bring, B = g.P, g.sb, g.tr, g.act, g.ts, g.cp, g.dma, g.bring, g.B
    xring = Ring([sb(f"{tag}x{i}", [128, 2048], F32, st) for i in range(2)])
    xnring = Ring([sb(f"{tag}xn{i}", [128, 2048], BF16, st) for i in range(2)])
    ss = sb(f"{tag}ss", [128, 1], F32, st)
    tmp = sb(f"{tag}tmp", [128, 1], F32, st)
    rs = sb(f"{tag}rs", [128, 1], F32, st)
    for j in range(nt):
        xt = xring.next()
        dma("sp", xt[:], src_rows(j), g.xres_dep, [xt])
        xn = xnring.next()
        act(xn[:], xt[:], AF.Square, [xt], [xn, ss], accum=ss[:, 0:1])
        g.rstd_from_ss(ss, D, tmp, rs, None)
        act(xn[:], xt[:], AF.Copy, [xt, rs], [xn], scale=rs[:, 0:1])
        for half in range(2):
            bk = bring.next()
            bv = bk.t[:].bitcast(BF16)
            for kk in range(8):
                k = half * 8 + kk
                tr(bv[:, kk * 128:(kk + 1) * 128], xn[:, k * 128:(k + 1) * 128], g.identb[:], [xn, g.identb], [bk])
            if gain is None:
                cp("dve" if half == 0 else "act", dst[:, half * 8:(half + 1) * 8, j * 128:(j + 1) * 128],
                   bv.rearrange("p (k t) -> p k t", k=8), [bk], [Acc(dst)])
            else:
                for kk in range(8):
                    k = half * 8 + kk
                    ts("dve", dst[:, k, j * 128:(j + 1) * 128], bv[:, kk * 128:(kk + 1) * 128], gain[:, k:k + 1], None,
                       ALU.mult, None, [bk, gain], [Acc(dst)])


def load_w(g, dst, src_fn, nk, width, gain, stg):
    for k in range(nk):
        s = stg.next()
        g.dma("sp", s[:, 0:width], src_fn(k), [], [s])
        if gain is None:
            g.wcast(dst[:, k, 0:width], s[:, 0:width], None, [s], [dst])
        else:
            g.wcast(dst[:, k, 0:width], s[:, 0:width], gain[:, k:k + 1], [s, gain], [dst])


def phase2(g):
    P, sb, mm, tr, act, ts, tt, stt, cp, dma, tap = g.P, g.sb, g.mm, g.tr, g.act, g.ts, g.tt, g.stt, g.cp, g.dma, g.tap
    bring, B, w_in = g.bring, g.B, g.w_in
    gmix = g.gt["g_mix"]
    oaT, obT = g.oaT, g.obT
    with ExitStack() as sa:
        hT = sb("hTm", [128, 16, 1024], BF16, sa)
        ysb = sb("ysb", [128, 8, 2048], BF16, sa)
        with ExitStack() as sl:
            own_hT(g, hT, sl, lambda j: g.xown[j * 128:(j + 1) * 128, :], None, 8, "m")
            P.barrier()
        with ExitStack() as sw:
            CW = 256
            wga_r = Ring([sb(f"wga{i}", [128, 16, CW], BF16, sw) for i in range(2)])
            wgb_r = Ring([sb(f"wgb{i}", [128, 16, CW], BF16, sw) for i in range(2)])
            wa_r = Ring([sb(f"wa{i}", [128, 8, CW], BF16, sw) for i in range(2)])
            wb_r = Ring([sb(f"wb{i}", [128, 8, CW], BF16, sw) for i in range(2)])
            stg = Ring([sb(f"stgm{i}", [128, CW], F32, sw) for i in range(4)])
            sga_r = Ring([sb(f"sga{i}", [128, CW], F32, sw) for i in range(2)])
            sgb_r = Ring([sb(f"sgb{i}", [128, CW], F32, sw) for i in range(2)])
            y1_r = Ring([sb(f"y1{i}", [128, CW], F32, sw) for i in range(2)])
            for n in range(D // CW):
                c0, c1 = n * CW, (n + 1) * CW
                wga, wgb, wa, wb = wga_r.next(), wgb_r.next(), wa_r.next(), wb_r.next()
                load_w(g, wga, lambda k: w_in[k * 128:(k + 1) * 128, 5184 + c0:5184 + c1], 16, CW, gmix, stg)
                load_w(g, wgb, lambda k: w_in[k * 128:(k + 1) * 128, 7232 + c0:7232 + c1], 16, CW, gmix, stg)
                load_w(g, wa, lambda k: g.w_a[k * 128:(k + 1) * 128, c0:c1], 8, CW, None, stg)
                load_w(g, wb, lambda k: g.w_b[k * 128:(k + 1) * 128, c0:c1], 8, CW, None, stg)
                for j in range(8):
                    tok = slice(j * 128, (j + 1) * 128)
                    bga, bgb = bring.next(), bring.next()
                    ba, bb = bga, bgb
                    for k in range(16):
                        mm(bga[:, 0:CW], hT[:, k, tok], wga[:, k, :], k == 0, k == 15, [hT, wga], [bga])
                    for h in range(8):
                        mm(ba[:, CW:2 * CW], oaT[:, h, tok], wa[:, h, :], h == 0, h == 7, [oaT, wa], [ba])
                    for k in range(16):
                        mm(bgb[:, 0:CW], hT[:, k, tok], wgb[:, k, :], k == 0, k == 15, [hT, wgb], [bgb])
                    for h in range(8):
                        mm(bb[:, CW:2 * CW], obT[:, h, tok], wb[:, h, :], h == 0, h == 7, [obT, wb], [bb])
                    sga, sgb, y1 = sga_r.next(), sgb_r.next(), y1_r.next()
                    act(sga[:], bga[:, 0:CW], AF.Sigmoid, [bga], [sga])
                    act(sgb[:], bgb[:, 0:CW], AF.Sigmoid, [bgb], [sgb])
                    tt("dve", y1[:], ba[:, CW:2 * CW], sga[:], ALU.mult, [ba, sga], [y1])
                    tt("dve", sgb[:], bb[:, CW:2 * CW], sgb[:], ALU.mult, [bb, sgb], [sgb])
                    tt("dve", ysb[:, j, c0:c1], y1[:], sgb[:], ALU.add, [y1, sgb], [Acc(ysb)])
            P.barrier()
        tap("y", ysb[:].rearrange("p j d -> p (j d)"), ysb)
        for j in range(8):
            for half in range(2):
                bk = bring.next()
                bv = bk.t[:].bitcast(BF16)
                for kk in range(8):
                    k = half * 8 + kk
                    tr(bv[:, kk * 128:(kk + 1) * 128], ysb[:, j, k * 128:(k + 1) * 128], g.identb[:], [ysb, g.identb], [bk])
                cp("dve" if half == 0 else "act", hT[:, half * 8:(half + 1) * 8, j * 128:(j + 1) * 128],
                   bv.rearrange("p (k t) -> p k t", k=8), [bk], [hT])
        with ExitStack() as sw:
            wo_r = Ring([sb(f"wo{i}", [128, 16, 512], BF16, sw) for i in range(2)])
            stg = Ring([sb(f"stgo{i}", [128, 512], F32, sw) for i in range(4)])
            xr = Ring([sb(f"xr{i}", [128, 512], F32, sw) for i in range(3)])
            for n in range(4):
                wo = wo_r.next()
                load_w(g, wo, lambda k: g.w_o[k * 128:(k + 1) * 128, n * 512:(n + 1) * 512], 16, 512, None, stg)
                for j in range(8):
                    tok = slice(j * 128, (j + 1) * 128)
                    bo = bring.next()
                    for k in range(16):
                        mm(bo[:], hT[:, k, tok], wo[:, k, :], k == 0, k == 15, [hT, wo], [bo])
                    x = xr.next()
                    dma("sp", x[:], g.xown[tok, n * 512:(n + 1) * 512], [], [x])
                    tt("dve", x[:], bo[:], x[:], ALU.add, [bo, x], [x])
                    dma("pool", g.xres[tok, n * 512:(n + 1) * 512], x[:], [x], [], wacc=[g.xres_t])
            P.barrier()
    g.xres_dep = [g.xres_t]


def phase4(g):
    P, sb, mm, tr, act, ts, tt, stt, cp, dma, tap = g.P, g.sb, g.mm, g.tr, g.act, g.ts, g.tt, g.stt, g.cp, g.dma, g.tap
    bring, B = g.bring, g.B
    with ExitStack() as sa:
        hT = sb("hTp", [128, 16, 1024], BF16, sa)
        pT = sb("pT", [128, 2, 1024], BF16, sa)
        x3 = sb("x3", [128, 8, 2048], F32, sa)
        gfin = sb("gfin", [128, 2048], F32, sa)
        ssq = sb("ssq", [128, 8, 4], F32, sa)
        dma("sp", gfin[:], g.g_final[0:1, :].to_broadcast([128, 2048]), [], [gfin])
        with ExitStack() as sl:
            own_hT(g, hT, sl, lambda j: g.xres[j * 128:(j + 1) * 128, :], g.gt["g_ple"], 8, "p")
            pt_ = sb("pt_", [128, 256], F32, sl)
            ptb = sb("ptb", [128, 256], BF16, sl)
            for j in range(8):
                dma("sp", pt_[:], g.p_own[j * 128:(j + 1) * 128, :], [], [pt_])
                cp("dve", ptb[:], pt_[:], [pt_], [ptb])
                bk = bring.next()
                bv = bk.t[:].bitcast(BF16)
                for c in range(2):
                    tr(bv[:, c * 128:(c + 1) * 128], ptb[:, c * 128:(c + 1) * 128], g.identb[:], [ptb, g.identb], [bk])
                cp("act", pT[:, :, j * 128:(j + 1) * 128], bv[:, 0:256].rearrange("p (c t) -> p c t", c=2), [bk], [pT])
            P.barrier()
        with ExitStack() as sw:
            wpg_r = Ring([sb(f"wpg{i}", [128, 16, 512], BF16, sw) for i in range(2)])
            wpe_r = Ring([sb(f"wpe{i}", [128, 2, 512], BF16, sw) for i in range(2)])
            stg = Ring([sb(f"stgp{i}", [128, 512], F32, sw) for i in range(4)])
            sg = sb("sgp", [128, 512], F32, sw)
            junk = sb("junkp", [128, 512], F32, sw)
            xr = Ring([sb(f"xrp{i}", [128, 512], F32, sw) for i in range(3)])
            for n in range(4):
                cols = slice(n * 512, (n + 1) * 512)
                wpg, wpe = wpg_r.next(), wpe_r.next()
                load_w(g, wpg, lambda k: g.w_pg[k * 128:(k + 1) * 128, cols], 16, 512, None, stg)
                load_w(g, wpe, lambda k: g.w_pe[k * 128:(k + 1) * 128, cols], 2, 512, None, stg)
                for j in range(8):
                    tok = slice(j * 128, (j + 1) * 128)
                    bg, be = bring.next(), bring.next()
                    for k in range(16):
                        mm(bg[:], hT[:, k, tok], wpg[:, k, :], k == 0, k == 15, [hT, wpg], [bg])
                    for c in range(2):
                        mm(be[:], pT[:, c, tok], wpe[:, c, :], c == 0, c == 1, [pT, wpe], [be])
                    x = xr.next()
                    dma("sp", x[:], g.xres[tok, cols], g.xres_dep, [x])
                    act(sg[:], bg[:], AF.Sigmoid, [bg], [sg])
                    tt("dve", sg[:], be[:], sg[:], ALU.mult, [be, sg], [sg])
                    tt("dve", x3[:, j, cols], x[:], sg[:], ALU.add, [x, sg], [Acc(x3)])
                    act(junk[:], x3[:, j, cols], AF.Square, [x3], [junk, ssq], accum=ssq[:, j, n:n + 1])
            P.barrier()
        tap("x3", x3[:].rearrange("p j d -> p (j d)"), x3)
        with ExitStack() as sf:
            tot = sb("tot", [128, 8], F32, sf)
            tmp = sb("tmpf", [128, 8], F32, sf)
            rs = sb("rsf", [128, 8], F32, sf)
            P.op("dve", lambda e: e.tensor_reduce(out=tot[:], in_=ssq[:], axis=AX.X, op=ALU.add), B([ssq]), B([tot]))
            g.rstd_from_ss(tot, D, tmp, rs, None)
            tap("rsf", rs[:], rs)
            tap("tot", tot[:], tot)
            tap("ssq", ssq[:].rearrange("p j n -> p (j n)"), ssq)
            tap("gfin", gfin[:], gfin)
            for j in range(8):
                stt(x3[:, j, :], x3[:, j, :], rs[:, j:j + 1], gfin[:], ALU.mult, ALU.mult, [x3, rs, gfin], [x3])
                dma("sp", g.out[j * 128:(j + 1) * 128, :], x3[:, j, :], [x3], [])
            P.barrier()


STOP_AFTER = None


def kernel(**inputs):
    maps = prep_inputs(inputs, NPREV=7, ncores=NCORES)
    nc = build(NPREV=7, stop_after=STOP_AFTER)
    res = run_bass_kernel_spmd(nc, maps, core_ids=list(range(NCORES)))
    return np.concatenate([r["out"] for r in res.results], axis=0).reshape(1, NCORES * 1024, D).astype(np.float32)


def phase3(g):
    P, sb, mm, tr, act, ts, tt, stt, cp, dma, tap = g.P, g.sb, g.mm, g.tr, g.act, g.ts, g.tt, g.stt, g.cp, g.dma, g.tap
    bring, banks, B = g.bring, g.banks, g.B
    identf, identb = g.identf, g.identb
    TP, NI = 512, 64
    NEG = -1.0e30
    uT3 = g.peer_uT.rearrange("(k p) e -> p k e", p=128)
    v3 = g.peer_v.rearrange("(i p) d -> p i d", p=128)
    with ExitStack() as s3:
        k1b = sb("k1b", [128, 128], BF16, s3)
        k2b = sb("k2b", [128, 128], BF16, s3)
        with ExitStack() as sl:
            kf = sb("kf", [128, 256], F32, sl)
            dma("sp", kf[:, 0:128], g.k1T, [], [kf])
            dma("sp", kf[:, 128:256], g.k2T, [], [kf])
            cp("dve", k1b[:], kf[:, 0:128], [kf], [k1b])
            cp("dve", k2b[:], kf[:, 128:256], [kf], [k2b])
            P.barrier()
        for ps in range(1024 // TP):
            with ExitStack() as sp_:
                hnT = sb(f"hnT{ps}", [128, 16, TP], BF16, sp_)
                qpT = sb(f"qpT{ps}", [128, 16, TP], BF16, sp_)
                statT = sb(f"statT{ps}", [128, 4, TP], F32, sp_)
                xacc = sb(f"xacc{ps}", [128, 4, 2048], F32, sp_)
                for jl in range(4):
                    dma("sp", xacc[:, jl, :], g.xres[(ps * 4 + jl) * 128:(ps * 4 + jl + 1) * 128, :], g.xres_dep, [xacc])
                with ExitStack() as sl:
                    own_hT(g, hnT, sl, lambda j: g.xres[(ps * 4 + j) * 128:(ps * 4 + j + 1) * 128, :], g.gt["g_ffn"], 4, f"f{ps}")
                    P.barrier()
                with ExitStack() as sq_:
                    wqp_r = Ring([sb(f"wqp{ps}{i}", [128, 16, 512], BF16, sq_) for i in range(2)])
                    stg = Ring([sb(f"stg3{ps}{i}", [128, 512], F32, sq_) for i in range(4)])
                    for mg in range(4):
                        wqp = wqp_r.next()
                        load_w(g, wqp, lambda k: g.peer_wq[k * 128:(k + 1) * 128, mg * 512:(mg + 1) * 512], 16, 512, None, stg)
                        for m4 in range(4):
                            bq = bring.next()
                            for k in range(16):
                                mm(bq[:], wqp[:, k, m4 * 128:(m4 + 1) * 128], hnT[:, k, :], k == 0, k == 15, [wqp, hnT], [bq])
                            cp("act" if m4 % 2 == 0 else "dve", qpT[:, mg * 4 + m4, :], bq[:], [bq], [Acc(qpT)])
                    P.barrier()
                with ExitStack() as sk_:
                    m8 = sb(f"m8{ps}", [128, 16, 16], F32, sk_)
                    wk = sb(f"wk{ps}", [128, 128], F32, sk_)
                    cand = sb(f"cand{ps}", [128, 8, 256], F32, sk_)
                    wk2 = sb(f"wk2{ps}", [128, 256], F32, sk_)
                    c8 = sb(f"c8{ps}", [128, 8, 16], F32, sk_)
                    ex = sb(f"ex{ps}", [128, 8, 16], F32, sk_)
                    zz = sb(f"zz{ps}", [128, 8], F32, sk_)
                    rz = sb(f"rz3{ps}", [128, 8], F32, sk_)
                    st4 = sb(f"st4{ps}", [128, 4, 8, 16], F32, sk_)
                    for jl in range(4):
                        tok = slice(jl * 128, (jl + 1) * 128)
                        bsc = [bring.next() for _ in range(4)]
                        for m in range(16):
                            mm(bsc[m // 4][:, (m % 4) * 128:(m % 4 + 1) * 128], qpT[:, m, tok], (k1b if m % 2 == 0 else k2b)[:],
                               True, True, [qpT, k1b, k2b], [bsc[m // 4]])
                        for m in range(16):
                            sc = bsc[m // 4][:, (m % 4) * 128:(m % 4 + 1) * 128]
                            bb = bsc[m // 4]
                            P.op("dve", lambda e, sc=sc, m=m: e.max(out=m8[:, m, 0:8], in_=sc), B([bb]), B([m8]))
                            P.op("dve", lambda e, sc=sc, m=m: e.match_replace(out=wk[:], in_to_replace=m8[:, m, 0:8], in_values=sc,
                                                                              imm_value=NEG), B([bb, m8]), B([wk]))
                            P.op("dve", lambda e, m=m: e.max(out=m8[:, m, 8:16], in_=wk[:]), B([wk]), B([m8]))
                        m84 = m8[:].rearrange("p (h two) a -> p h two a", two=2)
                        v1 = m84[:, :, 0, :]
                        v2 = m84[:, :, 1, :]
                        tt("dve", cand[:].rearrange("p h (a b) -> p h a b", a=16), v1.unsqueeze(3).to_broadcast([128, 8, 16, 16]),
                           v2.unsqueeze(2).to_broadcast([128, 8, 16, 16]), ALU.add, [m8], [cand])
                        for h in range(8):
                            P.op("dve", lambda e, h=h: e.max(out=c8[:, h, 0:8], in_=cand[:, h, :]), B([cand]), B([c8]))
                            P.op("dve", lambda e, h=h: e.match_replace(out=wk2[:], in_to_replace=c8[:, h, 0:8], in_values=cand[:, h, :],
                                                                       imm_value=NEG), B([cand, c8]), B([wk2]))
                            P.op("dve", lambda e, h=h: e.max(out=c8[:, h, 8:16], in_=wk2[:]), B([wk2]), B([c8]))
                        mx = c8[:, :, 0:1]
                        tau = c8[:, :, 15:16]
                        tt("dve", ex[:], c8[:], mx.to_broadcast([128, 8, 16]), ALU.subtract, [c8], [ex])
                        act(ex[:], ex[:], AF.Exp, [ex], [ex])
                        P.op("dve", lambda e, zz=zz, ex=ex: e.tensor_reduce(out=zz[:], in_=ex[:], axis=AX.X, op=ALU.add), B([ex]), B([zz]))
                        P.op("dve", lambda e, rz=rz, zz=zz: e.reciprocal(out=rz[:], in_=zz[:]), B([zz]), B([rz]))
                        cp("dve", st4[:, 0, :, :], v1, [m8], [st4])
                        tt("dve", st4[:, 1, :, :], v1, mx.to_broadcast([128, 8, 16]), ALU.subtract, [m8, c8], [st4])
                        tt("dve", st4[:, 2, :, :], tau.to_broadcast([128, 8, 16]), v1, ALU.subtract, [m8, c8], [st4])
                        ts("dve", st4[:, 2, :, :], st4[:, 2, :, :], -2.0e-5, None, ALU.add, None, [st4], [st4])
                        cp("dve", st4[:, 3, :, :], rz[:].unsqueeze(2).to_broadcast([128, 8, 16]), [rz], [st4])
                        bt = bring.next()
                        for q in range(4):
                            P.op("pe", lambda e, q=q, bt=bt: e.transpose(out=bt[:, q * 128:(q + 1) * 128],
                                                                         in_=st4[:, q, :, :].rearrange("p h a -> p (h a)"),
                                                                         identity=identf[:]), B([st4, identf]), B([bt]))
                        cp("act", statT[:, :, tok], bt[:].rearrange("p (q t) -> p q t", q=4), [bt], [statT])
                    P.barrier()
                qp4 = qpT.t[:].rearrange("p (h two) t -> p h two t", two=2)
                for sub in range(128 // NI):
                    i0 = sub * NI
                    with ExitStack() as sg_:
                        GT = sb(f"GT{ps}{sub}", [128, NI, TP], BF16, sg_)
                        Ptr = Ring([sb(f"Pt{ps}{sub}{i}", [128, NI], BF16, sg_) for i in range(6)])
                        Er = Ring([sb(f"E{ps}{sub}{i}", [128, 128], F32, sg_) for i in range(6)])
                        Qr = Ring([sb(f"Qt{ps}{sub}{i}", [128, 128], BF16, sg_) for i in range(6)])
                        rS = Ring(banks[0:4])
                        rg = Ring(banks[4:6])
                        q1r = Ring([sb(f"q1r{ps}{sub}{i}", [128, 8, 128], BF16, sg_) for i in range(3)])
                        q2r = Ring([sb(f"q2r{ps}{sub}{i}", [128, 8, 128], BF16, sg_) for i in range(3)])
                        DEPTH = 3
                        Sof = {}
                        qrep = {}

                        def issueS(t):
                            if t % 8 == 0:
                                q1t = q1r.next()
                                q2t = q2r.next()
                                for hf_, qt_ in ((0, q1t), (1, q2t)):
                                    src = qp4[:, :, hf_, t:t + 8].rearrange("p h t -> p t h").unsqueeze(3).to_broadcast([128, 8, 8, 16])
                                    eng_ = ("pool", "dve", "pool", "act")[(t // 8 * 2 + hf_) % 4]
                                    cp(eng_, qt_[:].rearrange("p t (h a) -> p t h a", a=16), src, [qpT], [qt_])
                                qrep[t // 8] = (q1t, q2t)
                            q1t, q2t = qrep[t // 8]
                            bS = rS.next()
                            mm(bS[:, 0:NI], q1t[:, t % 8, :], k1b[:, i0:i0 + NI], True, True, [q1t, k1b], [bS])
                            mm(bS[:, 128:256], q2t[:, t % 8, :], k2b[:], True, True, [q2t, k2b], [bS])
                            Sof[t] = bS

                        for t in range(DEPTH):
                            issueS(t)
                        for t in range(TP):
                            if t + DEPTH < TP:
                                issueS(t + DEPTH)
                            bS = Sof.pop(t)
                            Pt = Ptr.next()
                            E = Er.next()
                            Qt = Qr.next()
                            act(E[:], bS[:, 128:256], AF.Exp, [bS, statT], [E], bias=statT[:, 1, t:t + 1])
                            ts("dve", Pt[:], bS[:, 0:NI], statT[:, 0, t:t + 1], statT[:, 3, t:t + 1], ALU.is_equal, ALU.mult,
                               [bS, statT], [Pt])
                            stt(Qt[:], bS[:, 128:256], statT[:, 2, t:t + 1], E[:], ALU.is_ge, ALU.mult, [bS, statT, E], [Qt])
                            if t % 4 == 0:
                                bg = rg.next()
                            mm(bg[:, (t % 4) * NI:(t % 4 + 1) * NI], Qt[:], Pt[:], True, True, [Qt, Pt], [bg])
                            if t % 4 == 3:
                                cp("act", GT[:, :, t - 3:t + 1].rearrange("p i t -> p t i"),
                                   bg[:, 0:4 * NI].rearrange("p (t i) -> p t i", t=4), [bg], [Acc(GT)])
                        UTr = Ring([sb(f"UT{ps}{sub}{i}", [128, 16, 256], BF16, sg_) for i in range(4)])
                        glr = Ring([sb(f"gl{ps}{sub}{i}", [128, TP], BF16, sg_) for i in range(2)])
                        zr = Ring(banks[6:8])
                        for ig in range(NI // 2):
                            UT = UTr.next()
                            dma("pool", UT[:], uT3[:, :, (i0 + ig * 2) * 128:(i0 + ig * 2 + 2) * 128], [], [UT])
                            for il in range(2):
                                i = ig * 2 + il
                                bz = zr.next()
                                for k in range(16):
                                    mm(bz[:], UT[:, k, il * 128:(il + 1) * 128], hnT[:, k, :], k == 0, k == 15, [UT, hnT], [bz])
                                gl = glr.next()
                                act(gl[:], bz[:], AF.Gelu, [bz], [gl])
                                tt("dve", GT[:, i, :], GT[:, i, :], gl[:], ALU.mult, [GT, gl], [GT])
                        Vr = Ring([sb(f"Vg{ps}{sub}{i}", [128, 4, 512], BF16, sg_) for i in range(4)])
                        acc = banks[0:4]
                        for dc in range(4):
                            for ig in range(NI // 4):
                                Vg = Vr.next()
                                dma("pool", Vg[:], v3[:, i0 + ig * 4:i0 + ig * 4 + 4, dc * 512:(dc + 1) * 512], [], [Vg])
                                for il in range(4):
                                    i = ig * 4 + il
                                    for tq in range(4):
                                        mm(acc[tq][:], GT[:, i, tq * 128:(tq + 1) * 128], Vg[:, il, :], i == 0, i == NI - 1,
                                           [GT, Vg], [acc[tq]])
                            for tq in range(4):
                                tt("dve", xacc[:, tq, dc * 512:(dc + 1) * 512], acc[tq][:], xacc[:, tq, dc * 512:(dc + 1) * 512],
                                   ALU.add, [acc[tq], xacc], [xacc])
                        P.barrier()
                for jl in range(4):
                    dma("sp", g.xres[(ps * 4 + jl) * 128:(ps * 4 + jl + 1) * 128, :], xacc[:, jl, :], [xacc], [], wacc=[g.xres_t])
                P.barrier()
    g.xres_dep = [g.xres_t]
```

```python
import numpy as np
from contextlib import ExitStack
import concourse.bass as bass
import concourse.mybir as mybir
from concourse.bass_utils import run_bass_kernel_spmd

F32 = mybir.dt.float32
BF16 = mybir.dt.bfloat16
I32 = mybir.dt.int32
AF = mybir.ActivationFunctionType
ALU = mybir.AluOpType
AX = mybir.AxisListType

ENGS = ("pe", "act", "dve", "pool", "sp")
NCORES = 8
D = 2048
KD = 16
TOWN = 1024
EPS = 1e-6
PI = float(np.pi)


class Buf:
    __slots__ = ("name", "w", "r", "excl")

    def __init__(self, name, excl=False):
        self.name = name
        self.w = {}
        self.r = {}
        self.excl = excl


class Prog:
    SAME_ENG_SYNC = True

    def __init__(self, nc, es, dma_slots=8):
        self.nc = nc
        self.es = es
        self.q = {e: [] for e in ENGS}
        self.cnt = {e: 0 for e in ENGS}
        self.sem = {e: es.enter_context(nc.semaphore("s_" + e)) for e in ENGS}
        self.K = dma_slots
        self.dsem = {}
        self.dcnt = {}
        for e in ("sp", "pool", "act"):
            self.dsem[e] = [es.enter_context(nc.semaphore(f"d_{e}{k}")) for k in range(dma_slots)]
            self.dcnt[e] = 0
        self.seen = {e: {} for e in ENGS}
        self.nbuf = 0

    def buf(self, name=None, excl=False):
        self.nbuf += 1
        return Buf(name or f"b{self.nbuf}", excl)

    def _deps(self, eng, r, w, wacc=()):
        deps = {}

        def add(k, v):
            if deps.get(k, 0) < v:
                deps[k] = v

        for b in r:
            for k, v in b.w.items():
                add(k, v)
            if b.excl:
                for k, v in b.r.items():
                    if k != eng:
                        add(k, v)
        for b in w:
            for k, v in b.w.items():
                add(k, v)
            for k, v in b.r.items():
                add(k, v)
        for b in wacc:
            for k, v in b.r.items():
                add(k, v)
        waits = []
        seen = self.seen[eng]
        for k, v in deps.items():
            if k == eng and (eng == "pe" or not self.SAME_ENG_SYNC):
                continue
            if seen.get(k, 0) >= v:
                continue
            seen[k] = v
            waits.append((k, v))
        return waits

    def _mark(self, ev, r, w, wacc=()):
        k, v = ev
        for b in r:
            if b.r.get(k, 0) < v:
                b.r[k] = v
        for b in w:
            b.w = {k: v}
            b.r = {}
        for b in wacc:
            if b.w.get(k, 0) < v:
                b.w[k] = v

    def op(self, eng, fn, r=(), w=(), wacc=()):
        waits = self._deps(eng, r, w, wacc)
        self.cnt[eng] += 1
        ev = (eng, self.cnt[eng])
        self.q[eng].append((waits, fn, self.sem[eng], 1))
        self._mark(ev, r, w, wacc)
        return ev

    def dma(self, eng, out, in_, r=(), w=(), wacc=(), **kw):
        waits = self._deps(eng, r, w, wacc)
        j = self.dcnt[eng]
        self.dcnt[eng] = j + 1
        k = j % self.K
        val = 16 * (j // self.K + 1)
        key = ("d", eng, k)
        if j >= self.K:
            pv = val - 16
            if self.seen[eng].get(key, 0) < pv:
                self.seen[eng][key] = pv
                waits.append((key, pv))
        self.q[eng].append((waits, lambda e: e.dma_start(out=out, in_=in_, **kw), self.dsem[eng][k], 16))
        ev = (key, val)
        self._mark(ev, r, w, wacc)
        return ev

    def barrier(self):
        targets = [(e, self.cnt[e]) for e in ENGS if self.cnt[e] > 0]
        for eng in ("sp", "pool", "act"):
            j = self.dcnt[eng]
            for k in range(self.K):
                n = (j - 1 - k) // self.K + 1 if j > k else 0
                if n > 0:
                    targets.append((("d", eng, k), 16 * n))
        for e in ENGS:
            waits = []
            for k, v in targets:
                if k == e:
                    continue
                if self.seen[e].get(k, 0) >= v:
                    continue
                self.seen[e][k] = v
                waits.append((k, v))
            if waits:
                self.q[e].append((waits, None, None, 0))

    def _semh(self, k):
        if isinstance(k, tuple):
            return self.dsem[k[1]][k[2]]
        return self.sem[k]

    def emit(self):
        nc = self.nc
        with nc.Block() as block:
            def mk(ename):
                def body(e):
                    for waits, fn, sem, inc in self.q[ename]:
                        for k, v in waits:
                            e.wait_ge(self._semh(k), v)
                        if fn is not None:
                            ins = fn(e)
                            ins.then_inc(sem, inc)
                return body

            block.tensor(mk("pe"))
            block.scalar(mk("act"))
            block.vector(mk("dve"))
            block.gpsimd(mk("pool"))
            block.sync(mk("sp"))


class T:
    __slots__ = ("t", "b")

    def __init__(self, t, b):
        self.t = t
        self.b = b

    def __getitem__(self, idx):
        return self.t[idx]


class NS:
    pass


class Acc:
    __slots__ = ("t",)

    def __init__(self, t):
        self.t = t


class Ring:
    def __init__(self, tiles):
        self.tiles = tiles
        self.i = 0

    def next(self):
        t = self.tiles[self.i % len(self.tiles)]
        self.i += 1
        return t


def host_consts():
    c = {}
    c["c_ident"] = np.eye(128, dtype=np.float32)
    s = np.arange(128)[:, None]
    t = np.arange(128)[None, :]
    same = (s // 64) == (t // 64)
    U = (same & (s <= t)).astype(np.float32)
    R = (same & ((s % 64) <= 31)).astype(np.float32)
    L = same.astype(np.float32)
    c["c_tri"] = np.ascontiguousarray(np.stack([U, U - R, L - U], axis=1))
    c["c_causal"] = (s <= t).astype(np.float32)
    ind = np.zeros((128, 2), np.float32)
    ind[:64, 0] = 1.0
    ind[64:, 1] = 1.0
    c["c_ind"] = ind
    half = 32
    freqs = (10000.0 ** (-np.arange(half, dtype=np.float32) / half)).astype(np.float32)
    c["c_freq"] = np.ascontiguousarray(np.tile(freqs[None, :], (128, 2)))
    ph = np.concatenate([np.full(32, PI / 2), np.zeros(32)]).astype(np.float32)
    c["c_phase"] = np.ascontiguousarray(np.tile(ph[None, :], (128, 1)))
    return c


def build(NPREV=7, taps=(), stop_after=None, lite=False):
    NTP = NPREV * 8
    NT = NTP + 8
    nc = bass.Bass("TRN2", target_bir_lowering=False)

    def din(name, shape, dt=F32):
        return nc.dram_tensor(name, list(shape), dt, kind="ExternalInput").ap()

    xprev = din("xprev", [max(NPREV, 1) * 1024, D])
    xown = din("xown", [1024, D])
    posT = din("posT", [128, NT], I32)
    kvalid = din("kvalid", [128, NT])
    p_own = din("p_own", [1024, 256])
    w_in = din("w_in", [D, 9280])
    gT = {n: din(n, [128, 16]) for n in ("g_mix", "g_ffn", "g_ple")}
    g_final = din("g_final", [1, D])
    lbT = din("lbT", [128, 16])
    lb_logits = din("lb_logits", [2, 1024])
    g_hg = din("g_hg", [128, 8])
    g_q = din("g_q", [128, 4])
    g_kv = din("g_kv", [128, 4])
    w_uq = din("w_uq", [512, 1536])
    w_ukv = din("w_ukv", [512, 2048])
    w_a = din("w_a", [1024, D])
    w_b = din("w_b", [1024, D])
    w_o = din("w_o", [D, D])
    peer_wq = din("peer_wq", [D, D])
    k1T = din("k1T", [128, 128])
    k2T = din("k2T", [128, 128])
    peer_uT = din("peer_uT", [D, 128 if lite else 16384])
    peer_v = din("peer_v", [128 if lite else 16384, D])
    w_pg = din("w_pg", [D, D])
    w_pe = din("w_pe", [256, D])
    consts = {k: din(k, v.shape) for k, v in host_consts().items()}
    out = nc.dram_tensor("out", [1024, D], F32, kind="ExternalOutput").ap()
    tap_out = {}
    for name, shape in taps:
        tap_out[name] = nc.dram_tensor("tap_" + name, list(shape), F32, kind="ExternalOutput").ap()
    KT_s = nc.dram_tensor("KT_s", [128, NT, 8, 128], BF16).ap()
    KR_s = nc.dram_tensor("KR_s", [64, NT * 128], BF16).ap()
    V_s = nc.dram_tensor("V_s", [NT, 128, 8, 128], BF16).ap()
    xres = nc.dram_tensor("xres", [1024, D], F32).ap()

    with ExitStack() as es:
        P = Prog(nc, es)

        def sb(name, shape, dt, st=None):
            t = (st or es).enter_context(nc.sbuf_tensor(name, list(shape), dt))
            return T(t, P.buf(name))

        banks = [T(es.enter_context(nc.psum_tensor(f"bank{i}", [128, 512], F32)), P.buf(f"bank{i}", excl=True)) for i in range(8)]
        bring = Ring(banks)

        def B(l):
            return [x.b for x in l]

        def BW(l):
            return [x.b for x in l if not isinstance(x, Acc)], [x.t.b for x in l if isinstance(x, Acc)]

        def mm(o, lhsT, rhs, st, sp, r, w):
            P.op("pe", lambda e: e.matmul(o, lhsT=lhsT, rhs=rhs, start=st, stop=sp), B(r), *BW(w))

        def tr(o, i, ident, r, w):
            P.op("pe", lambda e: e.transpose(out=o, in_=i, identity=ident), B(r), *BW(w))

        def act(o, i, func, r, w, bias=None, scale=None, accum=None):
            kw = {}
            if bias is not None:
                kw["bias"] = bias
            if scale is not None:
                kw["scale"] = scale
            if accum is not None:
                kw["accum_out"] = accum
            P.op("act", lambda e: e.activation(out=o, in_=i, func=func, **kw), B(r), *BW(w))

        def ts(eng, o, i, s1, s2, op0, op1, r, w):
            if op1 is None:
                P.op(eng, lambda e: e.tensor_scalar(out=o, in0=i, scalar1=s1, scalar2=None, op0=op0), B(r), *BW(w))
            else:
                P.op(eng, lambda e: e.tensor_scalar(out=o, in0=i, scalar1=s1, scalar2=s2, op0=op0, op1=op1), B(r), *BW(w))

        def tt(eng, o, a, b, op, r, w):
            P.op(eng, lambda e: e.tensor_tensor(out=o, in0=a, in1=b, op=op), B(r), *BW(w))

        def stt(o, a, s, b, op0, op1, r, w):
            P.op("dve", lambda e: e.scalar_tensor_tensor(out=o, in0=a, scalar=s, in1=b, op0=op0, op1=op1), B(r), *BW(w))

        def cp(eng, o, i, r, w):
            if eng == "act":
                P.op("act", lambda e: e.copy(out=o, in_=i), B(r), *BW(w))
            else:
                P.op(eng, lambda e: e.tensor_copy(out=o, in_=i), B(r), *BW(w))

        wc_state = [0]

        def wcast(o, i, gain, r, w, engines=("act", "dve", "act", "dve", "pool")):
            eng = engines[wc_state[0] % len(engines)]
            wc_state[0] += 1
            if eng == "act":
                if gain is None:
                    cp("act", o, i, r, w)
                else:
                    act(o, i, AF.Copy, r, w, scale=gain)
            else:
                if gain is None:
                    cp(eng, o, i, r, w)
                else:
                    ts(eng, o, i, gain, None, ALU.mult, None, r, w)

        def dma(eng, o, i, r, w, wacc=(), **kw):
            P.dma(eng, o, i, B(r), B(w), B(wacc), **kw)

        def tap(name, src_ap, src_t, dst_slice=None):
            if name in tap_out:
                dst = tap_out[name] if dst_slice is None else dst_slice(tap_out[name])
                dma("pool", dst, src_ap, [src_t], [], max_dma_last_dim=2048)

        def rstd_from_ss(ss, n, tmp, rs, st_r):
            ts("dve", tmp[:], ss[:], 1.0 / n, EPS, ALU.mult, ALU.add, [ss], [tmp])
            act(tmp[:], tmp[:], AF.Sqrt, [tmp], [tmp])
            P.op("dve", lambda e: e.reciprocal(out=rs[:], in_=tmp[:]), B([tmp]), B([rs]))

        identf = sb("identf", [128, 128], F32)
        identb = sb("identb", [128, 128], BF16)
        tri = sb("tri", [128, 3, 128], F32)
        causal = sb("causal", [128, 128], F32)
        ind = sb("ind", [128, 2], F32)
        kval = sb("kval", [128, NT], F32)
        gt = {n: sb("sb_" + n, [128, 16], F32) for n in gT}
        lbc = sb("lbc", [128, 16], F32)
        ghg = sb("ghg", [128, 8], F32)
        gq = sb("gq", [128, 4], F32)
        gkv = sb("gkv", [128, 4], F32)
        dma("sp", identf[:], consts["c_ident"], [], [identf])
        dma("sp", tri[:], consts["c_tri"], [], [tri])
        dma("sp", causal[:], consts["c_causal"], [], [causal])
        dma("sp", ind[:], consts["c_ind"], [], [ind])
        dma("sp", kval[:], kvalid, [], [kval])
        for n in gT:
            dma("sp", gt[n][:], gT[n], [], [gt[n]])
        dma("sp", lbc[:], lbT, [], [lbc])
        dma("sp", ghg[:], g_hg, [], [ghg])
        dma("sp", gq[:], g_q, [], [gq])
        dma("sp", gkv[:], g_kv, [], [gkv])
        cp("dve", identb[:], identf[:], [identf], [identb])
        Utri = tri[:, 0, :]
        M1 = tri[:, 1, :]
        M2 = tri[:, 2, :]

        s1 = es.enter_context(ExitStack())
        tbl = sb("tbl", [128, NT, 64], F32, s1)
        with ExitStack() as s0:
            posi = sb("posi", [128, NT], I32, s0)
            posf = sb("posf", [128, NT], F32, s0)
            frq = sb("frq", [128, 64], F32, s0)
            phs = sb("phs", [128, 64], F32, s0)
            kk = sb("kk", [128, NT, 64], F32, s0)
            ki = sb("ki", [128, NT, 64], I32, s0)
            dma("sp", posi[:], posT, [], [posi])
            dma("sp", frq[:], consts["c_freq"], [], [frq])
            dma("sp", phs[:], consts["c_phase"], [], [phs])
            cp("dve", posf[:], posi[:], [posi], [posf])
            tt("dve", tbl[:], posf[:].unsqueeze(2).to_broadcast([128, NT, 64]),
               frq[:].unsqueeze(1).to_broadcast([128, NT, 64]), ALU.mult, [posf, frq], [tbl])
            tt("dve", tbl[:], tbl[:], phs[:].unsqueeze(1).to_broadcast([128, NT, 64]), ALU.add, [tbl, phs], [tbl])
            ts("dve", kk[:], tbl[:], 1.0 / (2 * PI), None, ALU.mult, None, [tbl], [kk])
            cp("dve", ki[:], kk[:], [kk], [ki])
            cp("dve", kk[:], ki[:], [ki], [kk])
            C1 = 6.28125
            C2 = float(2 * np.pi - 6.28125)
            stt(tbl[:], kk[:], -C1, tbl[:], ALU.mult, ALU.add, [kk, tbl], [tbl])
            stt(tbl[:], kk[:], -C2, tbl[:], ALU.mult, ALU.add, [kk, tbl], [tbl])
            ts("dve", kk[:], tbl[:], PI, -2 * PI, ALU.is_gt, ALU.mult, [tbl], [kk])
            tt("dve", tbl[:], tbl[:], kk[:], ALU.add, [tbl, kk], [tbl])
            ts("dve", kk[:], tbl[:], -PI, 2 * PI, ALU.is_lt, ALU.mult, [tbl], [kk])
            tt("dve", tbl[:], tbl[:], kk[:], ALU.add, [tbl, kk], [tbl])
            ts("dve", tbl[:], tbl[:], PI, -PI, ALU.min, ALU.max, [tbl], [tbl])
            act(tbl[:], tbl[:], AF.Sin, [tbl], [tbl])
            P.barrier()
        tap("tbl", tbl[:, NT - 1, :], tbl)

        lbrow = sb("lbrow", [128, 1024], F32, s1)
        omlrow = sb("omlrow", [128, 1024], F32, s1)
        with ExitStack() as s0:
            l1 = sb("l1", [128, 1024], F32, s0)
            dma("sp", lbrow[:], lb_logits[0:1, :].to_broadcast([128, 1024]), [], [lbrow])
            dma("sp", l1[:], lb_logits[1:2, :].to_broadcast([128, 1024]), [], [l1])
            tt("dve", lbrow[:], lbrow[:], l1[:], ALU.subtract, [lbrow, l1], [lbrow])
            act(lbrow[:], lbrow[:], AF.Sigmoid, [lbrow], [lbrow])
            ts("dve", omlrow[:], lbrow[:], -1.0, 1.0, ALU.mult, ALU.add, [lbrow], [omlrow])
            P.barrier()

        g = NS()
        g.__dict__.update(locals())
        if stop_after != "p0":
            phase1(g)
        sO = es.enter_context(ExitStack())
        g.oaT = oaT = sb("oaT", [128, 8, 1024], BF16, sO)
        g.obT = obT = sb("obT", [128, 8, 1024], BF16, sO)
        g.xres_t = T(None, P.buf("xres"))
        g.xres_dep = []
        early = stop_after is not None and (stop_after in ("p0", "p1a") or stop_after.startswith("x"))
        if not early:
            phase1b(g)
        if not early and stop_after != "p1b":
            phase1c(g)
        if not early and stop_after not in ("p1b", "p1c"):
            phase2(g)
        P.barrier()
        sO.close()
        s1.close()
        if not early and stop_after not in ("p1b", "p1c", "p2"):
            if stop_after != "nopeer":
                phase3(g)
            phase4(g)
        P.barrier()
        P.emit()
    return nc


def phase1(g):
    P, sb, mm, tr, act, ts, tt, stt, cp, dma, tap = g.P, g.sb, g.mm, g.tr, g.act, g.ts, g.tt, g.stt, g.cp, g.dma, g.tap
    NT, NTP, bring, s1 = g.NT, g.NTP, g.bring, g.s1
    identb, identf, tbl, lbrow, omlrow, ind, kval = g.identb, g.identf, g.tbl, g.lbrow, g.omlrow, g.ind, g.kval
    Utri, M1, M2, tri = g.Utri, g.M1, g.M2, g.tri
    w_in = g.w_in
    B = g.B

    es = g.es
    S = sb("S", [128, 8, 128], F32, s1)
    kmax2 = sb("kmax2", [128, 8], F32, s1)
    krmax2 = sb("krmax2", [128, 1], F32, s1)
    g.S, g.kmax2, g.krmax2 = S, kmax2, krmax2
    Sh = [T(S.t, P.buf(f"S1a_h{h}")) for h in range(8)]
    g.Sh1a = Sh
    P.op("dve", lambda e: e.memset(S[:], 0.0), [], B([S] + Sh))
    P.op("dve", lambda e: e.memset(kmax2[:], 0.0), [], B([kmax2]))
    P.op("dve", lambda e: e.memset(krmax2[:], 0.0), [], B([krmax2]))
    kts = T(None, P.buf("KT_s"))
    krs = T(None, P.buf("KR_s"))
    vs = T(None, P.buf("V_s"))
    g.kts, g.krs, g.vs = kts, krs, vs

    with ExitStack() as sa:
        wA = sb("wA", [128, 16, 2048], BF16, sa)
        wB = sb("wB", [128, 16, 576], BF16, sa)
        wkv = sb("wkv", [128, 4, 2048], BF16, sa)
        gmix = g.gt["g_mix"]
        with ExitStack() as sl:
            stg = Ring([sb(f"stg{i}", [128, 2048], F32, sl) for i in range(6)])
            for k in range(16):
                s = stg.next()
                dma("sp", s[:, :], w_in[k * 128:(k + 1) * 128, 1024:3072], [], [s])
                g.wcast(wA[:, k, :], s[:, :], gmix[:, k:k + 1], [s, gmix], [wA])
                s = stg.next()
                dma("sp", s[:, 0:576], w_in[k * 128:(k + 1) * 128, 4608:5184], [], [s])
                g.wcast(wB[:, k, :], s[:, 0:576], gmix[:, k:k + 1], [s, gmix], [wB])
            for c in range(4):
                s = stg.next()
                dma("sp", s[:, :], g.w_ukv[c * 128:(c + 1) * 128, :], [], [s])
                g.wcast(wkv[:, c, :], s[:, :], g.gkv[:, c:c + 1], [s, g.gkv], [wkv])
            P.barrier()

        xring = Ring([sb(f"xt{i}", [128, 2048], F32, sa) for i in range(2)])
        xnring = Ring([sb(f"xn{i}", [128, 2048], BF16, sa) for i in range(2)])
        hTring = Ring([sb(f"hT{i}", [128, 16, 128], BF16, sa) for i in range(2)])
        ss = sb("ss", [128, 4], F32, sa)
        tmp = sb("tmp", [128, 4], F32, sa)
        rs = sb("rs", [128, 4], F32, sa)
        fsb = sb("fsb", [128, 1024], F32, sa)
        ksb = sb("ksb", [128, 1024], F32, sa)
        vsb = sb("vsb", [128, 1024], BF16, sa)
        e2 = sb("e2", [128, 1024], F32, sa)
        kd = [sb(f"kd{j}", [128, 1024], BF16, sa) for j in range(2)]
        dec = sb("dec", [128, 8, 2], F32, sa)
        junk = sb("junk", [128, 512], BF16, sa)
        ckvn = sb("ckvn", [128, 512], BF16, sa)
        ckvT = sb("ckvT", [128, 4, 128], BF16, sa)
        knT = Ring([sb(f"knT{i}", [128, 8, 128], BF16, sa) for i in range(2)])
        Vt = Ring([sb(f"Vt{i}", [128, 8, 128], BF16, sa) for i in range(2)])
        sq = sb("sq", [128, 8, 128], F32, sa)
        knb = sb("knb", [128, 8, 128], BF16, sa)
        kn2 = sb("kn2", [128, 8], F32, sa)
        ra = sb("ra", [128, 64], F32, sa)
        rb = sb("rb", [128, 64], F32, sa)
        krp = sb("krp", [128, 64], BF16, sa)
        mkr = sb("mkr", [128, 64], F32, sa)
        krn = sb("krn", [128, 1], F32, sa)
        krT = Ring([sb(f"krT{i}", [64, 128], BF16, sa) for i in range(2)])

        sa_flags = g.stop_after or ""
        if sa_flags == "xw":
            return
        def tile_body(t):
            own = t >= NTP
            xt = xring.next()
            src = g.xown[(t - NTP) * 128:(t - NTP + 1) * 128, :] if own else g.xprev[t * 128:(t + 1) * 128, :]
            dma("sp", xt[:], src, [], [xt])
            xn = xnring.next()
            act(xn[:], xt[:], AF.Square, [xt], [xn, ss], accum=ss[:, 0:1])
            g.rstd_from_ss(T(ss.t[:, 0:1], ss.b), D, T(tmp.t[:, 0:1], tmp.b), T(rs.t[:, 0:1], rs.b), None)
            act(xn[:], xt[:], AF.Copy, [xt, rs], [xn], scale=rs[:, 0:1])
            hT = hTring.next()
            for half in range(2):
                bk = bring.next()
                bv = bk.t[:].bitcast(BF16)
                for kk in range(8):
                    k = half * 8 + kk
                    tr(bv[:, kk * 128:(kk + 1) * 128], xn[:, k * 128:(k + 1) * 128], identb[:], [xn, identb], [bk])
                cp("dve" if half == 0 else "act", hT[:, half * 8:(half + 1) * 8, :],
                   bv.rearrange("p (k t) -> p k t", k=8), [bk], [Acc(hT)])
            yield
            if not own:
                bA = [bring.next() for _ in range(4)]
                for n in range(4):
                    for k in range(16):
                        mm(bA[n][:], hT[:, k, :], wA[:, k, n * 512:(n + 1) * 512], k == 0, k == 15, [hT, wA], [bA[n]])
            bkv = bring.next()
            bkr = bring.next()
            for k in range(16):
                mm(bkv[:], hT[:, k, :], wB[:, k, 0:512], k == 0, k == 15, [hT, wB], [bkv])
            for k in range(16):
                mm(bkr[:, 0:64], hT[:, k, :], wB[:, k, 512:576], k == 0, k == 15, [hT, wB], [bkr])
            act(junk[:], bkv[:], AF.Square, [bkv], [junk, ss], accum=ss[:, 1:2])
            g.rstd_from_ss(T(ss.t[:, 1:2], ss.b), 512, T(tmp.t[:, 1:2], tmp.b), T(rs.t[:, 1:2], rs.b), None)
            act(ckvn[:], bkv[:], AF.Copy, [bkv, rs], [ckvn], scale=rs[:, 1:2])
            cp("dve", mkr[:], bkr[:, 0:64], [bkr], [mkr])
            if sa_flags == "xproj":
                return
            if not own:
                for n in range(2):
                    act(fsb[:, n * 512:(n + 1) * 512], bA[n][:], AF.Sigmoid, [bA[n]], [Acc(fsb)])
                for n in range(2):
                    cp("act", vsb[:, n * 512:(n + 1) * 512], bA[2 + n][:], [bA[2 + n]], [Acc(vsb)])
                tt("dve", fsb[:], fsb[:], omlrow[:], ALU.mult, [fsb, omlrow], [fsb])
                tt("dve", fsb[:], fsb[:], lbrow[:], ALU.add, [fsb, lbrow], [fsb])
                ts("dve", ksb[:], fsb[:], -1.0, 1.0, ALU.mult, ALU.add, [fsb], [ksb])
                act(fsb[:], fsb[:], AF.Ln, [fsb], [fsb])
            if sa_flags == "xhg":
                return
            bk = bring.next()
            bv = bk.t[:].bitcast(BF16)
            for c in range(4):
                tr(bv[:, c * 128:(c + 1) * 128], ckvn[:, c * 128:(c + 1) * 128], identb[:], [ckvn, identb], [bk])
            cp("dve", ckvT[:], bv[:, 0:512].rearrange("p (c t) -> p c t", c=4), [bk], [ckvT])
            if sa_flags == "xkv1":
                return
            wkv4 = wkv.t[:].rearrange("p c (h two d) -> p c h two d", h=8, two=2)
            btk = [bring.next() for _ in range(4)]
            for n in range(4):
                for c in range(4):
                    mm(btk[n][:], ckvT[:, c, :], wkv[:, c, n * 512:(n + 1) * 512], c == 0, c == 3, [ckvT, wkv], [btk[n]])
            if sa_flags == "xkv3a":
                return
            vt = Vt.next()
            for n in range(4):
                bview = btk[n][:].rearrange("p (h two d) -> p h two d", h=2, two=2)
                cp("dve" if n % 2 == 0 else "act", vt[:, 2 * n:2 * n + 2, :], bview[:, :, 1, :], [btk[n]], [Acc(vt)])
                cp("act" if n % 2 == 0 else "dve", knb[:, 2 * n:2 * n + 2, :], bview[:, :, 0, :], [btk[n]], [Acc(knb)])
            bkn = bring.next()
            bknv = bkn.t[:].bitcast(BF16)
            for h in range(8):
                tr(bknv[:, h * 128:(h + 1) * 128], knb[:, h, :], identb[:], [knb, identb], [bkn])
            kn = knT.next()
            cp("act", kn[:], bknv.rearrange("p (h t) -> p h t", h=8), [bkn], [kn])
            dma("pool", g.KT_s[:, t, :, :], kn[:], [kn], [], wacc=[kts])
            if sa_flags in ("xkv3b", "xkv3c"):
                return
            dma("pool", g.V_s[t, :, :, :], vt[:], [vt], [], wacc=[vs])
            if sa_flags == "xkv3":
                return
            tt("dve", sq[:], knb[:], knb[:], ALU.mult, [knb], [sq])
            if sa_flags == "xkv3d":
                return
            P.op("dve", lambda e: e.tensor_reduce(out=kn2[:], in_=sq[:], axis=AX.X, op=ALU.add), B([sq]), B([kn2]))
            tt("dve", kmax2[:], kmax2[:], kn2[:], ALU.max, [kmax2, kn2], [kmax2])
            cs = tbl[:, t, 0:32]
            sn = tbl[:, t, 32:64]
            tt("dve", ra[:].rearrange("p (two d) -> p two d", two=2), mkr[:].rearrange("p (two d) -> p two d", two=2),
               cs.unsqueeze(1).to_broadcast([128, 2, 32]), ALU.mult, [mkr, tbl], [ra])
            tt("dve", rb[:, 0:32], mkr[:, 32:64], sn, ALU.mult, [mkr, tbl], [Acc(rb)])
            tt("dve", rb[:, 32:64], mkr[:, 0:32], sn, ALU.mult, [mkr, tbl], [Acc(rb)])
            tt("dve", krp[:, 0:32], ra[:, 0:32], rb[:, 0:32], ALU.subtract, [ra, rb], [Acc(krp)])
            tt("dve", krp[:, 32:64], ra[:, 32:64], rb[:, 32:64], ALU.add, [ra, rb], [Acc(krp)])
            act(ra[:], krp[:], AF.Square, [krp], [ra, krn], accum=krn[:])
            tt("dve", krmax2[:], krmax2[:], krn[:], ALU.max, [krmax2, krn], [krmax2])
            if sa_flags == "xkv4":
                return
            bk = bring.next()
            bv = bk.t[:].bitcast(BF16)
            tr(bv[0:64, 0:128], krp[:, :], identb[:], [krp, identb], [bk])
            kr = krT.next()
            cp("act", kr[:], bv[0:64, 0:128], [bk], [kr])
            dma("pool", g.KR_s[:, t * 128:(t + 1) * 128], kr[:], [kr], [], wacc=[krs])
            if not own:
                bd = [bring.next() for _ in range(2)]
                for n in range(2):
                    mm(bd[n][:], M2, fsb[:, n * 512:(n + 1) * 512], True, True, [tri, fsb], [bd[n]])
                    act(e2[:, n * 512:(n + 1) * 512], bd[n][:], AF.Exp, [bd[n]], [Acc(e2)])
                bl = bring.next()
                for h in range(8):
                    mm(bl[:, h * 2:h * 2 + 2], fsb[:, h * 128:(h + 1) * 128], ind[:], True, True, [fsb, ind], [bl])
                act(dec[:].rearrange("p h j -> p (h j)"), bl[:, 0:16], AF.Exp, [bl], [dec])
                for j in range(2):
                    stt(kd[j][:], ksb[:], ind[:, j:j + 1], e2[:], ALU.mult, ALU.mult, [ksb, ind, e2], [kd[j]])
                for j in range(2):
                    bs = [bring.next() for _ in range(2)]
                    for h in range(8):
                        mm(bs[h // 4][:, (h % 4) * 128:(h % 4 + 1) * 128], kd[j][:, h * 128:(h + 1) * 128],
                           vsb[:, h * 128:(h + 1) * 128], True, True, [kd[j], vsb], [bs[h // 4]])
                    for h in range(8):
                        stt(S[:, h, :], S[:, h, :], dec[:, h, j:j + 1], bs[h // 4][:, (h % 4) * 128:(h % 4 + 1) * 128],
                            ALU.mult, ALU.add, [Sh[h], dec, bs[h // 4]], [Sh[h]])
        tiles = list(range(NT)) if not sa_flags.startswith("x") else [0, NT - 1]
        gens = [tile_body(t) for t in tiles]
        next(gens[0])
        for i_, gen in enumerate(gens):
            if i_ + 1 < len(gens):
                next(gens[i_ + 1])
            for _ in gen:
                pass
        if "S" in g.tap_out:
            g.dma("pool", g.tap_out["S"], S[:].rearrange("p h d -> p (h d)"), Sh, [])
        tap("kmax2", kmax2[:], kmax2)
        P.barrier()


def phase1b(g):
    P, sb, mm, tr, act, ts, tt, stt, cp, dma, tap = g.P, g.sb, g.mm, g.tr, g.act, g.ts, g.tt, g.stt, g.cp, g.dma, g.tap
    NT, NTP, bring, banks = g.NT, g.NTP, g.bring, g.banks
    identb, identf, tbl, lbrow, omlrow, ind, kval, causal = g.identb, g.identf, g.tbl, g.lbrow, g.omlrow, g.ind, g.kval, g.causal
    Utri, M1, M2, tri = g.Utri, g.M1, g.M2, g.tri
    w_in, S, B = g.w_in, g.S, g.B
    gmix = g.gt["g_mix"]
    oaT, obT = g.oaT, g.obT

    with ExitStack() as sa:
        hTown = sb("hTown", [128, 16, 1024], BF16, sa)
        ss = sb("ss1", [128, 4], F32, sa)
        tmp = sb("tmp1", [128, 4], F32, sa)
        rs = sb("rs1", [128, 4], F32, sa)
        with ExitStack() as sl:
            xring = Ring([sb(f"xo{i}", [128, 2048], F32, sl) for i in range(2)])
            xnring = Ring([sb(f"xno{i}", [128, 2048], BF16, sl) for i in range(2)])
            for j in range(8):
                xt = xring.next()
                dma("sp", xt[:], g.xown[j * 128:(j + 1) * 128, :], [], [xt])
                xn = xnring.next()
                act(xn[:], xt[:], AF.Square, [xt], [xn, ss], accum=ss[:, 0:1])
                g.rstd_from_ss(T(ss.t[:, 0:1], ss.b), D, T(tmp.t[:, 0:1], tmp.b), T(rs.t[:, 0:1], rs.b), None)
                act(xn[:], xt[:], AF.Copy, [xt, rs], [xn], scale=rs[:, 0:1])
                for half in range(2):
                    bk = bring.next()
                    bv = bk.t[:].bitcast(BF16)
                    for kk in range(8):
                        k = half * 8 + kk
                        tr(bv[:, kk * 128:(kk + 1) * 128], xn[:, k * 128:(k + 1) * 128], identb[:], [xn, identb], [bk])
                    cp("dve" if half == 0 else "act", hTown[:, half * 8:(half + 1) * 8, j * 128:(j + 1) * 128],
                       bv.rearrange("p (k t) -> p k t", k=8), [bk], [hTown])
            P.barrier()

        with ExitStack() as sh:
            whr = Ring([sb(f"wh{i}", [128, 16, 512], BF16, sh) for i in range(4)])
            stg = Ring([sb(f"stgh{i}", [128, 4, 128], F32, sh) for i in range(6)])
            ND = 4

            def RN(name, shape, dt):
                return Ring([sb(f"{name}{i}", shape, dt, sh) for i in range(ND)])
            f_r, k_r, q_r = RN("f_", [128, 128], F32), RN("k_", [128, 128], F32), RN("q_", [128, 128], F32)
            v_r, gate_r = RN("v_", [128, 128], BF16), RN("gate", [128, 128], F32)
            e1_r, en1_r, eb_r, e2_r = RN("e1", [128, 128], F32), RN("en1", [128, 128], F32), RN("eb", [128, 128], F32), RN("e2b", [128, 128], F32)
            dec_r = RN("decb", [128, 2], F32)
            qin_r, kin_r, qbp_r = RN("qin", [128, 128], BF16), RN("kin", [128, 128], BF16), RN("qbp", [128, 192], BF16)
            kd_r = [RN(f"kdb{i}", [128, 128], BF16) for i in range(2)]
            atm_r, sb0_r, sb1_r = RN("atm", [128, 128], BF16), RN("sb0", [128, 128], BF16), RN("sb1", [128, 128], BF16)
            on_r, onb_r = RN("on", [128, 128], F32), RN("onb", [128, 128], BF16)
            ss_r, tmp_r, rs_r = RN("ssh", [128, 1], F32), RN("tmph", [128, 1], F32), RN("rsh", [128, 1], F32)
            for qb_ in qbp_r.tiles:
                P.op("dve", lambda e, qb_=qb_: e.memset(qb_[:], 0.0), [], B([qb_]))
            Sh = [T(S.t, P.buf(f"S_h{h}")) for h in range(8)]
            w4 = w_in[:, 0:4096].rearrange("p (s c) -> p s c", s=4)

            def load_head(h):
                wh = whr.next()
                for k in range(16):
                    s = stg.next()
                    dma("sp", s[:], w4[k * 128:(k + 1) * 128, :, h * 128:(h + 1) * 128], [], [s])
                    g.wcast(wh[:, k, :], s[:].rearrange("p s c -> p (s c)"), gmix[:, k:k + 1], [s, gmix], [wh],
                            engines=("act", "dve", "pool", "dve", "act"))
                return wh

            def body(h, j, wh):
                lb_h = lbrow[:, h * 128:(h + 1) * 128]
                oml_h = omlrow[:, h * 128:(h + 1) * 128]
                S_ = Sh[h]
                f_, k_, q_, v_, gate = f_r.next(), k_r.next(), q_r.next(), v_r.next(), gate_r.next()
                e1, en1, eb, e2, dec = e1_r.next(), en1_r.next(), eb_r.next(), e2_r.next(), dec_r.next()
                qin, kin, qbp, atm = qin_r.next(), kin_r.next(), qbp_r.next(), atm_r.next()
                kd = [kd_r[0].next(), kd_r[1].next()]
                sb0, sb1, on, onb = sb0_r.next(), sb1_r.next(), on_r.next(), onb_r.next()
                ss, tmp, rs = ss_r.next(), tmp_r.next(), rs_r.next()
                bp = bring.next()
                for k in range(16):
                    mm(bp[:], hTown[:, k, j * 128:(j + 1) * 128], wh[:, k, :], k == 0, k == 15, [hTown, wh], [bp])
                yield
                act(f_[:], bp[:, 128:256], AF.Sigmoid, [bp], [f_])
                act(gate[:], bp[:, 384:512], AF.Silu, [bp], [gate])
                cp("act", v_[:], bp[:, 256:384], [bp], [v_])
                cp("act", q_[:], bp[:, 0:128], [bp], [q_])
                tt("dve", f_[:], f_[:], oml_h, ALU.mult, [f_, omlrow], [f_])
                tt("dve", f_[:], f_[:], lb_h, ALU.add, [f_, lbrow], [f_])
                ts("dve", k_[:], f_[:], -1.0, 1.0, ALU.mult, ALU.add, [f_], [k_])
                act(f_[:], f_[:], AF.Ln, [f_], [f_])
                yield
                bt = bring.next()
                P.op("pe", lambda e: e.transpose(out=bt[:, 0:128], in_=q_[:], identity=identf[:]), B([q_, identf]), B([bt]))
                P.op("pe", lambda e: e.transpose(out=bt[:, 128:256], in_=k_[:], identity=identf[:]), B([k_, identf]), B([bt]))
                bc = bring.next()
                mm(bc[:, 0:128], f_[:], M1, True, True, [f_, tri], [bc])
                mm(bc[:, 128:256], f_[:], Utri, True, True, [f_, tri], [bc])
                mm(bc[:, 256:384], M2, f_[:], True, True, [f_, tri], [bc])
                mm(bc[:, 384:386], f_[:], ind[:], True, True, [f_, ind], [bc])
                yield
                act(e1[:], bc[:, 0:128], AF.Exp, [bc], [e1])
                act(en1[:], bc[:, 0:128], AF.Exp, [bc], [en1], scale=-1.0)
                act(eb[:], bc[:, 128:256], AF.Exp, [bc], [eb])
                act(e2[:], bc[:, 256:384], AF.Exp, [bc], [e2])
                act(dec[:], bc[:, 384:386], AF.Exp, [bc], [dec])
                tt("dve", qin[:], bt[:, 0:128], e1[:], ALU.mult, [bt, e1], [qin])
                tt("dve", kin[:], bt[:, 128:256], en1[:], ALU.mult, [bt, en1], [kin])
                tt("dve", qbp[:, 0:64], bt[:, 0:64], eb[:, 0:64], ALU.mult, [bt, eb], [qbp])
                tt("dve", qbp[:, 128:192], bt[:, 64:128], eb[:, 64:128], ALU.mult, [bt, eb], [qbp])
                for jj in range(2):
                    stt(kd[jj][:], k_[:], ind[:, jj:jj + 1], e2[:], ALU.mult, ALU.mult, [k_, ind, e2], [kd[jj]])
                yield
                ba = bring.next()
                mm(ba[:, 0:128], kin[:], qin[:], True, True, [kin, qin], [ba])
                bs = bring.next()
                cp("act", sb0[:], S[:, h, :], [S_], [sb0])
                mm(bs[:, 0:128], kd[0][:], v_[:], True, True, [kd[0], v_], [bs])
                mm(bs[:, 128:256], kd[1][:], v_[:], True, True, [kd[1], v_], [bs])
                yield
                tt("dve", atm[:], ba[:, 0:128], Utri, ALU.mult, [ba, tri], [atm])
                stt(S[:, h, :], S[:, h, :], dec[:, 0:1], bs[:, 0:128], ALU.mult, ALU.add, [S_, dec, bs], [S_])
                cp("act", sb1[:], S[:, h, :], [S_], [sb1])
                stt(S[:, h, :], S[:, h, :], dec[:, 1:2], bs[:, 128:256], ALU.mult, ALU.add, [S_, dec, bs], [S_])
                yield
                bo = bring.next()
                mm(bo[:, 0:128], atm[:], v_[:], True, False, [atm, v_], [bo])
                mm(bo[:, 0:128], qbp[:, 0:128], sb0[:], False, False, [qbp, sb0], [bo])
                mm(bo[:, 0:128], qbp[:, 64:192], sb1[:], False, True, [qbp, sb1], [bo])
                yield
                act(on[:], bo[:, 0:128], AF.Square, [bo], [on, ss], accum=ss[:, 0:1])
                g.rstd_from_ss(ss, 128, tmp, rs, None)
                act(on[:], bo[:, 0:128], AF.Copy, [bo, rs], [on], scale=rs[:, 0:1])
                tt("dve", onb[:], on[:], gate[:], ALU.mult, [on, gate], [onb])
                yield
                bz = bring.next()
                bzv = bz.t[:].bitcast(BF16)
                tr(bzv[:, 0:128], onb[:], identb[:], [onb, identb], [bz])
                ts("dve", oaT[:, h, j * 128:(j + 1) * 128], bzv[:, 0:128], g.ghg[:, h:h + 1], None, ALU.mult, None,
                   [bz, g.ghg], [oaT])

            for hp in range(2):
                hs = tuple(range(4 * hp, 4 * hp + 4))
                whs = [load_head(h) for h in hs]
                for j in range(8):
                    alive = [body(h, j, wh) for h, wh in zip(hs, whs)]
                    while alive:
                        for gen in list(alive):
                            try:
                                next(gen)
                            except StopIteration:
                                alive.remove(gen)
            P.barrier()
        tap("oaT", oaT[:].rearrange("p h t -> p (h t)"), oaT)


def phase1c(g):
    P, sb, mm, tr, act, ts, tt, stt, cp, dma, tap = g.P, g.sb, g.mm, g.tr, g.act, g.ts, g.tt, g.stt, g.cp, g.dma, g.tap
    NT, NTP, bring, banks = g.NT, g.NTP, g.bring, g.banks
    identb, identf, tbl, kval, causal = g.identb, g.identf, g.tbl, g.kval, g.causal
    w_in, B = g.w_in, g.B
    gmix = g.gt["g_mix"]
    obT = g.obT
    kts, krs, vs = g.kts, g.krs, g.vs
    SCALE = float(1.0 / np.sqrt(192.0))

    with ExitStack() as sa:
        qnT = sb("qnT", [128, 8, 1024], BF16, sa)
        qra = sb("qra", [65, 8, 1024], BF16, sa)
        kmb = sb("kmb", [128, 8], F32, sa)
        ss = sb("ss2", [128, 4], F32, sa)
        tmp = sb("tmp2", [128, 4], F32, sa)
        rs = sb("rs2", [128, 4], F32, sa)
        ones = sb("ones", [128, 128], BF16, sa)
        P.op("dve", lambda e: e.memset(ones[:], 1.0), [], B([ones]))
        with ExitStack() as sl:
            km = sb("km", [128, 128], F32, sl)
            P.op("dve", lambda e: e.memset(km[:], 0.0), [], B([km]))
            kmT = sb("kmT", [8, 128], F32, sl)
            kmc = sb("kmc", [8, 1], F32, sl)
            dg = sb("dg", [8, 8], F32, sl)
            onesf = sb("onesf", [8, 128], F32, sl)
            ts("dve", km[:, 0:8], g.kmax2[:], g.krmax2[:, 0:1], None, ALU.add, None, [g.kmax2, g.krmax2, km], [km])
            bk = bring.next()
            P.op("pe", lambda e, bk=bk: e.transpose(out=bk[:, 0:128], in_=km[:], identity=identf[:]), B([km, identf]), B([bk]))
            cp("dve", kmT[:], bk[0:8, 0:128], [bk], [kmT])
            tap("km", km[:, 0:8], km)
            tap("kmT", kmT[:], kmT)
            P.op("dve", lambda e: e.tensor_reduce(out=kmc[:], in_=kmT[:], axis=AX.X, op=ALU.max), B([kmT]), B([kmc]))
            act(kmc[:], kmc[:], AF.Sqrt, [kmc], [kmc])
            ts("dve", dg[:], identf[0:8, 0:8], kmc[:, 0:1], None, ALU.mult, None, [identf, kmc], [dg])
            P.op("dve", lambda e: e.memset(onesf[:], 1.0), [], B([onesf]))
            bk2 = bring.next()
            mm(bk2[:, 0:8], onesf[:], dg[:], True, True, [onesf, dg], [bk2])
            cp("dve", kmb[:], bk2[:, 0:8], [bk2], [kmb])
            P.barrier()
        tap("kmb", kmb[:], kmb)

        with ExitStack() as sq_:
            hTown = sb("hTown2", [128, 16, 128], BF16, sq_)
            xt = sb("xq", [128, 2048], F32, sq_)
            xn = sb("xnq", [128, 2048], BF16, sq_)
            wq = sb("wq", [128, 16, 512], BF16, sq_)
            wuq = sb("wuq", [128, 4, 1536], BF16, sq_)
            stg = Ring([sb(f"stgq{i}", [128, 1536], F32, sq_) for i in range(4)])
            cqn = sb("cqn", [128, 512], BF16, sq_)
            cqT = sb("cqT", [128, 4, 128], BF16, sq_)
            sqT = sb("sqT", [128, 8, 128], BF16, sq_)
            junk = sb("junkq", [128, 512], BF16, sq_)
            qr2 = sb("qr2", [128, 8, 64], F32, sq_)
            ra = sb("raq", [128, 8, 64], F32, sq_)
            rb = sb("rbq", [128, 8, 64], F32, sq_)
            qrp = sb("qrp", [128, 8, 64], BF16, sq_)
            qn2 = sb("qn2", [128, 8], F32, sq_)
            qn2b = sb("qn2b", [128, 128], F32, sq_)
            P.op("dve", lambda e: e.memset(qn2b[:], 0.0), [], B([qn2b]))
            shT = sb("shT", [8, 128], BF16, sq_)
            for k in range(16):
                s = stg.next()
                dma("sp", s[:, 0:512], w_in[k * 128:(k + 1) * 128, 4096:4608], [], [s])
                g.wcast(wq[:, k, :], s[:, 0:512], gmix[:, k:k + 1], [s, gmix], [wq])
            for c in range(4):
                s = stg.next()
                dma("sp", s[:, :], g.w_uq[c * 128:(c + 1) * 128, :], [], [s])
                g.wcast(wuq[:, c, :], s[:, :], g.gq[:, c:c + 1], [s, g.gq], [wuq])
            wuq3 = wuq.t[:].rearrange("p c (h e) -> p c h e", h=8)
            for j in range(8):
                t = NTP + j
                dma("sp", xt[:], g.xown[j * 128:(j + 1) * 128, :], [], [xt])
                act(xn[:], xt[:], AF.Square, [xt], [xn, ss], accum=ss[:, 0:1])
                g.rstd_from_ss(T(ss.t[:, 0:1], ss.b), D, T(tmp.t[:, 0:1], tmp.b), T(rs.t[:, 0:1], rs.b), None)
                act(xn[:], xt[:], AF.Copy, [xt, rs], [xn], scale=rs[:, 0:1])
                for half in range(2):
                    bk = bring.next()
                    bv = bk.t[:].bitcast(BF16)
                    for kk in range(8):
                        k = half * 8 + kk
                        tr(bv[:, kk * 128:(kk + 1) * 128], xn[:, k * 128:(k + 1) * 128], identb[:], [xn, identb], [bk])
                    cp("dve" if half == 0 else "act", hTown[:, half * 8:(half + 1) * 8, :],
                       bv.rearrange("p (k t) -> p k t", k=8), [bk], [hTown])
                bq = bring.next()
                for k in range(16):
                    mm(bq[:], hTown[:, k, :], wq[:, k, :], k == 0, k == 15, [hTown, wq], [bq])
                act(junk[:], bq[:], AF.Square, [bq], [junk, ss], accum=ss[:, 1:2])
                g.rstd_from_ss(T(ss.t[:, 1:2], ss.b), 512, T(tmp.t[:, 1:2], tmp.b), T(rs.t[:, 1:2], rs.b), None)
                act(cqn[:], bq[:], AF.Copy, [bq, rs], [cqn], scale=rs[:, 1:2])
                bk = bring.next()
                bv = bk.t[:].bitcast(BF16)
                for c in range(4):
                    tr(bv[:, c * 128:(c + 1) * 128], cqn[:, c * 128:(c + 1) * 128], identb[:], [cqn, identb], [bk])
                cp("dve", cqT[:], bv[:, 0:512].rearrange("p (c t) -> p c t", c=4), [bk], [cqT])
                bqn = [bring.next() for _ in range(2)]
                for h in range(8):
                    for c in range(4):
                        mm(bqn[h // 4][:, (h % 4) * 128:(h % 4 + 1) * 128], wuq3[:, c, h, 0:128], cqT[:, c, :], c == 0, c == 3,
                           [wuq, cqT], [bqn[h // 4]])
                for n in range(2):
                    cp("dve", qnT[:, n * 4:(n + 1) * 4, j * 128:(j + 1) * 128], bqn[n][:].rearrange("p (h t) -> p h t", h=4),
                       [bqn[n]], [qnT])
                    act(sqT[:, n * 4:(n + 1) * 4, :], bqn[n][:].rearrange("p (h t) -> p h t", h=4), AF.Square, [bqn[n]], [sqT])
                bqr = bring.next()
                for c in range(4):
                    mm(bqr[:].rearrange("p (h e) -> p h e", h=8), cqT[:, c, :], wuq3[:, c, :, 128:192], c == 0, c == 3,
                       [cqT, wuq], [bqr])
                bqr3 = bqr[:].rearrange("p (h e) -> p h e", h=8)
                bqr4 = bqr[:].rearrange("p (h two d) -> p h two d", h=8, two=2)
                bn = bring.next()
                for h in range(8):
                    mm(bn[:, h:h + 1], sqT[:, h, :], ones[:, 0:1], True, True, [sqT, ones], [bn])
                act(qr2[:], bqr3, AF.Square, [bqr], [qr2])
                P.op("dve", lambda e: e.tensor_reduce(out=qn2[:], in_=qr2[:], axis=AX.X, op=ALU.add), B([qr2]), B([qn2]))
                tt("dve", qn2[:], qn2[:], bn[:, 0:8], ALU.add, [qn2, bn], [qn2])
                act(qn2[:], qn2[:], AF.Sqrt, [qn2], [qn2])
                stt(qn2b[:, 0:8], qn2[:], -1.0, kmb[:], ALU.mult, ALU.mult, [qn2, kmb, qn2b], [qn2b])
                bsh = bring.next()
                P.op("pe", lambda e, bsh=bsh: e.transpose(out=bsh[:, 0:128], in_=qn2b[:], identity=identf[:]),
                     B([qn2b, identf]), B([bsh]))
                cp("dve", shT[:], bsh[0:8, 0:128], [bsh], [shT])
                dma("pool", qra[64:65, :, j * 128:(j + 1) * 128], shT[:], [shT], [], wacc=[qra])
                cs = tbl[:, t, 0:32]
                sn = tbl[:, t, 32:64]
                tt("dve", ra[:].rearrange("p h (two d) -> p h two d", two=2), bqr4,
                   cs.unsqueeze(1).unsqueeze(1).to_broadcast([128, 8, 2, 32]), ALU.mult, [bqr, tbl], [ra])
                snb = sn.unsqueeze(1).to_broadcast([128, 8, 32])
                tt("dve", rb[:, :, 0:32], bqr3[:, :, 32:64], snb, ALU.mult, [bqr, tbl], [rb])
                tt("dve", rb[:, :, 32:64], bqr3[:, :, 0:32], snb, ALU.mult, [bqr, tbl], [rb])
                tt("dve", qrp[:, :, 0:32], ra[:, :, 0:32], rb[:, :, 0:32], ALU.subtract, [ra, rb], [qrp])
                tt("dve", qrp[:, :, 32:64], ra[:, :, 32:64], rb[:, :, 32:64], ALU.add, [ra, rb], [qrp])
                bk = bring.next()
                bv = bk.t[:].bitcast(BF16)
                for h in range(8):
                    tr(bv[0:64, h * 128:(h + 1) * 128], qrp[:, h, :], identb[:], [qrp, identb], [bk])
                cp("act", qra[0:64, :, j * 128:(j + 1) * 128], bv[0:64, :].rearrange("p (h t) -> p h t", h=8), [bk], [qra])
            P.barrier()
        tap("qnT", qnT[:].rearrange("p h t -> p (h t)"), qnT)
        tap("qra", qra[:].rearrange("p h t -> p (h t)"), qra, lambda d: d[0:65, :])

        with ExitStack() as st_:
            KR = sb("KR", [65, NT * 128], BF16, st_)
            KTr = Ring([sb(f"KTh{i}", [128, NT, 128], BF16, st_) for i in range(2)])
            Vr = Ring([sb(f"Vh{i}", [128, NT, 129], BF16, st_) for i in range(2)])
            PTr = Ring([sb(f"PT{i}", [128, 512], BF16, st_) for i in range(3)])
            rz = sb("rz", [128, 1], F32, st_)
            ob = sb("ob", [128, 128], BF16, st_)
            dma("sp", KR[0:64, :], g.KR_s, [krs], [KR])
            P.op("dve", lambda e: e.memset(KR[64:65, :], 1.0), [], B([KR]))
            acc = banks[0:4]
            sring = Ring(banks[4:8])
            V4 = g.V_s.rearrange("t p h d -> p t h d")
            for h in range(8):
                KTh = KTr.next()
                Vh = Vr.next()
                dma("sp", KTh[:], g.KT_s[:, :, h, :], [kts], [KTh])
                dma("sp", Vh[:, :, 0:128], V4[:, :, h, :], [vs], [Vh])
                cp("dve", Vh[:, :, 128:129], kval[:].unsqueeze(2), [kval, Vh], [Vh])
                for qt in range(2):
                    nkb = NTP + 4 * qt + 4
                    def issueS(kb, qt=qt, h=h, KTh=KTh):
                        dg_i = kb - (NTP + 4 * qt)
                        q0 = max(0, dg_i) * 128
                        c0 = qt * 512 + q0
                        c1 = qt * 512 + 512
                        st = sring.next()
                        mm(st[:, q0:512], KTh[:, kb, :], qnT[:, h, c0:c1], True, False, [KTh, qnT], [st])
                        mm(st[:, q0:512], KR[0:65, kb * 128:(kb + 1) * 128], qra[0:65, h, c0:c1], False, True, [KR, qra], [st])
                        return st, q0, dg_i

                    pend = issueS(0)
                    for kb in range(nkb):
                        st, q0, dg_i = pend
                        if kb + 1 < nkb:
                            pend = issueS(kb + 1)
                        PT = PTr.next()
                        act(PT[:, q0:512], st[:, q0:512], AF.Exp, [st], [PT], scale=SCALE)
                        if dg_i >= 0:
                            tt("dve", PT[:, q0:q0 + 128], PT[:, q0:q0 + 128], causal[:], ALU.mult, [PT, causal], [PT])
                        for jq in range(max(0, dg_i), 4):
                            last = NTP + 4 * qt + jq
                            mm(acc[jq][:, 0:129], PT[:, jq * 128:(jq + 1) * 128], Vh[:, kb, :], kb == 0, kb == last,
                               [PT, Vh], [acc[jq]])
                    for jq in range(4):
                        P.op("dve", lambda e, jq=jq: e.reciprocal(out=rz[:], in_=acc[jq][:, 128:129]), B([acc[jq]]), B([rz]))
                        act(ob[:], acc[jq][:, 0:128], AF.Copy, [acc[jq], rz], [ob], scale=rz[:, 0:1])
                        bz = sring.next()
                        bzv = bz.t[:].bitcast(BF16)
                        tr(bzv[:, 0:128], ob[:], identb[:], [ob, identb], [bz])
                        col = qt * 512 + jq * 128
                        cp("dve", obT[:, h, col:col + 128], bzv[:, 0:128], [bz], [obT])
            P.barrier()
        tap("obT", obT[:].rearrange("p h t -> p (h t)"), obT)


def prep_inputs(inp, NPREV=7, ncores=NCORES, lite=False):
    f = lambda a: np.ascontiguousarray(np.asarray(a))
    X = f(inp["x"])[0]
    pos = f(inp["positions"])[0].astype(np.int32)
    NT = NPREV * 8 + 8
    shared = {
        "w_in": f(inp["w_in"])[0],
        "g_mix": f(f(inp["norm_mix"])[0].reshape(16, 128).T),
        "g_ffn": f(f(inp["norm_ffn"])[0].reshape(16, 128).T),
        "g_ple": f(f(inp["norm_ple"])[0].reshape(16, 128).T),
        "g_final": f(inp["norm_final"]).reshape(1, D),
        "lbT": f(f(inp["lb_logits"]).reshape(2, 8, 128).transpose(2, 0, 1).reshape(128, 16)),
        "lb_logits": f(inp["lb_logits"]),
        "g_hg": f(f(inp["hg_norm"])[0].reshape(8, 128).T),
        "g_q": f(f(inp["mla_q_norm"])[0].reshape(4, 128).T),
        "g_kv": f(f(inp["mla_kv_norm"])[0].reshape(4, 128).T),
        "w_uq": f(inp["w_uq"])[0], "w_ukv": f(inp["w_ukv"])[0],
        "w_a": f(inp["w_a"])[0], "w_b": f(inp["w_b"])[0], "w_o": f(inp["w_o"])[0],
        "peer_wq": f(inp["peer_wq"])[0],
        "k1T": f(f(inp["peer_k1"])[0].T), "k2T": f(f(inp["peer_k2"])[0].T),
        "peer_uT": f(f(inp["peer_u"])[0].T), "peer_v": f(inp["peer_v"])[0],
        "w_pg": f(inp["w_pg"])[0], "w_pe": f(inp["w_pe"])[0],
    }
    if lite:
        shared["peer_uT"] = f(shared["peer_uT"][:, :128])
        shared["peer_v"] = f(shared["peer_v"][:128])
    shared.update(host_consts())
    maps = []
    for c in range(ncores):
        xprev = np.zeros((max(NPREV, 1) * 1024, D), np.float32)
        pall = np.zeros((NT * 128,), np.int32)
        valid = np.zeros((NT * 128,), np.float32)
        for s_ in range(NPREV):
            blk = c - NPREV + s_
            if blk >= 0:
                xprev[s_ * 1024:(s_ + 1) * 1024] = X[blk * 1024:(blk + 1) * 1024]
                pall[s_ * 1024:(s_ + 1) * 1024] = pos[blk * 1024:(blk + 1) * 1024]
                valid[s_ * 1024:(s_ + 1) * 1024] = 1.0
        pall[NPREV * 1024:] = pos[c * 1024:(c + 1) * 1024]
        valid[NPREV * 1024:] = 1.0
        m = dict(shared)
        m["xprev"] = xprev
        m["xown"] = f(X[c * 1024:(c + 1) * 1024])
        m["posT"] = f(pall.reshape(NT, 128).T)
        m["kvalid"] = f(valid.reshape(NT, 128).T)
        m["p_own"] = f(f(inp["p"])[0, 0, c * 1024:(c + 1) * 1024, :])
        maps.append(m)
    return maps


def own_hT(g, dst, st, src_rows, gain=None, nt=8, tag="h"):
    P, sb, tr, act, ts, cp, dma, bring, B = g.P, g.sb, g.tr, g.act, g.ts, g.cp, g.dma, g.bring, g.B
    xring = Ring([sb(f"{tag}x{i}", [128, 2048], F32, st) for i in range(2)])
    xnring = Ring([sb(f"{tag}xn{i}", [128, 2048], BF16, st) for i in range(2)])
    ss = sb(f"{tag}ss", [128, 1], F32, st)
    tmp = sb(f"{tag}tmp", [128, 1], F32, st)
    rs = sb(f"{tag}rs", [128, 1], F32, st)
    for j in range(nt):
        xt = xring.next()
        dma("sp", xt[:], src_rows(j), g.xres_dep, [xt])
        xn = xnring.next()
        act(xn[:], xt[:], AF.Square, [xt], [xn, ss], accum=ss[:, 0:1])
        g.rstd_from_ss(ss, D, tmp, rs, None)
        act(xn[:], xt[:], AF.Copy, [xt, rs], [xn], scale=rs[:, 0:1])
        for half in range(2):
            bk = bring.next()
            bv = bk.t[:].bitcast(BF16)
            for kk in range(8):
                k = half * 8 + kk
                tr(bv[:, kk * 128:(kk + 1) * 128], xn[:, k * 128:(k + 1) * 128], g.identb[:], [xn, g.identb], [bk])
            if gain is None:
                cp("dve" if half == 0 else "act", dst[:, half * 8:(half + 1) * 8, j * 128:(j + 1) * 128],
                   bv.rearrange("p (k t) -> p k t", k=8), [bk], [Acc(dst)])
            else:
                for kk in range(8):
                    k = half * 8 + kk
                    ts("dve", dst[:, k, j * 128:(j + 1) * 128], bv[:, kk * 128:(kk + 1) * 128], gain[:, k:k + 1], None,
                       ALU.mult, None, [bk, gain], [Acc(dst)])


def load_w(g, dst, src_fn, nk, width, gain, stg):
    for k in range(nk):
        s = stg.next()
        g.dma("sp", s[:, 0:width], src_fn(k), [], [s])
        if gain is None:
            g.wcast(dst[:, k, 0:width], s[:, 0:width], None, [s], [dst])
        else:
            g.wcast(dst[:, k, 0:width], s[:, 0:width], gain[:, k:k + 1], [s, gain], [dst])


def phase2(g):
    P, sb, mm, tr, act, ts, tt, stt, cp, dma, tap = g.P, g.sb, g.mm, g.tr, g.act, g.ts, g.tt, g.stt, g.cp, g.dma, g.tap
    bring, B, w_in = g.bring, g.B, g.w_in
    gmix = g.gt["g_mix"]
    oaT, obT = g.oaT, g.obT
    with ExitStack() as sa:
        hT = sb("hTm", [128, 16, 1024], BF16, sa)
        ysb = sb("ysb", [128, 8, 2048], BF16, sa)
        with ExitStack() as sl:
            own_hT(g, hT, sl, lambda j: g.xown[j * 128:(j + 1) * 128, :], None, 8, "m")
            P.barrier()
        with ExitStack() as sw:
            CW = 256
            wga_r = Ring([sb(f"wga{i}", [128, 16, CW], BF16, sw) for i in range(2)])
            wgb_r = Ring([sb(f"wgb{i}", [128, 16, CW], BF16, sw) for i in range(2)])
            wa_r = Ring([sb(f"wa{i}", [128, 8, CW], BF16, sw) for i in range(2)])
            wb_r = Ring([sb(f"wb{i}", [128, 8, CW], BF16, sw) for i in range(2)])
            stg = Ring([sb(f"stgm{i}", [128, CW], F32, sw) for i in range(4)])
            sga_r = Ring([sb(f"sga{i}", [128, CW], F32, sw) for i in range(2)])
            sgb_r = Ring([sb(f"sgb{i}", [128, CW], F32, sw) for i in range(2)])
            y1_r = Ring([sb(f"y1{i}", [128, CW], F32, sw) for i in range(2)])
            for n in range(D // CW):
                c0, c1 = n * CW, (n + 1) * CW
                wga, wgb, wa, wb = wga_r.next(), wgb_r.next(), wa_r.next(), wb_r.next()
                load_w(g, wga, lambda k: w_in[k * 128:(k + 1) * 128, 5184 + c0:5184 + c1], 16, CW, gmix, stg)
                load_w(g, wgb, lambda k: w_in[k * 128:(k + 1) * 128, 7232 + c0:7232 + c1], 16, CW, gmix, stg)
                load_w(g, wa, lambda k: g.w_a[k * 128:(k + 1) * 128, c0:c1], 8, CW, None, stg)
                load_w(g, wb, lambda k: g.w_b[k * 128:(k + 1) * 128, c0:c1], 8, CW, None, stg)
                for j in range(8):
                    tok = slice(j * 128, (j + 1) * 128)
                    bga, bgb = bring.next(), bring.next()
                    ba, bb = bga, bgb
                    for k in range(16):
                        mm(bga[:, 0:CW], hT[:, k, tok], wga[:, k, :], k == 0, k == 15, [hT, wga], [bga])
                    for h in range(8):
                        mm(ba[:, CW:2 * CW], oaT[:, h, tok], wa[:, h, :], h == 0, h == 7, [oaT, wa], [ba])
                    for k in range(16):
                        mm(bgb[:, 0:CW], hT[:, k, tok], wgb[:, k, :], k == 0, k == 15, [hT, wgb], [bgb])
                    for h in range(8):
                        mm(bb[:, CW:2 * CW], obT[:, h, tok], wb[:, h, :], h == 0, h == 7, [obT, wb], [bb])
                    sga, sgb, y1 = sga_r.next(), sgb_r.next(), y1_r.next()
                    act(sga[:], bga[:, 0:CW], AF.Sigmoid, [bga], [sga])
                    act(sgb[:], bgb[:, 0:CW], AF.Sigmoid, [bgb], [sgb])
                    tt("dve", y1[:], ba[:, CW:2 * CW], sga[:], ALU.mult, [ba, sga], [y1])
                    tt("dve", sgb[:], bb[:, CW:2 * CW], sgb[:], ALU.mult, [bb, sgb], [sgb])
                    tt("dve", ysb[:, j, c0:c1], y1[:], sgb[:], ALU.add, [y1, sgb], [Acc(ysb)])
            P.barrier()
        tap("y", ysb[:].rearrange("p j d -> p (j d)"), ysb)
        for j in range(8):
            for half in range(2):
                bk = bring.next()
                bv = bk.t[:].bitcast(BF16)
                for kk in range(8):
                    k = half * 8 + kk
                    tr(bv[:, kk * 128:(kk + 1) * 128], ysb[:, j, k * 128:(k + 1) * 128], g.identb[:], [ysb, g.identb], [bk])
                cp("dve" if half == 0 else "act", hT[:, half * 8:(half + 1) * 8, j * 128:(j + 1) * 128],
                   bv.rearrange("p (k t) -> p k t", k=8), [bk], [hT])
        with ExitStack() as sw:
            wo_r = Ring([sb(f"wo{i}", [128, 16, 512], BF16, sw) for i in range(2)])
            stg = Ring([sb(f"stgo{i}", [128, 512], F32, sw) for i in range(4)])
            xr = Ring([sb(f"xr{i}", [128, 512], F32, sw) for i in range(3)])
            for n in range(4):
                wo = wo_r.next()
                load_w(g, wo, lambda k: g.w_o[k * 128:(k + 1) * 128, n * 512:(n + 1) * 512], 16, 512, None, stg)
                for j in range(8):
                    tok = slice(j * 128, (j + 1) * 128)
                    bo = bring.next()
                    for k in range(16):
                        mm(bo[:], hT[:, k, tok], wo[:, k, :], k == 0, k == 15, [hT, wo], [bo])
                    x = xr.next()
                    dma("sp", x[:], g.xown[tok, n * 512:(n + 1) * 512], [], [x])
                    tt("dve", x[:], bo[:], x[:], ALU.add, [bo, x], [x])
                    dma("pool", g.xres[tok, n * 512:(n + 1) * 512], x[:], [x], [], wacc=[g.xres_t])
            P.barrier()
    g.xres_dep = [g.xres_t]


def phase4(g):
    P, sb, mm, tr, act, ts, tt, stt, cp, dma, tap = g.P, g.sb, g.mm, g.tr, g.act, g.ts, g.tt, g.stt, g.cp, g.dma, g.tap
    bring, B = g.bring, g.B
    with ExitStack() as sa:
        hT = sb("hTp", [128, 16, 1024], BF16, sa)
        pT = sb("pT", [128, 2, 1024], BF16, sa)
        x3 = sb("x3", [128, 8, 2048], F32, sa)
        gfin = sb("gfin", [128, 2048], F32, sa)
        ssq = sb("ssq", [128, 8, 4], F32, sa)
        dma("sp", gfin[:], g.g_final[0:1, :].to_broadcast([128, 2048]), [], [gfin])
        with ExitStack() as sl:
            own_hT(g, hT, sl, lambda j: g.xres[j * 128:(j + 1) * 128, :], g.gt["g_ple"], 8, "p")
            pt_ = sb("pt_", [128, 256], F32, sl)
            ptb = sb("ptb", [128, 256], BF16, sl)
            for j in range(8):
                dma("sp", pt_[:], g.p_own[j * 128:(j + 1) * 128, :], [], [pt_])
                cp("dve", ptb[:], pt_[:], [pt_], [ptb])
                bk = bring.next()
                bv = bk.t[:].bitcast(BF16)
                for c in range(2):
                    tr(bv[:, c * 128:(c + 1) * 128], ptb[:, c * 128:(c + 1) * 128], g.identb[:], [ptb, g.identb], [bk])
                cp("act", pT[:, :, j * 128:(j + 1) * 128], bv[:, 0:256].rearrange("p (c t) -> p c t", c=2), [bk], [pT])
            P.barrier()
        with ExitStack() as sw:
            wpg_r = Ring([sb(f"wpg{i}", [128, 16, 512], BF16, sw) for i in range(2)])
            wpe_r = Ring([sb(f"wpe{i}", [128, 2, 512], BF16, sw) for i in range(2)])
            stg = Ring([sb(f"stgp{i}", [128, 512], F32, sw) for i in range(4)])
            sg = sb("sgp", [128, 512], F32, sw)
            junk = sb("junkp", [128, 512], F32, sw)
            xr = Ring([sb(f"xrp{i}", [128, 512], F32, sw) for i in range(3)])
            for n in range(4):
                cols = slice(n * 512, (n + 1) * 512)
                wpg, wpe = wpg_r.next(), wpe_r.next()
                load_w(g, wpg, lambda k: g.w_pg[k * 128:(k + 1) * 128, cols], 16, 512, None, stg)
                load_w(g, wpe, lambda k: g.w_pe[k * 128:(k + 1) * 128, cols], 2, 512, None, stg)
                for j in range(8):
                    tok = slice(j * 128, (j + 1) * 128)
                    bg, be = bring.next(), bring.next()
                    for k in range(16):
                        mm(bg[:], hT[:, k, tok], wpg[:, k, :], k == 0, k == 15, [hT, wpg], [bg])
                    for c in range(2):
                        mm(be[:], pT[:, c, tok], wpe[:, c, :], c == 0, c == 1, [pT, wpe], [be])
                    x = xr.next()
                    dma("sp", x[:], g.xres[tok, cols], g.xres_dep, [x])
                    act(sg[:], bg[:], AF.Sigmoid, [bg], [sg])
                    tt("dve", sg[:], be[:], sg[:], ALU.mult, [be, sg], [sg])
                    tt("dve", x3[:, j, cols], x[:], sg[:], ALU.add, [x, sg], [Acc(x3)])
                    act(junk[:], x3[:, j, cols], AF.Square, [x3], [junk, ssq], accum=ssq[:, j, n:n + 1])
            P.barrier()
        tap("x3", x3[:].rearrange("p j d -> p (j d)"), x3)
        with ExitStack() as sf:
            tot = sb("tot", [128, 8], F32, sf)
            tmp = sb("tmpf", [128, 8], F32, sf)
            rs = sb("rsf", [128, 8], F32, sf)
            P.op("dve", lambda e: e.tensor_reduce(out=tot[:], in_=ssq[:], axis=AX.X, op=ALU.add), B([ssq]), B([tot]))
            g.rstd_from_ss(tot, D, tmp, rs, None)
            tap("rsf", rs[:], rs)
            tap("tot", tot[:], tot)
            tap("ssq", ssq[:].rearrange("p j n -> p (j n)"), ssq)
            tap("gfin", gfin[:], gfin)
            for j in range(8):
                stt(x3[:, j, :], x3[:, j, :], rs[:, j:j + 1], gfin[:], ALU.mult, ALU.mult, [x3, rs, gfin], [x3])
                dma("sp", g.out[j * 128:(j + 1) * 128, :], x3[:, j, :], [x3], [])
            P.barrier()


STOP_AFTER = None


def kernel(**inputs):
    maps = prep_inputs(inputs, NPREV=7, ncores=NCORES)
    nc = build(NPREV=7, stop_after=STOP_AFTER)
    res = run_bass_kernel_spmd(nc, maps, core_ids=list(range(NCORES)))
    return np.concatenate([r["out"] for r in res.results], axis=0).reshape(1, NCORES * 1024, D).astype(np.float32)


def phase3(g):
    P, sb, mm, tr, act, ts, tt, stt, cp, dma, tap = g.P, g.sb, g.mm, g.tr, g.act, g.ts, g.tt, g.stt, g.cp, g.dma, g.tap
    bring, banks, B = g.bring, g.banks, g.B
    identf, identb = g.identf, g.identb
    TP, NI = 512, 64
    NEG = -1.0e30
    uT3 = g.peer_uT.rearrange("(k p) e -> p k e", p=128)
    v3 = g.peer_v.rearrange("(i p) d -> p i d", p=128)
    with ExitStack() as s3:
        k1b = sb("k1b", [128, 128], BF16, s3)
        k2b = sb("k2b", [128, 128], BF16, s3)
        with ExitStack() as sl:
            kf = sb("kf", [128, 256], F32, sl)
            dma("sp", kf[:, 0:128], g.k1T, [], [kf])
            dma("sp", kf[:, 128:256], g.k2T, [], [kf])
            cp("dve", k1b[:], kf[:, 0:128], [kf], [k1b])
            cp("dve", k2b[:], kf[:, 128:256], [kf], [k2b])
            P.barrier()
        for ps in range(1024 // TP):
            with ExitStack() as sp_:
                hnT = sb(f"hnT{ps}", [128, 16, TP], BF16, sp_)
                qpT = sb(f"qpT{ps}", [128, 16, TP], BF16, sp_)
                statT = sb(f"statT{ps}", [128, 4, TP], F32, sp_)
                xacc = sb(f"xacc{ps}", [128, 4, 2048], F32, sp_)
                for jl in range(4):
                    dma("sp", xacc[:, jl, :], g.xres[(ps * 4 + jl) * 128:(ps * 4 + jl + 1) * 128, :], g.xres_dep, [xacc])
                with ExitStack() as sl:
                    own_hT(g, hnT, sl, lambda j: g.xres[(ps * 4 + j) * 128:(ps * 4 + j + 1) * 128, :], g.gt["g_ffn"], 4, f"f{ps}")
                    P.barrier()
                with ExitStack() as sq_:
                    wqp_r = Ring([sb(f"wqp{ps}{i}", [128, 16, 512], BF16, sq_) for i in range(2)])
                    stg = Ring([sb(f"stg3{ps}{i}", [128, 512], F32, sq_) for i in range(4)])
                    for mg in range(4):
                        wqp = wqp_r.next()
                        load_w(g, wqp, lambda k: g.peer_wq[k * 128:(k + 1) * 128, mg * 512:(mg + 1) * 512], 16, 512, None, stg)
                        for m4 in range(4):
                            bq = bring.next()
                            for k in range(16):
                                mm(bq[:], wqp[:, k, m4 * 128:(m4 + 1) * 128], hnT[:, k, :], k == 0, k == 15, [wqp, hnT], [bq])
                            cp("act" if m4 % 2 == 0 else "dve", qpT[:, mg * 4 + m4, :], bq[:], [bq], [Acc(qpT)])
                    P.barrier()
                with ExitStack() as sk_:
                    m8 = sb(f"m8{ps}", [128, 16, 16], F32, sk_)
                    wk = sb(f"wk{ps}", [128, 128], F32, sk_)
                    cand = sb(f"cand{ps}", [128, 8, 256], F32, sk_)
                    wk2 = sb(f"wk2{ps}", [128, 256], F32, sk_)
                    c8 = sb(f"c8{ps}", [128, 8, 16], F32, sk_)
                    ex = sb(f"ex{ps}", [128, 8, 16], F32, sk_)
                    zz = sb(f"zz{ps}", [128, 8], F32, sk_)
                    rz = sb(f"rz3{ps}", [128, 8], F32, sk_)
                    st4 = sb(f"st4{ps}", [128, 4, 8, 16], F32, sk_)
                    for jl in range(4):
                        tok = slice(jl * 128, (jl + 1) * 128)
                        bsc = [bring.next() for _ in range(4)]
                        for m in range(16):
                            mm(bsc[m // 4][:, (m % 4) * 128:(m % 4 + 1) * 128], qpT[:, m, tok], (k1b if m % 2 == 0 else k2b)[:],
                               True, True, [qpT, k1b, k2b], [bsc[m // 4]])
                        for m in range(16):
                            sc = bsc[m // 4][:, (m % 4) * 128:(m % 4 + 1) * 128]
                            bb = bsc[m // 4]
                            P.op("dve", lambda e, sc=sc, m=m: e.max(out=m8[:, m, 0:8], in_=sc), B([bb]), B([m8]))
                            P.op("dve", lambda e, sc=sc, m=m: e.match_replace(out=wk[:], in_to_replace=m8[:, m, 0:8], in_values=sc,
                                                                              imm_value=NEG), B([bb, m8]), B([wk]))
                            P.op("dve", lambda e, m=m: e.max(out=m8[:, m, 8:16], in_=wk[:]), B([wk]), B([m8]))
                        m84 = m8[:].rearrange("p (h two) a -> p h two a", two=2)
                        v1 = m84[:, :, 0, :]
                        v2 = m84[:, :, 1, :]
                        tt("dve", cand[:].rearrange("p h (a b) -> p h a b", a=16), v1.unsqueeze(3).to_broadcast([128, 8, 16, 16]),
                           v2.unsqueeze(2).to_broadcast([128, 8, 16, 16]), ALU.add, [m8], [cand])
                        for h in range(8):
                            P.op("dve", lambda e, h=h: e.max(out=c8[:, h, 0:8], in_=cand[:, h, :]), B([cand]), B([c8]))
                            P.op("dve", lambda e, h=h: e.match_replace(out=wk2[:], in_to_replace=c8[:, h, 0:8], in_values=cand[:, h, :],
                                                                       imm_value=NEG), B([cand, c8]), B([wk2]))
                            P.op("dve", lambda e, h=h: e.max(out=c8[:, h, 8:16], in_=wk2[:]), B([wk2]), B([c8]))
                        mx = c8[:, :, 0:1]
                        tau = c8[:, :, 15:16]
                        tt("dve", ex[:], c8[:], mx.to_broadcast([128, 8, 16]), ALU.subtract, [c8], [ex])
                        act(ex[:], ex[:], AF.Exp, [ex], [ex])
                        P.op("dve", lambda e, zz=zz, ex=ex: e.tensor_reduce(out=zz[:], in_=ex[:], axis=AX.X, op=ALU.add), B([ex]), B([zz]))
                        P.op("dve", lambda e, rz=rz, zz=zz: e.reciprocal(out=rz[:], in_=zz[:]), B([zz]), B([rz]))
                        cp("dve", st4[:, 0, :, :], v1, [m8], [st4])
                        tt("dve", st4[:, 1, :, :], v1, mx.to_broadcast([128, 8, 16]), ALU.subtract, [m8, c8], [st4])
                        tt("dve", st4[:, 2, :, :], tau.to_broadcast([128, 8, 16]), v1, ALU.subtract, [m8, c8], [st4])
                        ts("dve", st4[:, 2, :, :], st4[:, 2, :, :], -2.0e-5, None, ALU.add, None, [st4], [st4])
                        cp("dve", st4[:, 3, :, :], rz[:].unsqueeze(2).to_broadcast([128, 8, 16]), [rz], [st4])
                        bt = bring.next()
                        for q in range(4):
                            P.op("pe", lambda e, q=q, bt=bt: e.transpose(out=bt[:, q * 128:(q + 1) * 128],
                                                                         in_=st4[:, q, :, :].rearrange("p h a -> p (h a)"),
                                                                         identity=identf[:]), B([st4, identf]), B([bt]))
                        cp("act", statT[:, :, tok], bt[:].rearrange("p (q t) -> p q t", q=4), [bt], [statT])
                    P.barrier()
                qp4 = qpT.t[:].rearrange("p (h two) t -> p h two t", two=2)
                for sub in range(128 // NI):
                    i0 = sub * NI
                    with ExitStack() as sg_:
                        GT = sb(f"GT{ps}{sub}", [128, NI, TP], BF16, sg_)
                        Ptr = Ring([sb(f"Pt{ps}{sub}{i}", [128, NI], BF16, sg_) for i in range(6)])
                        Er = Ring([sb(f"E{ps}{sub}{i}", [128, 128], F32, sg_) for i in range(6)])
                        Qr = Ring([sb(f"Qt{ps}{sub}{i}", [128, 128], BF16, sg_) for i in range(6)])
                        rS = Ring(banks[0:4])
                        rg = Ring(banks[4:6])
                        q1r = Ring([sb(f"q1r{ps}{sub}{i}", [128, 8, 128], BF16, sg_) for i in range(3)])
                        q2r = Ring([sb(f"q2r{ps}{sub}{i}", [128, 8, 128], BF16, sg_) for i in range(3)])
                        DEPTH = 3
                        Sof = {}
                        qrep = {}

                        def issueS(t):
                            if t % 8 == 0:
                                q1t = q1r.next()
                                q2t = q2r.next()
                                for hf_, qt_ in ((0, q1t), (1, q2t)):
                                    src = qp4[:, :, hf_, t:t + 8].rearrange("p h t -> p t h").unsqueeze(3).to_broadcast([128, 8, 8, 16])
                                    eng_ = ("pool", "dve", "pool", "act")[(t // 8 * 2 + hf_) % 4]
                                    cp(eng_, qt_[:].rearrange("p t (h a) -> p t h a", a=16), src, [qpT], [qt_])
                                qrep[t // 8] = (q1t, q2t)
                            q1t, q2t = qrep[t // 8]
                            bS = rS.next()
                            mm(bS[:, 0:NI], q1t[:, t % 8, :], k1b[:, i0:i0 + NI], True, True, [q1t, k1b], [bS])
                            mm(bS[:, 128:256], q2t[:, t % 8, :], k2b[:], True, True, [q2t, k2b], [bS])
                            Sof[t] = bS

                        for t in range(DEPTH):
                            issueS(t)
                        for t in range(TP):
                            if t + DEPTH < TP:
                                issueS(t + DEPTH)
                            bS = Sof.pop(t)
                            Pt = Ptr.next()
                            E = Er.next()
                            Qt = Qr.next()
                            act(E[:], bS[:, 128:256], AF.Exp, [bS, statT], [E], bias=statT[:, 1, t:t + 1])
                            ts("dve", Pt[:], bS[:, 0:NI], statT[:, 0, t:t + 1], statT[:, 3, t:t + 1], ALU.is_equal, ALU.mult,
                               [bS, statT], [Pt])
                            stt(Qt[:], bS[:, 128:256], statT[:, 2, t:t + 1], E[:], ALU.is_ge, ALU.mult, [bS, statT, E], [Qt])
                            if t % 4 == 0:
                                bg = rg.next()
                            mm(bg[:, (t % 4) * NI:(t % 4 + 1) * NI], Qt[:], Pt[:], True, True, [Qt, Pt], [bg])
                            if t % 4 == 3:
                                cp("act", GT[:, :, t - 3:t + 1].rearrange("p i t -> p t i"),
                                   bg[:, 0:4 * NI].rearrange("p (t i) -> p t i", t=4), [bg], [Acc(GT)])
                        UTr = Ring([sb(f"UT{ps}{sub}{i}", [128, 16, 256], BF16, sg_) for i in range(4)])
                        glr = Ring([sb(f"gl{ps}{sub}{i}", [128, TP], BF16, sg_) for i in range(2)])
                        zr = Ring(banks[6:8])
                        for ig in range(NI // 2):
                            UT = UTr.next()
                            dma("pool", UT[:], uT3[:, :, (i0 + ig * 2) * 128:(i0 + ig * 2 + 2) * 128], [], [UT])
                            for il in range(2):
                                i = ig * 2 + il
                                bz = zr.next()
                                for k in range(16):
                                    mm(bz[:], UT[:, k, il * 128:(il + 1) * 128], hnT[:, k, :], k == 0, k == 15, [UT, hnT], [bz])
                                gl = glr.next()
                                act(gl[:], bz[:], AF.Gelu, [bz], [gl])
                                tt("dve", GT[:, i, :], GT[:, i, :], gl[:], ALU.mult, [GT, gl], [GT])
                        Vr = Ring([sb(f"Vg{ps}{sub}{i}", [128, 4, 512], BF16, sg_) for i in range(4)])
                        acc = banks[0:4]
                        for dc in range(4):
                            for ig in range(NI // 4):
                                Vg = Vr.next()
                                dma("pool", Vg[:], v3[:, i0 + ig * 4:i0 + ig * 4 + 4, dc * 512:(dc + 1) * 512], [], [Vg])
                                for il in range(4):
                                    i = ig * 4 + il
                                    for tq in range(4):
                                        mm(acc[tq][:], GT[:, i, tq * 128:(tq + 1) * 128], Vg[:, il, :], i == 0, i == NI - 1,
                                           [GT, Vg], [acc[tq]])
                            for tq in range(4):
                                tt("dve", xacc[:, tq, dc * 512:(dc + 1) * 512], acc[tq][:], xacc[:, tq, dc * 512:(dc + 1) * 512],
                                   ALU.add, [acc[tq], xacc], [xacc])
                        P.barrier()
                for jl in range(4):
                    dma("sp", g.xres[(ps * 4 + jl) * 128:(ps * 4 + jl + 1) * 128, :], xacc[:, jl, :], [xacc], [], wacc=[g.xres_t])
                P.barrier()
    g.xres_dep = [g.xres_t]
```
